# Optimizing a Trainium2 kernel written in Bass

```python
import math
import jax
import jax.numpy as jnp
from jax import lax
import numpy as np

D_MODEL = 2048
BATCH = 8
SEQ = 4096
DEPTH = 4

N_BRANCH = 4
BRANCH_WIDTH = D_MODEL // N_BRANCH
Q_BLOCK = 128
NORM_EPS = 1e-6

MLA_HEADS = 4
MLA_V_DIM = BRANCH_WIDTH // MLA_HEADS
MLA_NOPE_DIM = MLA_V_DIM
MLA_ROPE_DIM = MLA_NOPE_DIM // 2
MLA_QK_DIM = MLA_NOPE_DIM + MLA_ROPE_DIM
MLA_Q_LORA = D_MODEL // 4
MLA_KV_LORA = D_MODEL // 8
ROPE_THETA = 10000.0

FOX_HEADS = 4
FOX_HEAD_DIM = BRANCH_WIDTH // FOX_HEADS

DSA_HEADS = 4
DSA_HEAD_DIM = BRANCH_WIDTH // DSA_HEADS
IDX_HEADS = 8
IDX_DIM = 64
TOPK_MAX = 256

SWA_HEADS = 8
SWA_KV_HEADS = 2
SWA_HEAD_DIM = BRANCH_WIDTH // SWA_HEADS
WINDOW = 128

N_BUCKETS = 32
MAX_DISTANCE = 128
N_REL_HEADS = DSA_HEADS + SWA_HEADS

D_FF = 11 * D_MODEL // 4
N_EXPERTS = 8
TOP_K_EXPERTS = 2
D_FF_EXPERT = D_FF // 2

IN_WIDTHS = (MLA_Q_LORA, MLA_KV_LORA, MLA_ROPE_DIM,
             FOX_HEADS * FOX_HEAD_DIM, FOX_HEADS * FOX_HEAD_DIM, FOX_HEADS * FOX_HEAD_DIM, FOX_HEADS,
             DSA_HEADS * DSA_HEAD_DIM, DSA_HEAD_DIM, DSA_HEAD_DIM, IDX_HEADS * IDX_DIM, IDX_DIM, IDX_HEADS,
             SWA_HEADS * SWA_HEAD_DIM, SWA_KV_HEADS * SWA_HEAD_DIM, SWA_KV_HEADS * SWA_HEAD_DIM)
IN_COLS = sum(IN_WIDTHS)

kernel_name = 'hybrid_gated_mla_fox_dsa_swa_moe'


def rms_norm(x, g):
    xf = x.astype(jnp.float32)
    y = xf * lax.rsqrt(jnp.mean(xf * xf, axis=-1, keepdims=True) + NORM_EPS)
    return (y * g.astype(jnp.float32)).astype(x.dtype)


def rope(x, pos):
    half = x.shape[-1] // 2
    freqs = ROPE_THETA ** (-jnp.arange(half, dtype=jnp.float32) / half)
    ang = pos.astype(jnp.float32)[:, None] * freqs[None, :]
    cos = jnp.cos(ang)[:, None, :]
    sin = jnp.sin(ang)[:, None, :]
    xf = x.astype(jnp.float32)
    x1, x2 = xf[..., :half], xf[..., half:]
    return jnp.concatenate([x1 * cos - x2 * sin, x1 * sin + x2 * cos], axis=-1).astype(x.dtype)


def rel_bucket(dist):
    n = jnp.maximum(dist, 0)
    max_exact = N_BUCKETS // 2
    nf = jnp.maximum(n, 1).astype(jnp.float32)
    large = max_exact + (jnp.log(nf / max_exact) / math.log(MAX_DISTANCE / max_exact)
                         * (N_BUCKETS - max_exact)).astype(jnp.int32)
    large = jnp.minimum(large, N_BUCKETS - 1)
    return jnp.where(n < max_exact, n, large)


def split_columns(z):
    offsets = np.cumsum(IN_WIDTHS)[:-1].tolist()
    return jnp.split(z, offsets, axis=-1)


def ada_modulation(c, w, b):
    m = jax.nn.silu(c) @ w + b
    return [t[:, None, :] for t in jnp.split(m, 6, axis=-1)]


def causal_block_attention(q, k, v, scale, block_bias=None):
    b, s, h, dk = q.shape
    nb = s // Q_BLOCK
    q_blocks = jnp.moveaxis(q.reshape(b, nb, Q_BLOCK, h, dk), 1, 0)
    key_pos = jnp.arange(s)

    def one_block(args):
        i, qb = args
        q_pos = i * Q_BLOCK + jnp.arange(Q_BLOCK)
        logits = jnp.einsum('bqhd,bkhd->bhqk', qb, k).astype(jnp.float32) * scale
        if block_bias is not None:
            logits = logits + block_bias(i)
        causal = key_pos[None, :] <= q_pos[:, None]
        logits = jnp.where(causal, logits, -jnp.inf)
        p = jax.nn.softmax(logits, axis=-1).astype(v.dtype)
        return jnp.einsum('bhqk,bkhd->bqhd', p, v)

    out = lax.map(one_block, (jnp.arange(nb), q_blocks))
    return jnp.moveaxis(out, 0, 1).reshape(b, s, h, v.shape[-1])


def mla_mixer(cq, ckv, kr, pos, cq_norm, w_uq, ckv_norm, w_ukv, q_norm, k_norm):
    b, s, _ = cq.shape
    q = (rms_norm(cq, cq_norm) @ w_uq).reshape(b, s, MLA_HEADS, MLA_QK_DIM)
    kv = (rms_norm(ckv, ckv_norm) @ w_ukv).reshape(b, s, MLA_HEADS, MLA_NOPE_DIM + MLA_V_DIM)
    k_nope, v = kv[..., :MLA_NOPE_DIM], kv[..., MLA_NOPE_DIM:]
    k_rope = jnp.broadcast_to(kr[:, :, None, :], (b, s, MLA_HEADS, MLA_ROPE_DIM))
    k = jnp.concatenate([k_nope, k_rope], axis=-1)
    q = rms_norm(q, q_norm)
    k = rms_norm(k, k_norm)
    q = jnp.concatenate([q[..., :MLA_NOPE_DIM], rope(q[..., MLA_NOPE_DIM:], pos)], axis=-1)
    k = jnp.concatenate([k[..., :MLA_NOPE_DIM], rope(k[..., MLA_NOPE_DIM:], pos)], axis=-1)
    o = causal_block_attention(q, k, v, MLA_QK_DIM ** -0.5)
    return o.reshape(b, s, BRANCH_WIDTH)


def fox_mixer(q, k, v, f_logit, q_norm, k_norm, f_bias):
    b, s, _ = q.shape
    shp = (b, s, FOX_HEADS, FOX_HEAD_DIM)
    q = rms_norm(q.reshape(shp), q_norm)
    k = rms_norm(k.reshape(shp), k_norm)
    v = v.reshape(shp)
    log_f = jax.nn.log_sigmoid(f_logit.astype(jnp.float32) + f_bias.astype(jnp.float32))
    cum = jnp.moveaxis(jnp.cumsum(log_f, axis=1), 2, 1)

    def decay_bias(i):
        cq = lax.dynamic_slice_in_dim(cum, i * Q_BLOCK, Q_BLOCK, axis=2)
        return cq[:, :, :, None] - cum[:, :, None, :]

    o = causal_block_attention(q, k, v, FOX_HEAD_DIM ** -0.5, decay_bias)
    return o.reshape(b, s, BRANCH_WIDTH)


def dsa_mixer(q, k, v, iq, ik, iw, q_norm, k_norm, rel_table):
    b, s, _ = q.shape
    n_top = min(TOPK_MAX, s // 4)
    nb = s // Q_BLOCK
    q = rms_norm(q.reshape(b, s, DSA_HEADS, DSA_HEAD_DIM), q_norm)
    k = rms_norm(k, k_norm)
    iq = iq.reshape(b, s, IDX_HEADS, IDX_DIM)
    to_blocks = lambda a: jnp.moveaxis(a.reshape((b, nb, Q_BLOCK) + a.shape[2:]), 1, 0)
    key_pos = jnp.arange(s)
    gather = jax.vmap(lambda table, idx: table[idx])

    def one_block(args):
        i, qb, iqb, iwb = args
        q_pos = i * Q_BLOCK + jnp.arange(Q_BLOCK)
        raw = jnp.einsum('bqjd,bsd->bqjs', iqb, ik).astype(jnp.float32) * IDX_DIM ** -0.5
        w = iwb.astype(jnp.float32) * IDX_HEADS ** -0.5
        score = jnp.einsum('bqj,bqjs->bqs', w, jax.nn.relu(raw))
        causal = key_pos[None, :] <= q_pos[:, None]
        score = jnp.where(causal[None], score, -jnp.inf)
        _, idx = lax.top_k(score, n_top)
        valid = idx <= q_pos[None, :, None]
        k_sel = gather(k, idx)
        v_sel = gather(v, idx)
        logits = jnp.einsum('bqhd,bqkd->bhqk', qb, k_sel).astype(jnp.float32) * DSA_HEAD_DIM ** -0.5
        bias = rel_table[rel_bucket(q_pos[None, :, None] - idx)]
        logits = logits + jnp.moveaxis(bias, -1, 1).astype(jnp.float32)
        logits = jnp.where(valid[:, None], logits, -jnp.inf)
        p = jax.nn.softmax(logits, axis=-1).astype(v.dtype)
        return jnp.einsum('bhqk,bqkd->bqhd', p, v_sel)

    out = lax.map(one_block, (jnp.arange(nb), to_blocks(q), to_blocks(iq), to_blocks(iw)))
    return jnp.moveaxis(out, 0, 1).reshape(b, s, BRANCH_WIDTH)


def swa_mixer(q, k, v, q_norm, k_norm, sinks, rel_table):
    b, s, _ = q.shape
    nb = s // Q_BLOCK
    rep = SWA_HEADS // SWA_KV_HEADS
    q = rms_norm(q.reshape(b, s, SWA_HEADS, SWA_HEAD_DIM), q_norm)
    k = rms_norm(k.reshape(b, s, SWA_KV_HEADS, SWA_HEAD_DIM), k_norm)
    v = v.reshape(b, s, SWA_KV_HEADS, SWA_HEAD_DIM)
    qb = q.reshape(b, nb, Q_BLOCK, SWA_KV_HEADS, rep, SWA_HEAD_DIM)
    pad = lambda a: jnp.pad(a, ((0, 0), (Q_BLOCK, 0), (0, 0), (0, 0))).reshape(
        b, nb + 1, Q_BLOCK, SWA_KV_HEADS, SWA_HEAD_DIM)
    kp, vp = pad(k), pad(v)
    kb = jnp.concatenate([kp[:, :-1], kp[:, 1:]], axis=2)
    vb = jnp.concatenate([vp[:, :-1], vp[:, 1:]], axis=2)
    logits = jnp.einsum('bnqgrd,bnkgd->bngrqk', qb, kb).astype(jnp.float32) * SWA_HEAD_DIM ** -0.5
    qi = jnp.arange(Q_BLOCK)[:, None]
    ki = jnp.arange(2 * Q_BLOCK)[None, :]
    dist = qi + Q_BLOCK - ki
    bias = jnp.moveaxis(rel_table[rel_bucket(dist)], -1, 0).astype(jnp.float32)
    bias = bias.reshape(SWA_KV_HEADS, rep, Q_BLOCK, 2 * Q_BLOCK)
    in_window = (dist >= 0) & (dist < WINDOW)
    key_pos = jnp.arange(nb)[:, None, None] * Q_BLOCK - Q_BLOCK + ki[None]
    valid = in_window[None] & (key_pos >= 0)
    logits = jnp.where(valid[None, :, None, None], logits + bias, -jnp.inf)
    sink = sinks.astype(jnp.float32).reshape(SWA_KV_HEADS, rep)[None, None, :, :, None, None]
    m = jnp.maximum(jnp.max(logits, axis=-1, keepdims=True), sink)
    e = jnp.exp(logits - m)
    p = e / (jnp.sum(e, axis=-1, keepdims=True) + jnp.exp(sink - m))
    out = jnp.einsum('bngrqk,bnkgd->bnqgrd', p.astype(v.dtype), vb)
    return out.reshape(b, s, BRANCH_WIDTH)


def swiglu(h, w1, w3, w2):
    return (jax.nn.silu(h @ w1) * (h @ w3)) @ w2


def moe_swiglu(h, router, w1, w3, w2):
    logits = (h @ router).astype(jnp.float32)
    top_val, top_idx = lax.top_k(logits, TOP_K_EXPERTS)
    top_w = jax.nn.softmax(top_val, axis=-1)
    gates = jnp.sum(jax.nn.one_hot(top_idx, N_EXPERTS, dtype=jnp.float32) * top_w[..., None],
                    axis=-2).astype(h.dtype)
    y = gates[..., 0:1] * swiglu(h, w1[0], w3[0], w2[0])
    for e in range(1, N_EXPERTS):
        y = y + gates[..., e:e + 1] * swiglu(h, w1[e], w3[e], w2[e])
    return y


def setup_inputs(seed: int = 0) -> dict:
    key = jax.random.key(seed)
    keys = iter(jax.random.split(key, 48))
    f32 = jnp.float32

    def normal(shape, scale):
        return jax.random.normal(next(keys), shape, f32) * scale

    def gain(shape):
        return 1.0 + normal(shape, 0.05)

    n_dense = (DEPTH + 1) // 2
    n_moe = DEPTH // 2
    return {
        'x': normal((BATCH, SEQ, D_MODEL), 1.0),
        'c': normal((BATCH, D_MODEL), 1.0),
        'ada_w': normal((DEPTH, D_MODEL, 6 * D_MODEL), 0.5 * D_MODEL ** -0.5),
        'ada_b': normal((DEPTH, 6 * D_MODEL), 0.01),
        'norm_mix': gain((DEPTH, D_MODEL)),
        'norm_ffn': gain((DEPTH, D_MODEL)),
        'w_in': normal((DEPTH, D_MODEL, IN_COLS), D_MODEL ** -0.5),
        'mla_cq_norm': gain((DEPTH, MLA_Q_LORA)),
        'mla_w_uq': normal((DEPTH, MLA_Q_LORA, MLA_HEADS * MLA_QK_DIM), MLA_Q_LORA ** -0.5),
        'mla_ckv_norm': gain((DEPTH, MLA_KV_LORA)),
        'mla_w_ukv': normal((DEPTH, MLA_KV_LORA, MLA_HEADS * (MLA_NOPE_DIM + MLA_V_DIM)), MLA_KV_LORA ** -0.5),
        'mla_q_norm': gain((DEPTH, MLA_QK_DIM)),
        'mla_k_norm': gain((DEPTH, MLA_QK_DIM)),
        'fox_q_norm': gain((DEPTH, FOX_HEAD_DIM)),
        'fox_k_norm': gain((DEPTH, FOX_HEAD_DIM)),
        'fox_f_bias': 3.0 + normal((DEPTH, FOX_HEADS), 0.5),
        'dsa_q_norm': gain((DEPTH, DSA_HEAD_DIM)),
        'dsa_k_norm': gain((DEPTH, DSA_HEAD_DIM)),
        'swa_q_norm': gain((DEPTH, SWA_HEAD_DIM)),
        'swa_k_norm': gain((DEPTH, SWA_HEAD_DIM)),
        'swa_sinks': normal((DEPTH, SWA_HEADS), 0.5),
        'rel_bias': normal((N_BUCKETS, N_REL_HEADS), 0.5),
        'w_branch': normal((DEPTH, N_BRANCH, BRANCH_WIDTH, D_MODEL), BRANCH_WIDTH ** -0.5),
        'w_gate': normal((DEPTH, N_BRANCH, D_MODEL, D_MODEL), D_MODEL ** -0.5),
        'w_out': normal((DEPTH, D_MODEL, D_MODEL), D_MODEL ** -0.5),
        'ffn_w1': normal((n_dense, D_MODEL, D_FF), D_MODEL ** -0.5),
        'ffn_w3': normal((n_dense, D_MODEL, D_FF), D_MODEL ** -0.5),
        'ffn_w2': normal((n_dense, D_FF, D_MODEL), D_FF ** -0.5),
        'moe_router': normal((n_moe, D_MODEL, N_EXPERTS), D_MODEL ** -0.5),
        'moe_w1': normal((n_moe, N_EXPERTS, D_MODEL, D_FF_EXPERT), D_MODEL ** -0.5),
        'moe_w3': normal((n_moe, N_EXPERTS, D_MODEL, D_FF_EXPERT), D_MODEL ** -0.5),
        'moe_w2': normal((n_moe, N_EXPERTS, D_FF_EXPERT, D_MODEL), D_FF_EXPERT ** -0.5),
    }


def reference(x, c, ada_w, ada_b, norm_mix, norm_ffn, w_in,
              mla_cq_norm, mla_w_uq, mla_ckv_norm, mla_w_ukv, mla_q_norm, mla_k_norm,
              fox_q_norm, fox_k_norm, fox_f_bias,
              dsa_q_norm, dsa_k_norm,
              swa_q_norm, swa_k_norm, swa_sinks,
              rel_bias, w_branch, w_gate, w_out,
              ffn_w1, ffn_w3, ffn_w2,
              moe_router, moe_w1, moe_w3, moe_w2):
    b, s, _ = x.shape
    pos = jnp.arange(s)
    for layer in range(DEPTH):
        shift1, scale1, gate1, shift2, scale2, gate2 = ada_modulation(c, ada_w[layer], ada_b[layer])
        h = rms_norm(x, norm_mix[layer]) * (1 + scale1) + shift1
        (cq, ckv, kr, fq, fk, fv, fg, dq, dk, dv, iq, ik, iw, sq, sk, sv) = split_columns(h @ w_in[layer])
        o_mla = mla_mixer(cq, ckv, kr, pos, mla_cq_norm[layer], mla_w_uq[layer], mla_ckv_norm[layer],
                          mla_w_ukv[layer], mla_q_norm[layer], mla_k_norm[layer])
        o_fox = fox_mixer(fq, fk, fv, fg, fox_q_norm[layer], fox_k_norm[layer], fox_f_bias[layer])
        o_dsa = dsa_mixer(dq, dk, dv, iq, ik, iw, dsa_q_norm[layer], dsa_k_norm[layer],
                          rel_bias[:, :DSA_HEADS])
        o_swa = swa_mixer(sq, sk, sv, swa_q_norm[layer], swa_k_norm[layer], swa_sinks[layer],
                          rel_bias[:, DSA_HEADS:])
        merged = None
        for i, o in enumerate((o_mla, o_fox, o_dsa, o_swa)):
            term = jax.nn.sigmoid(h @ w_gate[layer, i]) * (o @ w_branch[layer, i])
            merged = term if merged is None else merged + term
        x = x + gate1 * (merged @ w_out[layer])
        h = rms_norm(x, norm_ffn[layer]) * (1 + scale2) + shift2
        if layer % 2 == 0:
            j = layer // 2
            y = swiglu(h, ffn_w1[j], ffn_w3[j], ffn_w2[j])
        else:
            j = layer // 2
            y = moe_swiglu(h, moe_router[j], moe_w1[j], moe_w3[j], moe_w2[j])
        x = x + gate2 * y
    return x
```

```python
import contextlib
import math
import numpy as np
import ml_dtypes
import concourse.bass as bass
import concourse.mybir as mybir
from concourse.bass_utils import run_bass_kernel_spmd

F32 = mybir.dt.float32
BF16 = mybir.dt.bfloat16
AF = mybir.ActivationFunctionType
ALU = mybir.AluOpType
AX = mybir.AxisListType
NPBF = ml_dtypes.bfloat16

S = 4096
D = 2048
NT = 32
NST = 8
DEPTH = 4
EPS = 1e-6
NEG = -30000.0

SM_OFF = {}
_o = 0
for _n, _w in (("cq", 512), ("ckv", 256), ("mq", 192), ("mk", 192), ("fq", 128), ("fk", 128),
               ("fb", 4), ("dq", 128), ("dk", 128), ("sq", 64), ("sk", 64), ("sink", 8)):
    SM_OFF[_n] = (_o, _w)
    _o += _w
NSM = _o

B_QN, B_QR, B_KN, B_KR, B_FQ, B_FK, B_DQ, B_DK, B_IK, B_IQ, B_SQ, B_SK = 0, 4, 6, 10, 12, 16, 20, 24, 25, 26, 30, 34
NFB = 35
V_MLA, V_FOX, V_DSA, V_SWA, NVT = 0, 512, 1024, 1152, 1280


class Sem:
    def __init__(self, h, name):
        self.h = h
        self.name = name


class Buf:
    def __init__(self, t, name):
        self.t = t
        self.name = name
        self.w = {}
        self.r = {}
        self.ds = None
        self.persist = False

    def __getitem__(self, k):
        return self.t[k]


class Eng:
    def __init__(self, name, e, sem):
        self.name = name
        self.e = e
        self.sem = sem
        self.cnt = 0
        self.seen = {}

    def waitd(self, d):
        for sem, val in d.items():
            if sem is self.sem and val > self.cnt:
                continue
            if self.seen.get(sem, 0) >= val:
                continue
            self.e.wait_ge(sem.h, val)
            self.seen[sem] = val


class K:
    def __init__(self):
        self.nc = bass.Bass("TRN2", target_bir_lowering=False)
        self.es = contextlib.ExitStack()
        self.sems = []
        self.bufs = []
        nc = self.nc
        self.E = {}
        for n, e in (("pe", nc.tensor), ("act", nc.scalar), ("dve", nc.vector),
                     ("pool", nc.gpsimd), ("sp", nc.sync)):
            self.E[n] = Eng(n, e, self.sem("prog_" + n))
        self.phase = None
        self.uid = 0
        self.dsp = DsPool(self)

    def sem(self, name):
        s = Sem(self.es.enter_context(self.nc.semaphore(name)), name)
        self.sems.append(s)
        return s

    def begin_phase(self):
        self.phase = contextlib.ExitStack()

    def end_phase(self):
        self.barrier()
        self.phase.close()
        self.phase = None

    def _reg(self, t, name):
        b = Buf(t, name)
        self.bufs.append(b)
        return b

    def sb(self, name, shape, dt, persist=False):
        st = self.es if (persist or self.phase is None) else self.phase
        self.uid += 1
        t = st.enter_context(self.nc.sbuf_tensor("%s_%d" % (name, self.uid), list(shape), dt))
        b = self._reg(t, name)
        b.persist = persist or self.phase is None
        return b

    def ps(self, name, shape, dt):
        t = self.es.enter_context(self.nc.psum_tensor(name, list(shape), dt))
        return self._reg(t, name)

    def dram(self, name, shape, dt, kind="Internal"):
        t = self.nc.dram_tensor(name, list(shape), dt, kind=kind)
        return self._reg(t.ap(), name)

    def op(self, eng, fn, r=(), w=(), sig=True):
        E = self.E[eng]
        for b in r:
            E.waitd(b.w)
        for b in w:
            E.waitd(b.w)
            E.waitd(b.r)
        ins = fn(E.e)
        if sig:
            E.cnt += 1
            ins.then_inc(E.sem.h, 1)
            val = E.cnt
        else:
            val = E.cnt + 1
        for b in r:
            b.r[E.sem] = max(b.r.get(E.sem, 0), val)
        for b in w:
            b.w[E.sem] = max(b.w.get(E.sem, 0), val)
        return ins

    def mm(self, out_b, out_ap, pairs, r, start=True, stop=True):
        n = len(pairs)
        for i, (l, rh) in enumerate(pairs):
            self.op("pe", lambda e, l=l, rh=rh, i=i: e.matmul(
                out_ap, lhsT=l, rhs=rh, start=(start and i == 0), stop=(stop and i == n - 1)),
                r=r if i == 0 else (), w=(out_b,), sig=(i == n - 1))

    def dma(self, q, out_b, out_ap, in_b, in_ap, side):
        E = self.E[q]
        E.waitd(in_b.w)
        E.waitd(out_b.w)
        E.waitd(out_b.r)
        if side.ds is None:
            side.ds = self.dsp.get(side.persist)
        ins = E.e.dma_start(out=out_ap, in_=in_ap)
        side.ds[1] += 16
        ins.then_inc(side.ds[0].h, 16)
        s, v = side.ds
        in_b.r[s] = max(in_b.r.get(s, 0), v)
        out_b.w[s] = max(out_b.w.get(s, 0), v)
        return ins

    def barrier(self):
        allb = {E.sem: E.cnt for E in self.E.values()}
        for b in self.bufs:
            if b.ds is not None:
                allb[b.ds[0]] = max(allb.get(b.ds[0], 0), b.ds[1])
        for E in self.E.values():
            E.waitd(allb)


class DsPool:
    def __init__(self, k):
        self.k = k
        self.free = []
        self.used = []

    def get(self, persist=False):
        if persist:
            return [self.k.sem("dmap%d" % len(self.k.sems)), 0]
        c = self.free.pop() if self.free else [self.k.sem("dmas%d" % len(self.k.sems)), 0]
        self.used.append(c)
        return c

    def recycle(self):
        self.free.extend(self.used)
        self.used = []


def _bcast_row(buf, row_ap):
    return row_ap.partition_broadcast(128)


def build_program(nlayers=DEPTH, debug=False, stop=None):
    k = K()
    nc = k.nc
    dsp = k.dsp

    def sbt(name, shape, dt, persist=False):
        return k.sb(name, shape, dt, persist)

    op, mm, dma = k.op, k.mm, k.dma

    def din(name, shape, dt=F32):
        return k.dram(name, shape, dt, "ExternalInput")

    x_in = din("x", [S, D])
    cT_in = din("cT", [128, 16])
    ada_w = din("ada_w", [nlayers * D, 6 * D])
    ada_b = din("ada_b", [nlayers, 6 * D])
    norm_mix = din("norm_mix", [nlayers, D])
    norm_ffn = din("norm_ffn", [nlayers, D])
    small = din("small", [nlayers, NSM])
    w_in = din("w_in_p", [nlayers * D, 5120])
    w_uq = din("w_uq_p", [nlayers * 512, 768])
    w_ukv = din("w_ukv_p", [nlayers * 256, 1024])
    rel_bias = din("rel_bias", [32, 12])
    w_branch = din("w_branch", [nlayers * 4 * 512, D])
    w_gate = din("w_gate", [nlayers * 4 * D, D])
    w_out = din("w_out", [nlayers * D, D])
    nf_ = (nlayers + 1) // 2
    nm_ = nlayers // 2
    ffn_w1 = din("ffn_w1", [nf_ * D, 5632])
    ffn_w3 = din("ffn_w3", [nf_ * D, 5632])
    ffn_w2 = din("ffn_w2", [nf_ * 5632, D])
    moe_router = din("moe_router", [nm_ * D, 8]) if nm_ else None
    moe_w1 = din("moe_w1", [nm_ * 8 * D, 2816]) if nm_ else None
    moe_w3 = din("moe_w3", [nm_ * 8 * D, 2816]) if nm_ else None
    moe_w2 = din("moe_w2", [nm_ * 8 * 2816, D]) if nm_ else None
    ident_in = din("ident", [128, 128], BF16)
    cs_in = din("cs_tab", [S, 64])
    negcm_in = din("negcm", [128, 4 * 512])
    negtri_in = din("negtri", [128, 128])
    ohd_in = din("ohd", [32, 384])
    ohs_in = din("ohs", [32, 384])
    negw_in = din("negw", [128, 256])
    tri_in = din("tri", [128, 128])
    y_out = k.dram("y", [S, D], F32, "ExternalOutput")

    XA = k.dram("XA", [S, D], F32)
    XB = k.dram("XB", [S, D], F32)
    HT = k.dram("HT", [16 * 128, S], BF16)
    FT = k.dram("FT", [NFB * 128, S], BF16)
    VT = k.dram("VT", [S, NVT], BF16)
    OT = k.dram("OT", [D, S], BF16)
    CUML = k.dram("CUML", [S, 4], F32)
    CUMT = k.dram("CUMT", [4, S], F32)
    IW = k.dram("IW", [S, 8], F32)
    MOD = k.dram("MOD", [nlayers, 6 * D], F32)
    TD = k.dram("TD", [12 * 128, 384], F32)
    MTD = k.dram("MTD", [NST * 128, 32 * 512], BF16)
    WSETS = []
    for si in range(2):
        WSETS.append(dict(
            WIN=k.dram("WIN%d" % si, [D, 5120], BF16), WUQ=k.dram("WUQ%d" % si, [512, 768], BF16),
            WUKV=k.dram("WUKV%d" % si, [256, 1024], BF16), WG=k.dram("WG%d" % si, [4 * D, D], BF16),
            WB=k.dram("WB%d" % si, [4 * 512, D], BF16), WO=k.dram("WO%d" % si, [D, D], BF16),
            FW1=k.dram("FW1_%d" % si, [8 * D, 2816], BF16), FW3=k.dram("FW3_%d" % si, [8 * D, 2816], BF16),
            FW2=k.dram("FW2_%d" % si, [8 * 2816, D], BF16)))
    dbg = {}
    if debug:
        for nm, shp, dt in (("dbg_ft", [NFB * 128, S], BF16), ("dbg_vt", [S, NVT], BF16),
                            ("dbg_ot", [D, S], BF16), ("dbg_xa", [S, D], F32),
                            ("dbg_mod", [nlayers, 2048], F32), ("dbg_cum", [S, 4], F32)):
            dbg[nm] = k.dram(nm, shp, dt, "ExternalOutput")

    PB = [k.ps("pb%d" % i, [128, 512], F32) for i in range(8)]

    def pbf(i):
        return PB[i][:].bitcast(BF16)

    ident = k.sb("ident", [128, 128], BF16, True)
    ones_b = k.sb("ones_b", [128, 128], BF16, True)
    ones_f = k.sb("ones_f", [128, 512], F32, True)
    negcm = k.sb("negcm", [128, 4, 512], F32, True)
    negtri = k.sb("negtri", [128, 128], F32, True)
    tri = k.sb("tri", [128, 128], F32, True)
    ew2 = k.sb("ew2", [128, 4, 256], BF16, True)
    ebs = k.sb("ebs", [128, 2, 2, 512], BF16, True)
    smt = k.sb("smt", [128, NSM], F32, True)
    neg29 = k.sb("neg29", [128, 1], F32, True)

    dma("sp", ident, ident[:], ident_in, ident_in[:, :], ident)
    dma("sp", negcm, negcm[:].rearrange("p a b -> p (a b)"), negcm_in, negcm_in[:, :], negcm)
    dma("sp", negtri, negtri[:], negtri_in, negtri_in[:, :], negtri)
    dma("sp", tri, tri[:], tri_in, tri_in[:, :], tri)
    op("dve", lambda e: e.memset(ones_b[:], 1.0), w=(ones_b,))
    op("dve", lambda e: e.memset(ones_f[:], 1.0), w=(ones_f,))
    op("dve", lambda e: e.memset(neg29[:], -1e29), w=(neg29,))

    rr = [0]

    def cast_eng():
        rr[0] += 1
        return ("act", "dve", "pool")[rr[0] % 3]

    def copy_op(eng, out_ap, in_ap, r, w):
        if eng == "act":
            op("act", lambda e: e.copy(out=out_ap, in_=in_ap), r=r, w=w)
        else:
            op(eng, lambda e: e.tensor_copy(out=out_ap, in_=in_ap), r=r, w=w)

    k.begin_phase()
    ct = sbt("ct", [128, 16], F32)
    sc = sbt("sc", [128, 16], F32)
    awt = [sbt("awt%d" % i, [128, 16, 512], F32) for i in range(2)]
    abt = [sbt("abt%d" % i, [1, 512], F32) for i in range(2)]
    mrow = [sbt("mrow%d" % i, [1, 512], F32) for i in range(2)]
    dma("sp", ct, ct[:], cT_in, cT_in[:, :], ct)
    op("act", lambda e: e.activation(out=sc[:], in_=ct[:], func=AF.Silu), r=(ct,), w=(sc,))
    it = 0
    for L in range(nlayers):
        for n in range(24):
            a, bt, mr = awt[it % 2], abt[it % 2], mrow[it % 2]
            src = ada_w[L * D:(L + 1) * D, n * 512:(n + 1) * 512].rearrange("(k p) n -> p k n", p=128)
            dma("sp", a, a[:], ada_w, src, a)
            dma("sp", bt, bt[:], ada_b, ada_b[L:L + 1, n * 512:(n + 1) * 512], bt)
            pz = PB[it % 2]
            mm(pz, pz[0:1, :], [(sc[:, kc:kc + 1], a[:, kc, :]) for kc in range(16)], r=(sc, a))
            op("dve", lambda e, mr=mr, pz=pz, bt=bt: e.tensor_tensor(out=mr[:], in0=pz[0:1, :], in1=bt[:], op=ALU.add),
               r=(pz, bt), w=(mr,))
            dma("pool", MOD, MOD[L:L + 1, n * 512:(n + 1) * 512], mr, mr[:], mr)
            it += 1
    k.end_phase()
    dsp.recycle()

    k.begin_phase()
    relt = sbt("relt", [32, 12], F32)
    oht = [sbt("ohd_t", [32, 384], F32), sbt("ohs_t", [32, 384], F32)]
    negw = sbt("negw", [128, 256], F32)
    tbc = [sbt("tbc%d" % i, [32, 128], F32) for i in range(2)]
    tdt = [sbt("tdt%d" % i, [128, 384], F32) for i in range(2)]
    w2t = [sbt("w2t%d" % i, [128, 256], F32) for i in range(2)]
    dma("sp", relt, relt[:], rel_bias, rel_bias[:, :], relt)
    dma("sp", oht[0], oht[0][:], ohd_in, ohd_in[:, :], oht[0])
    dma("sp", oht[1], oht[1][:], ohs_in, ohs_in[:, :], oht[1])
    dma("sp", negw, negw[:], negw_in, negw_in[:, :], negw)
    for h in range(12):
        tb, td, w2 = tbc[h % 2], tdt[h % 2], w2t[h % 2]
        oh = oht[0] if h < 4 else oht[1]
        op("dve", lambda e, tb=tb, h=h: e.tensor_copy(out=tb[:], in_=relt[:, h:h + 1].to_broadcast([32, 128])),
           r=(relt,), w=(tb,))
        pz = PB[h % 2]
        mm(pz, pz[:, 0:384], [(tb[:], oh[:])], r=(tb, oh))
        op("act", lambda e, td=td, pz=pz: e.copy(out=td[:], in_=pz[:, 0:384]), r=(pz,), w=(td,))
        dma("pool", TD, TD[h * 128:(h + 1) * 128, :], td, td[:], td)
        skew = bass.AP(TD.t.tensor, h * 128 * 384 + 127, [[383, 128], [1, 256]])
        dma("sp", w2, w2[:], TD, skew, w2)
        if h < 4:
            op("act", lambda e, w2=w2, h=h: e.activation(out=ew2[:, h, :], in_=w2[:], func=AF.Exp), r=(w2,), w=(ew2,))
        else:
            hh = h - 4
            g, hi = hh // 4, hh % 4
            op("dve", lambda e, w2=w2: e.tensor_tensor(out=w2[:], in0=w2[:], in1=negw[:], op=ALU.add), r=(w2, negw), w=(w2,))
            for rel in range(2):
                cs = slice(128, 256) if rel == 0 else slice(0, 128)
                op("act", lambda e, w2=w2, g=g, rel=rel, hi=hi, cs=cs: e.activation(
                    out=ebs[:, g, rel, hi * 128:(hi + 1) * 128], in_=w2[:, cs], func=AF.Exp), r=(w2,), w=(ebs,))
    k.end_phase()
    dsp.recycle()

    BGW = 1024
    bgf = [k.sb("bgf%d" % i, [128, BGW], F32, True) for i in range(2)]
    bgb = [k.sb("bgb%d" % i, [128, BGW], BF16, True) for i in range(2)]

    def precast_items(L, BGW=BGW):
        W = WSETS[L % 2]
        j2 = L // 2
        items = []

        def add(src_b, src_ap, dst_b, dst_ap, R, C):
            ncb = -(-C // BGW)
            while C % ncb:
                ncb += 1
            cb = C // ncb
            nr = max(1, BGW // cb)
            nblk = R // 128
            for c in range(ncb):
                for b0 in range(0, nblk, nr):
                    n = min(nr, nblk - b0)
                    sv = src_ap[b0 * 128:(b0 + n) * 128, c * cb:(c + 1) * cb].rearrange("(n p) c -> p n c", p=128)
                    dv = dst_ap[b0 * 128:(b0 + n) * 128, c * cb:(c + 1) * cb].rearrange("(n p) c -> p n c", p=128)
                    items.append((src_b, sv, dst_b, dv, n, cb))
        add(w_in, w_in[L * D:(L + 1) * D, :], W["WIN"], W["WIN"][:, :], D, 5120)
        add(w_uq, w_uq[L * 512:(L + 1) * 512, :], W["WUQ"], W["WUQ"][:, :], 512, 768)
        add(w_ukv, w_ukv[L * 256:(L + 1) * 256, :], W["WUKV"], W["WUKV"][:, :], 256, 1024)
        add(w_gate, w_gate[L * 4 * D:(L + 1) * 4 * D, :], W["WG"], W["WG"][:, :], 4 * D, D)
        add(w_branch, w_branch[L * 2048:(L + 1) * 2048, :], W["WB"], W["WB"][:, :], 2048, D)
        add(w_out, w_out[L * D:(L + 1) * D, :], W["WO"], W["WO"][:, :], D, D)
        if L % 2 == 1:
            add(moe_w1, moe_w1[j2 * 8 * D:(j2 + 1) * 8 * D, :], W["FW1"], W["FW1"][:, :], 8 * D, 2816)
            add(moe_w3, moe_w3[j2 * 8 * D:(j2 + 1) * 8 * D, :], W["FW3"], W["FW3"][:, :], 8 * D, 2816)
            add(moe_w2, moe_w2[j2 * 8 * 2816:(j2 + 1) * 8 * 2816, :], W["FW2"], W["FW2"][:, :], 8 * 2816, D)
        else:
            for e_ in range(2):
                add(ffn_w1, ffn_w1[j2 * D:(j2 + 1) * D, e_ * 2816:(e_ + 1) * 2816], W["FW1"], W["FW1"][e_ * D:(e_ + 1) * D, :], D, 2816)
                add(ffn_w3, ffn_w3[j2 * D:(j2 + 1) * D, e_ * 2816:(e_ + 1) * 2816], W["FW3"], W["FW3"][e_ * D:(e_ + 1) * D, :], D, 2816)
            add(ffn_w2, ffn_w2[j2 * 5632:(j2 + 1) * 5632, :], W["FW2"], W["FW2"][0:5632, :], 5632, D)
        return items

    class Bg:
        def __init__(self):
            self.gen = None
            self.rate = 0.0
            self.credit = 0.0

        def start(self, L, hooks, fg=False):
            if fg:
                self.tf = [sbt("pcf%d" % i, [128, 4096], F32) for i in range(2)]
                self.tb = [sbt("pcb%d" % i, [128, 4096], BF16) for i in range(2)]
                items = precast_items(L, 4096)
            else:
                self.tf, self.tb = bgf, bgb
                items = precast_items(L)
            self.gen = self._run(items, fg)
            self.rate = len(items) / float(hooks)
            self.credit = 0.0

        def _run(self, items, fg=False):
            q = "sp" if fg else "pool"

            def load(t):
                src_b, sv, dst_b, dv, n, cb = items[t]
                ft = self.tf[t % 2]
                dma(q, ft, ft[:, 0:n * cb].rearrange("p (n c) -> p n c", n=n), src_b, sv, ft)
            load(0)
            for t in range(len(items)):
                src_b, sv, dst_b, dv, n, cb = items[t]
                ft, bt = self.tf[t % 2], self.tb[t % 2]
                if t + 1 < len(items):
                    load(t + 1)
                copy_op(("act", "dve", "pool")[t % 3] if fg else "pool", bt[:, 0:n * cb], ft[:, 0:n * cb], (ft,), (bt,))
                dma("pool", dst_b, dv, bt, bt[:, 0:n * cb].rearrange("p (n c) -> p n c", n=n), bt)
                yield

        def tick(self, w=1.0):
            if self.gen is None:
                return
            self.credit += self.rate * w
            while self.credit >= 1.0 and self.gen is not None:
                self.credit -= 1.0
                self._step()

        def _step(self):
            try:
                next(self.gen)
            except StopIteration:
                self.gen = None

        def finish(self):
            while self.gen is not None:
                self._step()

    bg = Bg()

    def rstd_from_ssq(ssq_ap, out_ap, d, bufs):
        op("act", lambda e: e.activation(out=out_ap, in_=ssq_ap, func=AF.Sqrt, scale=1.0 / d, bias=EPS), r=bufs, w=bufs)
        op("dve", lambda e: e.reciprocal(out=out_ap, in_=out_ap), r=bufs, w=bufs)

    def load_bcast(tile_b, tile_ap, src_b, row_ap):
        dma("sp", tile_b, tile_ap, src_b, row_ap.partition_broadcast(128), tile_b)

    def finish():
        if debug:
            k.begin_phase()
            cp = [sbt("cp%d" % i, [128, 4096], BF16) for i in range(2)]
            cpf = [sbt("cpf%d" % i, [128, 2048], F32) for i in range(2)]
            n = 0
            for src, dst, rows in ((FT, dbg["dbg_ft"], NFB * 128), (OT, dbg["dbg_ot"], D)):
                for r0 in range(0, rows, 128):
                    t = cp[n % 2]
                    n += 1
                    dma("sp", t, t[:], src, src[r0:r0 + 128, :], t)
                    dma("pool", dst, dst[r0:r0 + 128, :], t, t[:], t)
            for r0 in range(0, S, 128):
                t = cp[n % 2]
                n += 1
                dma("sp", t, t[:, 0:NVT], VT, VT[r0:r0 + 128, :], t)
                dma("pool", dbg["dbg_vt"], dbg["dbg_vt"][r0:r0 + 128, :], t, t[:, 0:NVT], t)
                t = cpf[n % 2]
                dma("sp", t, t[:], XA, XA[r0:r0 + 128, :], t)
                dma("pool", dbg["dbg_xa"], dbg["dbg_xa"][r0:r0 + 128, :], t, t[:], t)
                t = cpf[(n + 1) % 2]
                dma("sp", t, t[:, 0:4], CUML, CUML[r0:r0 + 128, :], t)
                dma("pool", dbg["dbg_cum"], dbg["dbg_cum"][r0:r0 + 128, :], t, t[:, 0:4], t)
            t = cpf[0]
            dma("sp", t, t[0:nlayers, :], MOD, MOD[:, 0:2048], t)
            dma("pool", dbg["dbg_mod"], dbg["dbg_mod"][:, :], t, t[0:nlayers, :], t)
            k.end_phase()
        k.barrier()
        k.es.close()
        return nc

    xcur = x_in
    for L in range(nlayers):
        moe = (L % 2 == 1)
        j2 = L // 2
        NE = 8 if moe else 2
        last = (L == nlayers - 1)
        xmid = XA
        xnext = y_out if last else XB

        if L == 0:
            k.begin_phase()
            bg.start(0, 1, fg=True)
            bg.finish()
            k.end_phase()
            dsp.recycle()
        bg.finish()
        k.barrier()
        WS = WSETS[L % 2]
        WIN, WUQ, WUKV, WG, WB, WO, FW1, FW3, FW2 = (WS[n_] for n_ in ("WIN", "WUQ", "WUKV", "WG", "WB", "WO", "FW1", "FW3", "FW2"))
        if not last:
            bg.start(L + 1, 5200 if moe else 4300)

        k.begin_phase()
        load_bcast(smt, smt[:], small, small[L:L + 1, :])
        gm = sbt("gm", [128, D], F32)
        sh = sbt("sh", [128, D], F32)
        tmpD = sbt("tmpD", [128, D], F32)
        load_bcast(gm, gm[:], MOD, MOD[L:L + 1, D:2 * D])
        load_bcast(tmpD, tmpD[:], norm_mix, norm_mix[L:L + 1, :])
        op("dve", lambda e: e.scalar_tensor_tensor(out=gm[:], in0=gm[:], scalar=1.0, in1=tmpD[:], op0=ALU.add, op1=ALU.mult),
           r=(gm, tmpD), w=(gm,))
        load_bcast(sh, sh[:], MOD, MOD[L:L + 1, 0:D])
        xt = [sbt("xt%d" % i, [128, D], F32) for i in range(2)]
        hb = sbt("hb", [128, D], BF16)
        hts = sbt("hts", [128, 16, 512], BF16)
        wint = [sbt("wint%d" % i, [128, 16, 512], BF16) for i in range(2)]
        wuqt = sbt("wuqt", [128, 4, 768], BF16)
        wukvt = sbt("wukvt", [128, 2, 1024], BF16)
        fts = sbt("fts", [128, NFB, 512], BF16)
        vts = [sbt("vts%d" % i, [128, NVT], BF16) for i in range(4)]
        zc = sbt("zc", [128, 1024], F32)
        sq = sbt("sq", [128, 1024], F32)
        nb = sbt("nb", [128, 1024], BF16)
        nT = sbt("nT", [128, 4, 128], BF16)
        st8 = sbt("st8", [128, 16], F32)
        rot = sbt("rot", [128, 4, 64], F32)
        rt = sbt("rt", [128, 4, 4, 32], F32)
        cst = [sbt("cst%d" % i, [128, 64], F32) for i in range(4)]
        iwt = sbt("iwt", [128, 8], F32)
        lft = sbt("lft", [128, 4], F32)
        cumt = sbt("cumt", [128, 4], F32)
        runt = sbt("runt", [128, 4], F32)
        cumTt = sbt("cumTt", [4, 128], F32)
        identf = sbt("identf", [128, 128], F32)
        dma("sp", wuqt, wuqt[:], WUQ, WUQ[:, :].rearrange("(k p) n -> p k n", p=128), wuqt)
        dma("sp", wukvt, wukvt[:], WUKV, WUKV[:, :].rearrange("(k p) n -> p k n", p=128), wukvt)
        op("dve", lambda e: e.memset(runt[:], 0.0), w=(runt,))
        op("dve", lambda e: e.tensor_copy(out=identf[:], in_=ident[:]), r=(ident,), w=(identf,))
        op("dve", lambda e: e.memset(fts[:, B_IK, :], 0.0), w=(fts,))

        def sm(name):
            o, w_ = SM_OFF[name]
            return smt[:, o:o + w_]

        def hnorm(src3, H, d, gain_ap, out3, extra=None):
            sq3 = sq[:, 0:H * d].rearrange("p (h d) -> p h d", h=H)
            op("dve", lambda e: e.tensor_tensor(out=sq3, in0=src3, in1=src3, op=ALU.mult), r=(zc,), w=(sq,))
            op("dve", lambda e: e.tensor_reduce(out=st8[:, 0:H], in_=sq3, axis=AX.X, op=ALU.add), r=(sq,), w=(st8,))
            rstd_from_ssq(st8[:, 0:H], st8[:, 0:H], d, (st8,))
            op("dve", lambda e: e.tensor_tensor(out=sq3, in0=src3, in1=st8[:, 0:H].unsqueeze(2).to_broadcast([128, H, d]), op=ALU.mult),
               r=(zc, st8), w=(sq,))
            op("dve", lambda e: e.tensor_tensor(out=out3, in0=sq3, in1=gain_ap.unsqueeze(1).to_broadcast([128, H, d]), op=ALU.mult),
               r=(sq, smt), w=(nb,))

        def transposes(src_b, blocks, bank, dst_b, dst_ap, rows=128):
            n = len(blocks)
            pv = pbf(bank)
            for i, bap in enumerate(blocks):
                op("pe", lambda e, i=i, bap=bap: e.transpose(out=pv[0:rows, i * 128:(i + 1) * 128], in_=bap, identity=ident[:]),
                   r=(src_b, ident) if i == 0 else (), w=(PB[bank],), sig=(i == n - 1))
            op("act", lambda e: e.copy(out=dst_ap, in_=pv[0:rows, 0:n * 128].rearrange("p (n t) -> p n t", n=n)),
               r=(PB[bank],), w=(dst_b,))

        def rope_apply(src3, out3, cs, H):
            cosb = cs[:, 0:32].unsqueeze(1).to_broadcast([128, H, 32])
            sinb = cs[:, 32:64].unsqueeze(1).to_broadcast([128, H, 32])
            x1, x2 = src3[:, :, 0:32], src3[:, :, 32:64]
            op("dve", lambda e: e.tensor_tensor(out=rt[:, 0], in0=x1, in1=cosb, op=ALU.mult), r=(rot, cst_b[0]), w=(rt,))
            op("dve", lambda e: e.tensor_tensor(out=rt[:, 1], in0=x2, in1=sinb, op=ALU.mult), r=(rot, cst_b[0]), w=(rt,))
            op("dve", lambda e: e.tensor_tensor(out=rt[:, 2], in0=x1, in1=sinb, op=ALU.mult), r=(rot, cst_b[0]), w=(rt,))
            op("dve", lambda e: e.tensor_tensor(out=rt[:, 3], in0=x2, in1=cosb, op=ALU.mult), r=(rot, cst_b[0]), w=(rt,))
            op("dve", lambda e: e.tensor_tensor(out=out3[:, :, 0:32], in0=rt[:, 0], in1=rt[:, 1], op=ALU.subtract), r=(rt,), w=(nb,))
            op("dve", lambda e: e.tensor_tensor(out=out3[:, :, 32:64], in0=rt[:, 2], in1=rt[:, 3], op=ALU.add), r=(rt,), w=(nb,))

        cst_b = [None]
        for st in range(NST):
            for tt in range(4):
                ti = st * 4 + tt
                xtile = xt[ti % 2]
                dma("sp", xtile, xtile[:], xcur, xcur[ti * 128:(ti + 1) * 128, :], xtile)
                dma("sp", cst[tt], cst[tt][:], cs_in, cs_in[ti * 128:(ti + 1) * 128, :], cst[tt])
                op("act", lambda e: e.activation(out=tmpD[:], in_=xtile[:], func=AF.Square, accum_out=st8[:, 8:9]),
                   r=(xtile,), w=(tmpD, st8))
                rstd_from_ssq(st8[:, 8:9], st8[:, 8:9], D, (st8,))
                op("dve", lambda e: e.scalar_tensor_tensor(out=tmpD[:], in0=xtile[:], scalar=st8[:, 8:9], in1=gm[:], op0=ALU.mult, op1=ALU.mult),
                   r=(xtile, st8, gm), w=(tmpD,))
                op("pool", lambda e: e.tensor_tensor(out=hb[:], in0=tmpD[:], in1=sh[:], op=ALU.add), r=(tmpD, sh), w=(hb,))
                for half in range(2):
                    transposes(hb, [hb[:, (half * 8 + i) * 128:(half * 8 + i + 1) * 128] for i in range(8)], 6 + half,
                               hts, hts[:, half * 8:half * 8 + 8, tt * 128:(tt + 1) * 128])
            for half in range(2):
                dma("pool", HT, HT[half * 1024:(half + 1) * 1024, st * 512:(st + 1) * 512].rearrange("(k p) t -> p k t", p=128),
                    hts, hts[:, half * 8:(half + 1) * 8, :], hts)
            for c in range(10):
                wt = wint[c % 2]
                ncols = {1: 320, 6: 332, 9: 256}.get(c, 512)
                dma("sp", wt, wt[:], WIN, WIN[:, c * 512:(c + 1) * 512].rearrange("(k p) n -> p k n", p=128), wt)
                for tt in range(4):
                    pz = PB[tt % 4]
                    ts_ = slice(tt * 128, (tt + 1) * 128)
                    mm(pz, pz[:, 0:ncols], [(hts[:, kc, ts_], wt[:, kc, 0:ncols]) for kc in range(16)], r=(hts, wt))
                for tt in range(4):
                    ti = st * 4 + tt
                    bg.tick()
                    cst_b[0] = cst[tt]
                    cs = cst[tt]
                    pz = PB[tt % 4]
                    ts_ = slice(tt * 128, (tt + 1) * 128)
                    vt = vts[tt]
                    if c in (4,):
                        op("act", lambda e: e.copy(out=vt[:, V_FOX:V_FOX + 512], in_=pz[:, 0:512]), r=(pz,), w=(vt,))
                        continue
                    op("act", lambda e: e.copy(out=zc[:, 0:ncols], in_=pz[:, 0:ncols]), r=(pz,), w=(zc,))
                    if c == 0:
                        hnorm(zc[:, 0:512].rearrange("p (h d) -> p h d", h=1), 1, 512, sm("cq"), nb[:, 0:512].rearrange("p (h d) -> p h d", h=1))
                        transposes(nb, [nb[:, i * 128:(i + 1) * 128] for i in range(4)], 6, nT, nT[:, 0:4, :])
                        p0, p1 = PB[4], PB[5]
                        mm(p0, p0[:, 0:512], [(nT[:, kc, :], wuqt[:, kc, 0:512]) for kc in range(4)], r=(nT, wuqt))
                        mm(p1, p1[:, 0:256], [(nT[:, kc, :], wuqt[:, kc, 512:768]) for kc in range(4)], r=(nT, wuqt))
                        op("act", lambda e: e.copy(out=zc[:, 0:512], in_=p0[:, 0:512]), r=(p0,), w=(zc,))
                        op("act", lambda e: e.copy(out=zc[:, 512:768], in_=p1[:, 0:256]), r=(p1,), w=(zc,))
                        op("dve", lambda e: e.tensor_tensor(out=sq[:, 0:768], in0=zc[:, 0:768], in1=zc[:, 0:768], op=ALU.mult), r=(zc,), w=(sq,))
                        op("dve", lambda e: e.tensor_reduce(out=st8[:, 0:4], in_=sq[:, 0:512].rearrange("p (h d) -> p h d", h=4), axis=AX.X, op=ALU.add), r=(sq,), w=(st8,))
                        op("dve", lambda e: e.tensor_reduce(out=st8[:, 4:8], in_=sq[:, 512:768].rearrange("p (h d) -> p h d", h=4), axis=AX.X, op=ALU.add), r=(sq,), w=(st8,))
                        op("dve", lambda e: e.tensor_tensor(out=st8[:, 0:4], in0=st8[:, 0:4], in1=st8[:, 4:8], op=ALU.add), r=(st8,), w=(st8,))
                        rstd_from_ssq(st8[:, 0:4], st8[:, 0:4], 192, (st8,))
                        o_, _w = SM_OFF["mq"]
                        gq_n, gq_r = smt[:, o_:o_ + 128], smt[:, o_ + 128:o_ + 192]
                        z3 = zc[:, 0:512].rearrange("p (h d) -> p h d", h=4)
                        s3 = sq[:, 0:512].rearrange("p (h d) -> p h d", h=4)
                        op("dve", lambda e: e.tensor_tensor(out=s3, in0=z3, in1=st8[:, 0:4].unsqueeze(2).to_broadcast([128, 4, 128]), op=ALU.mult), r=(zc, st8), w=(sq,))
                        op("dve", lambda e: e.tensor_tensor(out=nb[:, 0:512].rearrange("p (h d) -> p h d", h=4), in0=s3, in1=gq_n.unsqueeze(1).to_broadcast([128, 4, 128]), op=ALU.mult), r=(sq, smt), w=(nb,))
                        r3 = zc[:, 512:768].rearrange("p (h d) -> p h d", h=4)
                        op("dve", lambda e: e.tensor_tensor(out=rot[:], in0=r3, in1=st8[:, 0:4].unsqueeze(2).to_broadcast([128, 4, 64]), op=ALU.mult), r=(zc, st8), w=(rot,))
                        op("dve", lambda e: e.tensor_tensor(out=rot[:], in0=rot[:], in1=gq_r.unsqueeze(1).to_broadcast([128, 4, 64]), op=ALU.mult), r=(rot, smt), w=(rot,))
                        rope_apply(rot, nb[:, 512:768].rearrange("p (h d) -> p h d", h=4), cs, 4)
                        transposes(nb, [nb[:, i * 128:(i + 1) * 128] for i in range(6)], 7, fts, fts[:, B_QN:B_QN + 6, ts_])
                    elif c == 1:
                        hnorm(zc[:, 0:256].rearrange("p (h d) -> p h d", h=1), 1, 256, sm("ckv"), nb[:, 0:256].rearrange("p (h d) -> p h d", h=1))
                        transposes(nb, [nb[:, i * 128:(i + 1) * 128] for i in range(2)], 6, nT, nT[:, 0:2, :])
                        p0, p1 = PB[4], PB[5]
                        mm(p0, p0[:, 0:512], [(nT[:, kc, :], wukvt[:, kc, 0:512]) for kc in range(2)], r=(nT, wukvt))
                        mm(p1, p1[:, 0:512], [(nT[:, kc, :], wukvt[:, kc, 512:1024]) for kc in range(2)], r=(nT, wukvt))
                        op("act", lambda e: e.copy(out=vt[:, V_MLA:V_MLA + 512], in_=p1[:, 0:512]), r=(p1,), w=(vt,))
                        op("act", lambda e: e.copy(out=zc[:, 512:1024], in_=p0[:, 0:512]), r=(p0,), w=(zc,))
                        op("dve", lambda e: e.tensor_tensor(out=sq[:, 0:512], in0=zc[:, 512:1024], in1=zc[:, 512:1024], op=ALU.mult), r=(zc,), w=(sq,))
                        op("dve", lambda e: e.tensor_reduce(out=st8[:, 0:4], in_=sq[:, 0:512].rearrange("p (h d) -> p h d", h=4), axis=AX.X, op=ALU.add), r=(sq,), w=(st8,))
                        op("dve", lambda e: e.tensor_tensor(out=sq[:, 512:576], in0=zc[:, 256:320], in1=zc[:, 256:320], op=ALU.mult), r=(zc,), w=(sq,))
                        op("dve", lambda e: e.tensor_reduce(out=st8[:, 4:5], in_=sq[:, 512:576], axis=AX.X, op=ALU.add), r=(sq,), w=(st8,))
                        op("dve", lambda e: e.tensor_scalar(out=st8[:, 0:4], in0=st8[:, 0:4], scalar1=st8[:, 4:5], scalar2=None, op0=ALU.add), r=(st8,), w=(st8,))
                        rstd_from_ssq(st8[:, 0:4], st8[:, 0:4], 192, (st8,))
                        o_, _w = SM_OFF["mk"]
                        gk_n, gk_r = smt[:, o_:o_ + 128], smt[:, o_ + 128:o_ + 192]
                        z3 = zc[:, 512:1024].rearrange("p (h d) -> p h d", h=4)
                        s3 = sq[:, 0:512].rearrange("p (h d) -> p h d", h=4)
                        op("dve", lambda e: e.tensor_tensor(out=s3, in0=z3, in1=st8[:, 0:4].unsqueeze(2).to_broadcast([128, 4, 128]), op=ALU.mult), r=(zc, st8), w=(sq,))
                        op("dve", lambda e: e.tensor_tensor(out=nb[:, 0:512].rearrange("p (h d) -> p h d", h=4), in0=s3, in1=gk_n.unsqueeze(1).to_broadcast([128, 4, 128]), op=ALU.mult), r=(sq, smt), w=(nb,))
                        krb = zc[:, 256:320].unsqueeze(1).to_broadcast([128, 4, 64])
                        op("dve", lambda e: e.tensor_tensor(out=rot[:], in0=krb, in1=st8[:, 0:4].unsqueeze(2).to_broadcast([128, 4, 64]), op=ALU.mult), r=(zc, st8), w=(rot,))
                        op("dve", lambda e: e.tensor_tensor(out=rot[:], in0=rot[:], in1=gk_r.unsqueeze(1).to_broadcast([128, 4, 64]), op=ALU.mult), r=(rot, smt), w=(rot,))
                        rope_apply(rot, nb[:, 512:768].rearrange("p (h d) -> p h d", h=4), cs, 4)
                        transposes(nb, [nb[:, i * 128:(i + 1) * 128] for i in range(6)], 7, fts, fts[:, B_KN:B_KN + 6, ts_])
                    elif c in (2, 3, 5):
                        gname, blk = {2: ("fq", B_FQ), 3: ("fk", B_FK), 5: ("dq", B_DQ)}[c]
                        hnorm(zc[:, 0:512].rearrange("p (h d) -> p h d", h=4), 4, 128, sm(gname), nb[:, 0:512].rearrange("p (h d) -> p h d", h=4))
                        transposes(nb, [nb[:, i * 128:(i + 1) * 128] for i in range(4)], 6 + (c % 2), fts, fts[:, blk:blk + 4, ts_])
                    elif c == 6:
                        hnorm(zc[:, 0:128].rearrange("p (h d) -> p h d", h=1), 1, 128, sm("dk"), nb[:, 0:128].rearrange("p (h d) -> p h d", h=1))
                        op("pool", lambda e: e.tensor_copy(out=vt[:, V_DSA:V_DSA + 128], in_=zc[:, 128:256]), r=(zc,), w=(vt,))
                        op("pool", lambda e: e.tensor_copy(out=nb[:, 128:192], in_=zc[:, 256:320]), r=(zc,), w=(nb,))
                        transposes(nb, [nb[:, 0:128]], 6, fts, fts[:, B_DK:B_DK + 1, ts_])
                        transposes(nb, [nb[:, 128:192]], 7, fts, fts[0:64, B_IK:B_IK + 1, ts_], rows=64)
                        op("dve", lambda e: e.tensor_scalar(out=iwt[:], in0=zc[:, 320:328], scalar1=float(8 ** -0.5 * 64 ** -0.5), scalar2=None, op0=ALU.mult), r=(zc,), w=(iwt,))
                        dma("pool", IW, IW[ti * 128:(ti + 1) * 128, :], iwt, iwt[:], iwt)
                        op("dve", lambda e: e.tensor_tensor(out=lft[:], in0=zc[:, 328:332], in1=sm("fb"), op=ALU.add), r=(zc, smt), w=(lft,))
                        op("act", lambda e: e.activation(out=lft[:], in_=lft[:], func=AF.Exp, scale=-1.0), r=(lft,), w=(lft,))
                        op("act", lambda e: e.activation(out=lft[:], in_=lft[:], func=AF.Ln, bias=1.0), r=(lft,), w=(lft,))
                        p0 = PB[4]
                        mm(p0, p0[:, 0:4], [(tri[:], lft[:])], r=(tri, lft))
                        mm(p0, p0[:, 4:8], [(ones_f[:, 0:128], lft[:])], r=(ones_f, lft))
                        op("dve", lambda e: e.tensor_tensor(out=cumt[:], in0=p0[:, 0:4], in1=runt[:], op=ALU.add), r=(p0, runt), w=(cumt,))
                        op("dve", lambda e: e.tensor_tensor(out=runt[:], in0=p0[:, 4:8], in1=runt[:], op=ALU.add), r=(p0, runt), w=(runt,))
                        dma("pool", CUML, CUML[ti * 128:(ti + 1) * 128, :], cumt, cumt[:], cumt)
                        p1 = PB[5]
                        op("pe", lambda e: e.transpose(out=p1[0:4, 0:128], in_=cumt[:], identity=identf[:]), r=(cumt, identf), w=(p1,))
                        op("act", lambda e: e.copy(out=cumTt[:], in_=p1[0:4, 0:128]), r=(p1,), w=(cumTt,))
                        dma("pool", CUMT, CUMT[:, ti * 128:(ti + 1) * 128], cumTt, cumTt[:], cumTt)
                    elif c == 7:
                        op("pool", lambda e: e.tensor_copy(out=nb[:, 0:512], in_=zc[:, 0:512]), r=(zc,), w=(nb,))
                        transposes(nb, [nb[:, i * 128:(i + 1) * 128] for i in range(4)], 7, fts, fts[:, B_IQ:B_IQ + 4, ts_])
                    elif c == 8:
                        hnorm(zc[:, 0:512].rearrange("p (h d) -> p h d", h=8), 8, 64, sm("sq"), nb[:, 0:512].rearrange("p (h d) -> p h d", h=8))
                        transposes(nb, [nb[:, i * 128:(i + 1) * 128] for i in range(4)], 6, fts, fts[:, B_SQ:B_SQ + 4, ts_])
                    elif c == 9:
                        hnorm(zc[:, 0:128].rearrange("p (h d) -> p h d", h=2), 2, 64, sm("sk"), nb[:, 0:128].rearrange("p (h d) -> p h d", h=2))
                        op("pool", lambda e: e.tensor_copy(out=vt[:, V_SWA:V_SWA + 128], in_=zc[:, 128:256]), r=(zc,), w=(vt,))
                        transposes(nb, [nb[:, 0:128]], 7, fts, fts[:, B_SK:B_SK + 1, ts_])
                        dma("pool", VT, VT[ti * 128:(ti + 1) * 128, :], vt, vt[:], vt)
            for b0 in range(0, NFB, 7):
                dma("pool", FT, FT[b0 * 128:(b0 + 7) * 128, st * 512:(st + 1) * 512].rearrange("(b p) t -> p b t", p=128),
                    fts, fts[:, b0:b0 + 7, :], fts)
        k.end_phase()
        dsp.recycle()

        if stop == "p1":
            return finish()
        def attn_pair(psS, pairs, r, pT, exp_scale, bias_ap=None, bias_b=None, addmask=None, pre=None):
            mm(psS, psS[:, 0:512], pairs, r=r)
            if pre is not None:
                pre(psS)
            if addmask is not None:
                op("dve", lambda e: e.tensor_tensor(out=mtmp[:], in0=psS[:, 0:512], in1=addmask, op=ALU.add), r=(psS, negcm), w=(mtmp,))
                op("act", lambda e: e.activation(out=pT[:], in_=mtmp[:], func=AF.Exp, scale=exp_scale), r=(mtmp,), w=(pT,))
            else:
                op("act", lambda e: e.activation(out=pT[:], in_=psS[:, 0:512], func=AF.Exp, scale=exp_scale), r=(psS,), w=(pT,))

        def finalize(psO, psD, rows, out_b, rdt, extra_add=None):
            if extra_add is not None:
                op("dve", lambda e: e.tensor_tensor(out=rdt[0:rows, :], in0=psD[0:rows, :], in1=extra_add, op=ALU.add), r=(psD, est), w=(rdt,))
                op("dve", lambda e: e.reciprocal(out=rdt[0:rows, :], in_=rdt[0:rows, :]), r=(rdt,), w=(rdt,))
            else:
                op("dve", lambda e: e.reciprocal(out=rdt[0:rows, :], in_=psD[0:rows, :]), r=(psD,), w=(rdt,))
            op("dve", lambda e: e.tensor_tensor(out=out_b[0:rows, :], in0=psO[0:rows, :], in1=rdt[0:rows, :], op=ALU.mult), r=(psO, rdt), w=(out_b,))

        k.begin_phase()
        kn = [sbt("kn%d" % i, [128, S], BF16) for i in range(2)]
        kr = [sbt("kr%d" % i, [64, S], BF16) for i in range(2)]
        vv = [sbt("vv%d" % i, [128, 32, 128], BF16) for i in range(2)]
        qn = [sbt("qn%d" % i, [128, 512], BF16) for i in range(2)]
        qr = [sbt("qr%d" % i, [64, 512], BF16) for i in range(2)]
        pTa = [sbt("pTa%d" % i, [128, 512], BF16) for i in range(2)]
        mtmp = sbt("mtmp", [128, 512], F32)
        rdta = sbt("rdta", [128, 512], F32)
        otla = [sbt("otla%d" % i, [128, 512], BF16) for i in range(2)]
        ca = sbt("ca", [1, S], F32)
        cnq = [sbt("cnq%d" % i, [1, 512], F32) for i in range(2)]
        ikt = sbt("ikt", [64, S], BF16)
        iqt = [sbt("iqt%d" % i, [64, 8, 128], BF16) for i in range(2)]
        iwq = [sbt("iwq%d" % i, [128, 8], F32) for i in range(2)]
        It = sbt("It", [128, S], F32)
        Wk = sbt("Wk", [128, S], F32)
        Mt = sbt("Mt", [128, S], BF16)
        MT = sbt("MT", [128, 32, 512], BF16)
        rl = [sbt("rl%d" % i, [128, 512], F32) for i in range(2)]
        m8 = sbt("m8", [128, 8], F32)

        def gen_mla_fox():
            cnt = 0
            for mixer in ("mla", "fox"):
                scale = (192 ** -0.5) if mixer == "mla" else (128 ** -0.5)
                qblk, kblk, vbase, obase = (B_QN, B_KN, V_MLA, 0) if mixer == "mla" else (B_FQ, B_FK, V_FOX, 512)
                for h in range(4):
                    knh, vvh = kn[h % 2], vv[h % 2]
                    dma("sp", knh, knh[:], FT, FT[(kblk + h) * 128:(kblk + h + 1) * 128, :], knh)
                    for q4 in range(4):
                        dma("sp", vvh, vvh[:, q4 * 8:(q4 + 1) * 8, :], VT,
                            VT[q4 * 1024:(q4 + 1) * 1024, vbase + h * 128:vbase + (h + 1) * 128].rearrange("(kb p) d -> p kb d", p=128), vvh)
                    if mixer == "mla":
                        krh = kr[h % 2]
                        dma("sp", krh, krh[:], FT, FT[B_KR * 128 + h * 64:B_KR * 128 + (h + 1) * 64, :], krh)
                    else:
                        dma("sp", ca, ca[:], CUMT, CUMT[h:h + 1, :], ca)
                        op("pool", lambda e: e.tensor_scalar(out=ca[:], in0=ca[:], scalar1=float(1.0 / scale), scalar2=None, op0=ALU.mult), r=(ca,), w=(ca,))
                    yield 0.5
                    for j in range(NST):
                        qs = slice(j * 512, (j + 1) * 512)
                        qnj = qn[(h * NST + j) % 2]
                        dma("sp", qnj, qnj[:], FT, FT[(qblk + h) * 128:(qblk + h + 1) * 128, qs], qnj)
                        if mixer == "mla":
                            qrj = qr[(h * NST + j) % 2]
                            dma("sp", qrj, qrj[:], FT, FT[B_QR * 128 + h * 64:B_QR * 128 + (h + 1) * 64, qs], qrj)
                        else:
                            cnj = cnq[(h * NST + j) % 2]
                            op("pool", lambda e: e.tensor_scalar(out=cnj[:], in0=ca[0:1, qs], scalar1=-1.0, scalar2=None, op0=ALU.mult), r=(ca,), w=(cnj,))
                        psO, psD = PB[4], PB[5]
                        nkb = 4 * j + 4
                        for kb in range(nkb):
                            ks = slice(kb * 128, (kb + 1) * 128)
                            psS = PB[cnt % 2]
                            pT = pTa[cnt % 2]
                            cnt += 1
                            if mixer == "mla":
                                pairs = [(knh[:, ks], qnj[:]), (krh[0:64, ks], qrj[0:64, :])]
                                r = (knh, krh, qnj, qrj)
                            else:
                                pairs = [(knh[:, ks], qnj[:]), (ca[0:1, ks], ones_f[0:1, 0:512]), (ones_f[0:1, 0:128], cnj[0:1, :])]
                                r = (knh, qnj, ca, cnj, ones_f)
                            mm(psS, psS[:, 0:512], pairs, r=r)
                            if kb >= 4 * j:
                                op("dve", lambda e: e.tensor_tensor(out=mtmp[:], in0=psS[:, 0:512], in1=negcm[:, kb - 4 * j, :], op=ALU.add), r=(psS, negcm), w=(mtmp,))
                                op("act", lambda e: e.activation(out=pT[:], in_=mtmp[:], func=AF.Exp, scale=scale), r=(mtmp,), w=(pT,))
                            else:
                                op("act", lambda e: e.activation(out=pT[:], in_=psS[:, 0:512], func=AF.Exp, scale=scale), r=(psS,), w=(pT,))
                            mm(psO, psO[:, 0:512], [(vvh[:, kb, :], pT[:])], r=(vvh, pT), start=(kb == 0), stop=(kb == nkb - 1))
                            mm(psD, psD[:, 0:512], [(ones_b[:], pT[:])], r=(ones_b, pT), start=(kb == 0), stop=(kb == nkb - 1))
                            yield (3.5 if mixer == "mla" else 7.0)
                        ot = otla[j % 2]
                        finalize(psO, psD, 128, ot, rdta)
                        dma("pool", OT, OT[obase + h * 128:obase + (h + 1) * 128, qs], ot, ot[:], ot)
                        yield 3.0

        def gen_indexer():
            cnt = 0
            dma("sp", ikt, ikt[:], FT, FT[B_IK * 128:B_IK * 128 + 64, :], ikt)
            for j in range(NST):
                op("pool", lambda e: e.memset(MT[:, 4 * j:4 * j + 4, :], 0.0), w=(MT,))
                for u in range(4):
                    i = 4 * j + u
                    nk = (i + 1) * 128
                    iq_, iw_ = iqt[i % 2], iwq[i % 2]
                    dma("sp", iq_, iq_[:], FT, FT[B_IQ * 128:(B_IQ + 4) * 128, i * 128:(i + 1) * 128].rearrange("(jj p) t -> p jj t", p=64), iq_)
                    dma("sp", iw_, iw_[:], IW, IW[i * 128:(i + 1) * 128, :], iw_)
                    for c0 in range(0, nk, 512):
                        cw = min(512, nk - c0)
                        for jj in range(8):
                            psS = PB[2 + cnt % 2]
                            rlt = rl[cnt % 2]
                            cnt += 1
                            mm(psS, psS[:, 0:cw], [(iq_[0:64, jj, :], ikt[0:64, c0:c0 + cw])], r=(iq_, ikt))
                            op("act", lambda e: e.activation(out=rlt[:, 0:cw], in_=psS[:, 0:cw], func=AF.Relu), r=(psS,), w=(rlt,))
                            if jj == 0:
                                op("dve", lambda e: e.tensor_scalar(out=It[:, c0:c0 + cw], in0=rlt[:, 0:cw], scalar1=iw_[:, 0:1], scalar2=None, op0=ALU.mult),
                                   r=(rlt, iw_), w=(It,))
                            else:
                                op("dve", lambda e: e.scalar_tensor_tensor(out=It[:, c0:c0 + cw], in0=rlt[:, 0:cw], scalar=iw_[:, jj:jj + 1], in1=It[:, c0:c0 + cw],
                                                                             op0=ALU.mult, op1=ALU.add), r=(rlt, iw_, It), w=(It,))
                            if jj % 2 == 1:
                                yield 2.0 * cw / 960.0 + 0.2
                    op("dve", lambda e: e.tensor_tensor(out=It[:, i * 128:(i + 1) * 128], in0=It[:, i * 128:(i + 1) * 128], in1=negtri[:], op=ALU.add), r=(It, negtri), w=(It,))
                    if nk > 256:
                        for rnd in range(32):
                            src = It if rnd == 0 else Wk
                            op("dve", lambda e: e.max(out=m8[:], in_=src[:, 0:nk]), r=(src,), w=(m8,))
                            if rnd < 31:
                                op("dve", lambda e: e.match_replace(out=Wk[:, 0:nk], in_to_replace=m8[:], in_values=src[:, 0:nk], imm_value=-1e30), r=(src, m8), w=(Wk,))
                            yield 2.0 * nk / 960.0 + 0.3
                        thr, thr_b = m8[:, 7:8], m8
                    else:
                        thr, thr_b = neg29[:, 0:1], neg29
                    op("dve", lambda e: e.tensor_scalar(out=Mt[:, 0:nk], in0=It[:, 0:nk], scalar1=thr, scalar2=None, op0=ALU.is_ge), r=(It, thr_b), w=(Mt,))
                    for kb0 in range(0, i + 1, 8):
                        n = min(8, i + 1 - kb0)
                        tb_ = 6 + (kb0 // 8) % 2
                        pv = pbf(tb_)
                        for t_ in range(n):
                            kb = kb0 + t_
                            op("pe", lambda e: e.transpose(out=pv[:, t_ * 128:(t_ + 1) * 128], in_=Mt[:, kb * 128:(kb + 1) * 128], identity=ident[:]),
                               r=(Mt, ident) if t_ == 0 else (), w=(PB[tb_],), sig=(t_ == n - 1))
                        op("act", lambda e: e.copy(out=MT[:, kb0:kb0 + n, u * 128:(u + 1) * 128], in_=pv[:, 0:n * 128].rearrange("p (n t) -> p n t", n=n)),
                           r=(PB[tb_],), w=(MT,))
                        yield 1.0
                nkb = 4 * j + 4
                dma("pool", MTD, MTD[j * 128:(j + 1) * 128, 0:nkb * 512], MT, MT[:, 0:nkb, :].rearrange("p a b -> p (a b)"), MT)
                yield 1.0

        gens = [[gen_mla_fox(), 0.0], [gen_indexer(), 0.0]]
        while gens:
            g_ = min(gens, key=lambda x: x[1])
            try:
                g_[1] += next(g_[0])
            except StopIteration:
                gens.remove(g_)
            bg.tick()
        k.end_phase()
        dsp.recycle()

        k.begin_phase()
        dkt = sbt("dkt", [128, S], BF16)
        dvv = sbt("dvv", [128, 32, 128], BF16)
        mtj = [sbt("mtj%d" % i, [128, 32, 512], BF16) for i in range(2)]
        dq = [sbt("dq%d" % i, [128, 512], BF16) for i in range(2)]
        pTt = [sbt("pT%d" % i, [128, 512], BF16) for i in range(3)]
        rdt = sbt("rdt", [128, 512], F32)
        otl = [sbt("otl%d" % i, [128, 512], BF16) for i in range(2)]
        dma("sp", dkt, dkt[:], FT, FT[B_DK * 128:(B_DK + 1) * 128, :], dkt)
        for q4 in range(4):
            dma("sp", dvv, dvv[:, q4 * 8:(q4 + 1) * 8, :], VT,
                VT[q4 * 1024:(q4 + 1) * 1024, V_DSA:V_DSA + 128].rearrange("(kb p) d -> p kb d", p=128), dvv)
        dscale = 128 ** -0.5
        cnt = 0
        for j in range(NST):
            qs = slice(j * 512, (j + 1) * 512)
            nkb = 4 * j + 4
            MTj = mtj[j % 2]
            dma("sp", MTj, MTj[:, 0:nkb, :].rearrange("p a b -> p (a b)"), MTD, MTD[j * 128:(j + 1) * 128, 0:nkb * 512], MTj)
            for h in range(4):
                dqh = dq[h % 2]
                dma("sp", dqh, dqh[:], FT, FT[(B_DQ + h) * 128:(B_DQ + h + 1) * 128, qs], dqh)
                psO, psD = PB[4 + (h % 2) * 2], PB[5 + (h % 2) * 2]
                for kb in range(nkb):
                    ks = slice(kb * 128, (kb + 1) * 128)
                    psS = PB[cnt % 3]
                    pT = pTt[cnt % 3]
                    cnt += 1
                    bg.tick()
                    mm(psS, psS[:, 0:512], [(dkt[:, ks], dqh[:])], r=(dkt, dqh))
                    op("act", lambda e: e.activation(out=pT[:], in_=psS[:, 0:512], func=AF.Exp, scale=dscale), r=(psS,), w=(pT,))
                    op("dve", lambda e: e.tensor_tensor(out=pT[:], in0=pT[:], in1=MTj[:, kb, :], op=ALU.mult), r=(pT, MTj), w=(pT,))
                    rb = kb - 4 * j
                    if rb >= -1:
                        lo, hi = max(0, rb * 128), min(512, rb * 128 + 256)
                        op("dve", lambda e: e.tensor_tensor(out=pT[:, lo:hi], in0=pT[:, lo:hi], in1=ew2[:, h, lo - rb * 128:hi - rb * 128], op=ALU.mult),
                           r=(pT, ew2), w=(pT,))
                    mm(psO, psO[:, 0:512], [(dvv[:, kb, :], pT[:])], r=(dvv, pT), start=(kb == 0), stop=(kb == nkb - 1))
                    mm(psD, psD[:, 0:512], [(ones_b[:], pT[:])], r=(ones_b, pT), start=(kb == 0), stop=(kb == nkb - 1))
                ot = otl[h % 2]
                finalize(psO, psD, 128, ot, rdt)
                dma("pool", OT, OT[1024 + h * 128:1024 + (h + 1) * 128, qs], ot, ot[:], ot)
        k.end_phase()
        dsp.recycle()

        k.begin_phase()
        skt = sbt("skt", [64, 2, S], BF16)
        svv = sbt("svv", [128, 32, 128], BF16)
        sqt = [sbt("sqt%d" % i, [64, 8, 128], BF16) for i in range(2)]
        pTt = [sbt("pT%d" % i, [128, 512], BF16) for i in range(2)]
        rdt = sbt("rdt", [128, 512], F32)
        otl = [sbt("otl%d" % i, [64, 512], BF16) for i in range(2)]
        es8 = sbt("es8", [128, 8], F32)
        est = sbt("est", [64, 2, 512], F32)
        dma("sp", skt, skt[:], FT, FT[B_SK * 128:(B_SK + 1) * 128, :].rearrange("(g p) t -> p g t", p=64), skt)
        for q4 in range(4):
            dma("sp", svv, svv[:, q4 * 8:(q4 + 1) * 8, :], VT,
                VT[q4 * 1024:(q4 + 1) * 1024, V_SWA:V_SWA + 128].rearrange("(kb p) d -> p kb d", p=128), svv)
        o_, _w = SM_OFF["sink"]
        op("act", lambda e: e.activation(out=es8[:], in_=smt[:, o_:o_ + 8], func=AF.Exp), r=(smt,), w=(es8,))
        for hh in range(8):
            op("dve", lambda e: e.tensor_copy(out=est[:, hh // 4, (hh % 4) * 128:(hh % 4 + 1) * 128], in_=es8[0:64, hh:hh + 1].to_broadcast([64, 128])),
               r=(es8,), w=(est,))
        sscale = 64 ** -0.5
        cnt = 0
        for i in range(NT):
            sq_ = sqt[i % 2]
            dma("sp", sq_, sq_[:], FT, FT[B_SQ * 128:(B_SQ + 4) * 128, i * 128:(i + 1) * 128].rearrange("(hh p) t -> p hh t", p=64), sq_)
            for g in range(2):
                psO, psD = PB[4 + (g % 2) * 2], PB[5 + (g % 2) * 2]
                rels = [(1, i)] if i == 0 else [(0, i - 1), (1, i)]
                bg.tick()
                for ri, (rel, kb) in enumerate(rels):
                    psS = PB[cnt % 2]
                    pT = pTt[cnt % 2]
                    cnt += 1
                    mm(psS, psS[:, 0:512], [(skt[0:64, g, kb * 128:(kb + 1) * 128], sq_[0:64, 4 * g:4 * g + 4, :])], r=(skt, sq_))
                    op("act", lambda e: e.activation(out=pT[:], in_=psS[:, 0:512], func=AF.Exp, scale=sscale), r=(psS,), w=(pT,))
                    op("dve", lambda e: e.tensor_tensor(out=pT[:], in0=pT[:], in1=ebs[:, g, rel, :], op=ALU.mult), r=(pT, ebs), w=(pT,))
                    mm(psO, psO[0:64, 0:512], [(svv[:, kb, g * 64:(g + 1) * 64], pT[:])], r=(svv, pT), start=(ri == 0), stop=(ri == len(rels) - 1))
                    mm(psD, psD[0:64, 0:512], [(ones_b[:, 0:64], pT[:])], r=(ones_b, pT), start=(ri == 0), stop=(ri == len(rels) - 1))
                ot = otl[g % 2]
                finalize(psO, psD, 64, ot, rdt, extra_add=est[:, g, :])
                dma("pool", OT, OT[1536 + 256 * g:1536 + 256 * (g + 1), i * 128:(i + 1) * 128].rearrange("(hi d) t -> d hi t", d=64),
                    ot, ot[:].rearrange("p (hi t) -> p hi t", hi=4), ot)
        k.end_phase()
        dsp.recycle()
        if stop == "attn":
            return finish()

        k.begin_phase()
        g1 = sbt("g1", [128, D], F32)
        load_bcast(g1, g1[:], MOD, MOD[L:L + 1, 2 * D:3 * D])
        hts = sbt("hts", [128, 16, 512], BF16)
        ots = sbt("ots", [128, 16, 512], BF16)
        wgt = [sbt("wgt%d" % i, [128, 16, 256], BF16) for i in range(4)]
        wbt = [sbt("wbt%d" % i, [128, 4, 256], BF16) for i in range(4)]
        mT = sbt("mT", [128, 16, 512], BF16)
        sg = [sbt("sg%d" % i, [128, 512], F32) for i in range(2)]
        tm = [sbt("tm%d" % i, [128, 512], F32) for i in range(2)]
        acc = sbt("acc", [128, 512], F32)
        wot = [sbt("wot%d" % i, [128, 16, 256], BF16) for i in range(2)]
        xt4 = [sbt("xt4_%d" % i, [128, D], F32) for i in range(4)]
        cnt = 0
        for st in range(NST):
            tsl = slice(st * 512, (st + 1) * 512)
            for half in range(2):
                dma("sp", hts, hts[:, half * 8:(half + 1) * 8, :], HT, HT[half * 1024:(half + 1) * 1024, tsl].rearrange("(k p) t -> p k t", p=128), hts)
                dma("sp", ots, ots[:, half * 8:(half + 1) * 8, :], OT, OT[half * 1024:(half + 1) * 1024, tsl].rearrange("(k p) t -> p k t", p=128), ots)
            for tt in range(4):
                ti = st * 4 + tt
                dma("sp", xt4[tt], xt4[tt][:], xcur, xcur[ti * 128:(ti + 1) * 128, :], xt4[tt])
            for mg in range(8):
                for i in range(4):
                    for half in range(2):
                        dma("sp", wgt[i], wgt[i][:, half * 8:(half + 1) * 8, :], WG,
                            WG[i * D + half * 1024:i * D + (half + 1) * 1024, mg * 256:(mg + 1) * 256].rearrange("(k p) n -> p k n", p=128), wgt[i])
                    dma("sp", wbt[i], wbt[i][:], WB, WB[i * 512:(i + 1) * 512, mg * 256:(mg + 1) * 256].rearrange("(k p) n -> p k n", p=128), wbt[i])
                for m2 in range(2):
                    m = 2 * mg + m2
                    ms = slice(m2 * 128, (m2 + 1) * 128)
                    for i in range(4):
                        psG, psB = PB[cnt % 2], PB[2 + cnt % 2]
                        sgt, tmt = sg[cnt % 2], tm[cnt % 2]
                        cnt += 1
                        bg.tick()
                        mm(psG, psG[:, 0:512], [(wgt[i][:, kc, ms], hts[:, kc, :]) for kc in range(16)], r=(wgt[i], hts))
                        mm(psB, psB[:, 0:512], [(wbt[i][:, kc, ms], ots[:, 4 * i + kc, :]) for kc in range(4)], r=(wbt[i], ots))
                        op("act", lambda e: e.activation(out=sgt[:], in_=psG[:, 0:512], func=AF.Sigmoid), r=(psG,), w=(sgt,))
                        if i == 0:
                            op("dve", lambda e: e.tensor_tensor(out=acc[:], in0=psB[:, 0:512], in1=sgt[:], op=ALU.mult), r=(psB, sgt), w=(acc,))
                        else:
                            op("dve", lambda e: e.tensor_tensor(out=tmt[:], in0=psB[:, 0:512], in1=sgt[:], op=ALU.mult), r=(psB, sgt), w=(tmt,))
                            if i < 3:
                                op("pool", lambda e: e.tensor_tensor(out=acc[:], in0=acc[:], in1=tmt[:], op=ALU.add), r=(acc, tmt), w=(acc,))
                            else:
                                op("pool", lambda e: e.tensor_tensor(out=mT[:, m, :], in0=acc[:], in1=tmt[:], op=ALU.add), r=(acc, tmt), w=(mT,))
            for n in range(8):
                wo = wot[n % 2]
                ns = slice(n * 256, (n + 1) * 256)
                for half in range(2):
                    dma("sp", wo, wo[:, half * 8:(half + 1) * 8, :], WO, WO[half * 1024:(half + 1) * 1024, ns].rearrange("(k p) n -> p k n", p=128), wo)
                for tt in range(4):
                    bg.tick()
                    pso = PB[4 + tt]
                    mm(pso, pso[:, 0:256], [(mT[:, kc, tt * 128:(tt + 1) * 128], wo[:, kc, :]) for kc in range(16)], r=(mT, wo))
                    op("dve", lambda e: e.tensor_tensor(out=tm[tt % 2][:, 0:256], in0=pso[:, 0:256], in1=g1[:, ns], op=ALU.mult), r=(pso, g1), w=(tm[tt % 2],))
                    op("pool", lambda e: e.tensor_tensor(out=xt4[tt][:, ns], in0=xt4[tt][:, ns], in1=tm[tt % 2][:, 0:256], op=ALU.add), r=(xt4[tt], tm[tt % 2]), w=(xt4[tt],))
            for tt in range(4):
                ti = st * 4 + tt
                dma("pool", xmid, xmid[ti * 128:(ti + 1) * 128, :], xt4[tt], xt4[tt][:], xt4[tt])
        k.end_phase()
        dsp.recycle()

        if stop == "p3":
            return finish()
        k.begin_phase()
        gm = sbt("gm2", [128, D], F32)
        sh = sbt("sh2", [128, D], F32)
        g2 = sbt("g2", [128, D], F32)
        tmpD = sbt("tmpD2", [128, D], F32)
        load_bcast(gm, gm[:], MOD, MOD[L:L + 1, 4 * D:5 * D])
        load_bcast(tmpD, tmpD[:], norm_ffn, norm_ffn[L:L + 1, :])
        op("dve", lambda e: e.scalar_tensor_tensor(out=gm[:], in0=gm[:], scalar=1.0, in1=tmpD[:], op0=ALU.add, op1=ALU.mult), r=(gm, tmpD), w=(gm,))
        load_bcast(sh, sh[:], MOD, MOD[L:L + 1, 3 * D:4 * D])
        load_bcast(g2, g2[:], MOD, MOD[L:L + 1, 5 * D:6 * D])
        xt4 = [sbt("xt4_%d" % i, [128, D], F32) for i in range(4)]
        hb = sbt("hb2", [128, D], BF16)
        h2T = sbt("h2T", [128, 16, 512], BF16)
        at = sbt("at", [128, 22, 512], BF16)
        w1t = [sbt("w1t%d" % i, [128, 16, 256], BF16) for i in range(2)]
        w3t = [sbt("w3t%d" % i, [128, 16, 256], BF16) for i in range(2)]
        w2t = [sbt("w2t%d" % i, [128, 11, 512], BF16) for i in range(2)]
        sg = [sbt("sgf%d" % i, [128, 512], F32) for i in range(2)]
        tm = [sbt("tmf%d" % i, [128, 512], F32) for i in range(2)]
        st8 = sbt("st8f", [128, 16], F32)
        if moe:
            rtf = sbt("rtf", [128, 16, 8], F32)
            rtb = sbt("rtb", [128, 16, 8], BF16)
            lg = sbt("lg", [128, 8], F32)
            m8 = sbt("m8f", [128, 8], F32)
            gt = [sbt("gt%d" % i, [128, 8], F32) for i in range(4)]
            e1 = sbt("e1", [128, 8], F32)
            dma("sp", rtf, rtf[:], moe_router, moe_router[j2 * D:(j2 + 1) * D, :].rearrange("(k p) n -> p k n", p=128), rtf)
            op("dve", lambda e: e.tensor_copy(out=rtb[:], in_=rtf[:]), r=(rtf,), w=(rtb,))
        cnt = 0
        wc = 0
        for st in range(NST):
            for tt in range(4):
                ti = st * 4 + tt
                xtile = xt4[tt]
                dma("sp", xtile, xtile[:], xmid, xmid[ti * 128:(ti + 1) * 128, :], xtile)
                op("act", lambda e: e.activation(out=tmpD[:], in_=xtile[:], func=AF.Square, accum_out=st8[:, 8:9]), r=(xtile,), w=(tmpD, st8))
                rstd_from_ssq(st8[:, 8:9], st8[:, 8:9], D, (st8,))
                op("dve", lambda e: e.scalar_tensor_tensor(out=tmpD[:], in0=xtile[:], scalar=st8[:, 8:9], in1=gm[:], op0=ALU.mult, op1=ALU.mult), r=(xtile, st8, gm), w=(tmpD,))
                op("pool", lambda e: e.tensor_tensor(out=hb[:], in0=tmpD[:], in1=sh[:], op=ALU.add), r=(tmpD, sh), w=(hb,))
                for half in range(2):
                    bank = 6 + half
                    pv = pbf(bank)
                    for i_ in range(8):
                        kc = half * 8 + i_
                        op("pe", lambda e: e.transpose(out=pv[:, i_ * 128:(i_ + 1) * 128], in_=hb[:, kc * 128:(kc + 1) * 128], identity=ident[:]),
                           r=(hb, ident) if i_ == 0 else (), w=(PB[bank],), sig=(i_ == 7))
                    op("act", lambda e: e.copy(out=h2T[:, half * 8:half * 8 + 8, tt * 128:(tt + 1) * 128], in_=pv[:, 0:1024].rearrange("p (n t) -> p n t", n=8)),
                       r=(PB[bank],), w=(h2T,))
                if moe:
                    pz = PB[4]
                    mm(pz, pz[:, 0:8], [(h2T[:, kc, tt * 128:(tt + 1) * 128], rtb[:, kc, :]) for kc in range(16)], r=(h2T, rtb))
                    op("dve", lambda e: e.tensor_copy(out=lg[:], in_=pz[:, 0:8]), r=(pz,), w=(lg,))
                    op("dve", lambda e: e.max(out=m8[:], in_=lg[:]), r=(lg,), w=(m8,))
                    op("dve", lambda e: e.tensor_tensor(out=st8[:, 0:1], in0=m8[:, 0:1], in1=m8[:, 1:2], op=ALU.subtract), r=(m8,), w=(st8,))
                    op("act", lambda e: e.activation(out=st8[:, 1:2], in_=st8[:, 0:1], func=AF.Sigmoid), r=(st8,), w=(st8,))
                    op("dve", lambda e: e.tensor_scalar(out=st8[:, 2:3], in0=st8[:, 1:2], scalar1=-1.0, scalar2=1.0, op0=ALU.mult, op1=ALU.add), r=(st8,), w=(st8,))
                    op("dve", lambda e: e.tensor_scalar(out=e1[:], in0=lg[:], scalar1=m8[:, 0:1], scalar2=st8[:, 1:2], op0=ALU.is_equal, op1=ALU.mult), r=(lg, m8, st8), w=(e1,))
                    op("dve", lambda e: e.tensor_scalar(out=gt[tt][:], in0=lg[:], scalar1=m8[:, 1:2], scalar2=st8[:, 2:3], op0=ALU.is_equal, op1=ALU.mult), r=(lg, m8, st8), w=(gt[tt],))
                    op("dve", lambda e: e.tensor_tensor(out=gt[tt][:], in0=gt[tt][:], in1=e1[:], op=ALU.add), r=(gt[tt], e1), w=(gt[tt],))
            for ex in range(NE):
                for fg in range(11):
                    w1, w3 = w1t[wc % 2], w3t[wc % 2]
                    wc += 1
                    for half in range(2):
                        dma("sp", w1, w1[:, half * 8:(half + 1) * 8, :], FW1,
                            FW1[ex * D + half * 1024:ex * D + (half + 1) * 1024, fg * 256:(fg + 1) * 256].rearrange("(k p) n -> p k n", p=128), w1)
                        dma("sp", w3, w3[:, half * 8:(half + 1) * 8, :], FW3,
                            FW3[ex * D + half * 1024:ex * D + (half + 1) * 1024, fg * 256:(fg + 1) * 256].rearrange("(k p) n -> p k n", p=128), w3)
                    for fc in range(2):
                        f = 2 * fg + fc
                        fs = slice(fc * 128, (fc + 1) * 128)
                        psG, psU = PB[cnt % 2], PB[2 + cnt % 2]
                        sgt = sg[cnt % 2]
                        cnt += 1
                        bg.tick()
                        mm(psG, psG[:, 0:512], [(w1[:, kc, fs], h2T[:, kc, :]) for kc in range(16)], r=(w1, h2T))
                        mm(psU, psU[:, 0:512], [(w3[:, kc, fs], h2T[:, kc, :]) for kc in range(16)], r=(w3, h2T))
                        op("act", lambda e: e.activation(out=sgt[:], in_=psG[:, 0:512], func=AF.Silu), r=(psG,), w=(sgt,))
                        op("dve", lambda e: e.tensor_tensor(out=at[:, f, :], in0=psU[:, 0:512], in1=sgt[:], op=ALU.mult), r=(psU, sgt), w=(at,))
                for n in range(4):
                    ns = slice(n * 512, (n + 1) * 512)
                    for f2 in range(2):
                        w2 = w2t[wc % 2]
                        wc += 1
                        bg.tick()
                        dma("sp", w2, w2[:], FW2, FW2[ex * 2816 + f2 * 1408:ex * 2816 + (f2 + 1) * 1408, ns].rearrange("(f p) n -> p f n", p=128), w2)
                        for tt in range(4):
                            psY = PB[4 + tt]
                            mm(psY, psY[:, 0:512], [(at[:, f2 * 11 + fl, tt * 128:(tt + 1) * 128], w2[:, fl, :]) for fl in range(11)], r=(at, w2),
                               start=(f2 == 0), stop=(f2 == 1))
                    for tt in range(4):
                        psY = PB[4 + tt]
                        tmt = tm[tt % 2]
                        if moe:
                            op("dve", lambda e: e.scalar_tensor_tensor(out=tmt[:], in0=psY[:, 0:512], scalar=gt[tt][:, ex:ex + 1], in1=g2[:, ns], op0=ALU.mult, op1=ALU.mult),
                               r=(psY, gt[tt], g2), w=(tmt,))
                        else:
                            op("dve", lambda e: e.tensor_tensor(out=tmt[:], in0=psY[:, 0:512], in1=g2[:, ns], op=ALU.mult), r=(psY, g2), w=(tmt,))
                        op("pool", lambda e: e.tensor_tensor(out=xt4[tt][:, ns], in0=xt4[tt][:, ns], in1=tmt[:], op=ALU.add), r=(xt4[tt], tmt), w=(xt4[tt],))
            for tt in range(4):
                ti = st * 4 + tt
                dma("pool", xnext, xnext[ti * 128:(ti + 1) * 128, :], xt4[tt], xt4[tt][:], xt4[tt])
        k.end_phase()
        dsp.recycle()
        xcur = xnext

    return finish()


def _rel_bucket_np(n):
    n = np.maximum(n, 0)
    nf = np.maximum(n, 1).astype(np.float32)
    large = 16 + (np.log(nf / 16) / math.log(128 / 16) * 16).astype(np.int32)
    large = np.minimum(large, 31)
    return np.where(n < 16, n, large)


def _constants():
    c = {}
    c["ident"] = np.eye(128, dtype=np.float32).astype(NPBF)
    half = 32
    freqs = (10000.0 ** (-np.arange(half, dtype=np.float32) / half)).astype(np.float32)
    ang = np.arange(S, dtype=np.float32)[:, None] * freqs[None, :]
    c["cs_tab"] = np.concatenate([np.cos(ang), np.sin(ang)], axis=1).astype(np.float32)
    s_ = np.arange(128)[:, None]
    t_ = np.arange(512)[None, :]
    c["negcm"] = np.concatenate([np.where(r * 128 + s_ <= t_, 0.0, NEG) for r in range(4)], axis=1).astype(np.float32)
    qq = np.arange(128)[:, None]
    kk = np.arange(128)[None, :]
    c["negtri"] = np.where(kk <= qq, 0.0, -1e30).astype(np.float32)
    c["tri"] = (np.arange(128)[:, None] <= np.arange(128)[None, :]).astype(np.float32)
    dist = np.arange(384) - 127
    bk = _rel_bucket_np(dist)
    oh = np.zeros((32, 384), np.float32)
    for j in range(384):
        if dist[j] >= 0:
            oh[bk[j], j] = 1.0
    c["ohs"] = oh.copy()
    ohd = oh.copy()
    ohd[31, dist >= 0] -= 1.0
    c["ohd"] = ohd
    tt = np.arange(256)[None, :]
    dd = tt - s_
    c["negw"] = np.where((dd >= 0) & (dd < 128), 0.0, NEG).astype(np.float32)
    return c


def _prep_inputs(inp, nl=DEPTH, cores=8):
    f = lambda a: np.ascontiguousarray(np.asarray(a, dtype=np.float32))
    nf_, nm_ = (nl + 1) // 2, nl // 2
    w_in = f(inp["w_in"][:nl])
    offs = np.cumsum([0, 512, 256, 64, 512, 512, 512, 4, 512, 128, 128, 512, 64, 8, 512, 128, 128])
    (cq, ckv, kr, fq, fk, fv, fg, dq, dk, dv, iq, ik, iw, sq, sk, sv) = [slice(offs[i], offs[i + 1]) for i in range(16)]
    wp = np.zeros((nl, D, 5120), np.float32)

    def put(c, o, sl):
        wp[:, :, c * 512 + o:c * 512 + o + (sl.stop - sl.start)] = w_in[:, :, sl]
    put(0, 0, cq); put(1, 0, ckv); put(1, 256, kr); put(2, 0, fq); put(3, 0, fk); put(4, 0, fv); put(5, 0, dq)
    put(6, 0, dk); put(6, 128, dv); put(6, 256, ik); put(6, 320, iw); put(6, 328, fg)
    put(7, 0, iq); put(8, 0, sq); put(9, 0, sk); put(9, 128, sv)
    uq = f(inp["mla_w_uq"][:nl]).reshape(nl, 512, 4, 192)
    uqp = np.concatenate([uq[..., :128].reshape(nl, 512, 512), uq[..., 128:].reshape(nl, 512, 256)], axis=-1)
    ukv = f(inp["mla_w_ukv"][:nl]).reshape(nl, 256, 4, 256)
    ukvp = np.concatenate([ukv[..., :128].reshape(nl, 256, 512), ukv[..., 128:].reshape(nl, 256, 512)], axis=-1)
    small = np.concatenate([f(inp[n][:nl]) for n in ("mla_cq_norm", "mla_ckv_norm", "mla_q_norm", "mla_k_norm", "fox_q_norm", "fox_k_norm",
                                                      "fox_f_bias", "dsa_q_norm", "dsa_k_norm", "swa_q_norm", "swa_k_norm", "swa_sinks")], axis=1)
    assert small.shape == (nl, NSM)
    shared = {
        "ada_w": f(inp["ada_w"][:nl]).reshape(nl * D, 6 * D), "ada_b": f(inp["ada_b"][:nl]),
        "norm_mix": f(inp["norm_mix"][:nl]), "norm_ffn": f(inp["norm_ffn"][:nl]), "small": np.ascontiguousarray(small),
        "w_in_p": wp.reshape(nl * D, 5120), "w_uq_p": np.ascontiguousarray(uqp).reshape(nl * 512, 768),
        "w_ukv_p": np.ascontiguousarray(ukvp).reshape(nl * 256, 1024), "rel_bias": f(inp["rel_bias"]),
        "w_branch": f(inp["w_branch"][:nl]).reshape(nl * 4 * 512, D), "w_gate": f(inp["w_gate"][:nl]).reshape(nl * 4 * D, D),
        "w_out": f(inp["w_out"][:nl]).reshape(nl * D, D),
        "ffn_w1": f(inp["ffn_w1"][:nf_]).reshape(nf_ * D, 5632), "ffn_w3": f(inp["ffn_w3"][:nf_]).reshape(nf_ * D, 5632),
        "ffn_w2": f(inp["ffn_w2"][:nf_]).reshape(nf_ * 5632, D),
    }
    if nm_:
        shared.update({
            "moe_router": f(inp["moe_router"][:nm_]).reshape(nm_ * D, 8),
            "moe_w1": f(inp["moe_w1"][:nm_]).reshape(nm_ * 8 * D, 2816), "moe_w3": f(inp["moe_w3"][:nm_]).reshape(nm_ * 8 * D, 2816),
            "moe_w2": f(inp["moe_w2"][:nm_]).reshape(nm_ * 8 * 2816, D),
        })
    shared.update(_constants())
    maps = []
    for b in range(cores):
        m = dict(shared)
        m["x"] = f(inp["x"][b])
        m["cT"] = np.ascontiguousarray(f(inp["c"][b]).reshape(16, 128).T)
        maps.append(m)
    return maps


def kernel(**inputs):
    maps = _prep_inputs(inputs)
    nc = build_program()
    res = run_bass_kernel_spmd(nc, maps, core_ids=list(range(8)))
    return np.stack([np.asarray(r["y"], dtype=np.float32) for r in res.results], axis=0)
```

```python
import contextlib
import math
import numpy as np
import ml_dtypes
import concourse.bass as bass
import concourse.mybir as mybir
from concourse.bass_utils import run_bass_kernel_spmd

F32 = mybir.dt.float32
BF16 = mybir.dt.bfloat16
AF = mybir.ActivationFunctionType
ALU = mybir.AluOpType
AX = mybir.AxisListType
NPBF = ml_dtypes.bfloat16

S = 4096
D = 2048
NT = 32
NST = 8
DEPTH = 4
EPS = 1e-6
NEG = -30000.0

SM_OFF = {}
_o = 0
for _n, _w in (("cq", 512), ("ckv", 256), ("mq", 192), ("mk", 192), ("fq", 128), ("fk", 128),
               ("fb", 4), ("dq", 128), ("dk", 128), ("sq", 64), ("sk", 64), ("sink", 8)):
    SM_OFF[_n] = (_o, _w)
    _o += _w
NSM = _o

B_QN, B_QR, B_KN, B_KR, B_FQ, B_FK, B_DQ, B_DK, B_IK, B_IQ, B_SQ, B_SK = 0, 4, 6, 10, 12, 16, 20, 24, 25, 26, 30, 34
NFB = 35
V_MLA, V_FOX, V_DSA, V_SWA, NVT = 0, 512, 1024, 1152, 1280


class Sem:
    def __init__(self, h, name):
        self.h = h
        self.name = name


class Buf:
    def __init__(self, t, name):
        self.t = t
        self.name = name
        self.w = {}
        self.r = {}
        self.ds = None
        self.persist = False

    def __getitem__(self, k):
        return self.t[k]


class Eng:
    def __init__(self, name, e, sem):
        self.name = name
        self.e = e
        self.sem = sem
        self.cnt = 0
        self.seen = {}

    def waitd(self, d):
        for sem, val in d.items():
            if sem is self.sem and val > self.cnt:
                continue
            if self.seen.get(sem, 0) >= val:
                continue
            self.e.wait_ge(sem.h, val)
            self.seen[sem] = val


class K:
    def __init__(self):
        self.nc = bass.Bass("TRN2", target_bir_lowering=False)
        self.es = contextlib.ExitStack()
        self.sems = []
        self.bufs = []
        nc = self.nc
        self.E = {}
        for n, e in (("pe", nc.tensor), ("act", nc.scalar), ("dve", nc.vector),
                     ("pool", nc.gpsimd), ("sp", nc.sync)):
            self.E[n] = Eng(n, e, self.sem("prog_" + n))
        self.phase = None
        self.uid = 0
        self.dsp = DsPool(self)

    def sem(self, name):
        s = Sem(self.es.enter_context(self.nc.semaphore(name)), name)
        self.sems.append(s)
        return s

    def begin_phase(self):
        self.phase = contextlib.ExitStack()

    def end_phase(self):
        self.barrier()
        self.phase.close()
        self.phase = None

    def _reg(self, t, name):
        b = Buf(t, name)
        self.bufs.append(b)
        return b

    def sb(self, name, shape, dt, persist=False):
        st = self.es if (persist or self.phase is None) else self.phase
        self.uid += 1
        t = st.enter_context(self.nc.sbuf_tensor("%s_%d" % (name, self.uid), list(shape), dt))
        b = self._reg(t, name)
        b.persist = persist or self.phase is None
        return b

    def ps(self, name, shape, dt):
        t = self.es.enter_context(self.nc.psum_tensor(name, list(shape), dt))
        return self._reg(t, name)

    def dram(self, name, shape, dt, kind="Internal"):
        t = self.nc.dram_tensor(name, list(shape), dt, kind=kind)
        return self._reg(t.ap(), name)

    def op(self, eng, fn, r=(), w=(), sig=True):
        E = self.E[eng]
        for b in r:
            E.waitd(b.w)
        for b in w:
            E.waitd(b.w)
            E.waitd(b.r)
        ins = fn(E.e)
        if sig:
            E.cnt += 1
            ins.then_inc(E.sem.h, 1)
            val = E.cnt
        else:
            val = E.cnt + 1
        for b in r:
            b.r[E.sem] = max(b.r.get(E.sem, 0), val)
        for b in w:
            b.w[E.sem] = max(b.w.get(E.sem, 0), val)
        return ins

    def mm(self, out_b, out_ap, pairs, r, start=True, stop=True):
        n = len(pairs)
        for i, (l, rh) in enumerate(pairs):
            self.op("pe", lambda e, l=l, rh=rh, i=i: e.matmul(
                out_ap, lhsT=l, rhs=rh, start=(start and i == 0), stop=(stop and i == n - 1)),
                r=r if i == 0 else (), w=(out_b,), sig=(i == n - 1))

    def dma(self, q, out_b, out_ap, in_b, in_ap, side):
        E = self.E[q]
        E.waitd(in_b.w)
        E.waitd(out_b.w)
        E.waitd(out_b.r)
        if side.ds is None:
            side.ds = self.dsp.get(side.persist)
        ins = E.e.dma_start(out=out_ap, in_=in_ap)
        side.ds[1] += 16
        ins.then_inc(side.ds[0].h, 16)
        s, v = side.ds
        in_b.r[s] = max(in_b.r.get(s, 0), v)
        out_b.w[s] = max(out_b.w.get(s, 0), v)
        return ins

    def barrier(self):
        allb = {E.sem: E.cnt for E in self.E.values()}
        for b in self.bufs:
            if b.ds is not None:
                allb[b.ds[0]] = max(allb.get(b.ds[0], 0), b.ds[1])
        for E in self.E.values():
            E.waitd(allb)


class DsPool:
    def __init__(self, k):
        self.k = k
        self.free = []
        self.used = []

    def get(self, persist=False):
        if persist:
            return [self.k.sem("dmap%d" % len(self.k.sems)), 0]
        c = self.free.pop() if self.free else [self.k.sem("dmas%d" % len(self.k.sems)), 0]
        self.used.append(c)
        return c

    def recycle(self):
        self.free.extend(self.used)
        self.used = []


def _bcast_row(buf, row_ap):
    return row_ap.partition_broadcast(128)


def build_program(nlayers=DEPTH, debug=False, stop=None):
    k = K()
    nc = k.nc
    dsp = k.dsp

    def sbt(name, shape, dt, persist=False):
        return k.sb(name, shape, dt, persist)

    op, mm, dma = k.op, k.mm, k.dma

    def din(name, shape, dt=F32):
        return k.dram(name, shape, dt, "ExternalInput")

    x_in = din("x", [S, D])
    cT_in = din("cT", [128, 16])
    ada_w = din("ada_w", [nlayers * D, 6 * D])
    ada_b = din("ada_b", [nlayers, 6 * D])
    norm_mix = din("norm_mix", [nlayers, D])
    norm_ffn = din("norm_ffn", [nlayers, D])
    small = din("small", [nlayers, NSM])
    w_in = din("w_in_p", [nlayers * D, 5120])
    w_uq = din("w_uq_p", [nlayers * 512, 768])
    w_ukv = din("w_ukv_p", [nlayers * 256, 1024])
    rel_bias = din("rel_bias", [32, 12])
    w_branch = din("w_branch", [nlayers * 4 * 512, D])
    w_gate = din("w_gate", [nlayers * 4 * D, D])
    w_out = din("w_out", [nlayers * D, D])
    nf_ = (nlayers + 1) // 2
    nm_ = nlayers // 2
    ffn_w1 = din("ffn_w1", [nf_ * D, 5632])
    ffn_w3 = din("ffn_w3", [nf_ * D, 5632])
    ffn_w2 = din("ffn_w2", [nf_ * 5632, D])
    moe_router = din("moe_router", [nm_ * D, 8]) if nm_ else None
    moe_w1 = din("moe_w1", [nm_ * 8 * D, 2816]) if nm_ else None
    moe_w3 = din("moe_w3", [nm_ * 8 * D, 2816]) if nm_ else None
    moe_w2 = din("moe_w2", [nm_ * 8 * 2816, D]) if nm_ else None
    ident_in = din("ident", [128, 128], BF16)
    cs_in = din("cs_tab", [S, 64])
    negcm_in = din("negcm", [128, 4 * 512])
    negtri_in = din("negtri", [128, 128])
    ohd_in = din("ohd", [32, 384])
    ohs_in = din("ohs", [32, 384])
    negw_in = din("negw", [128, 256])
    tri_in = din("tri", [128, 128])
    y_out = k.dram("y", [S, D], F32, "ExternalOutput")

    XA = k.dram("XA", [S, D], F32)
    XB = k.dram("XB", [S, D], F32)
    HT = k.dram("HT", [16 * 128, S], BF16)
    FT = k.dram("FT", [NFB * 128, S], BF16)
    VT = k.dram("VT", [S, NVT], BF16)
    OT = k.dram("OT", [D, S], BF16)
    CUML = k.dram("CUML", [S, 4], F32)
    CUMT = k.dram("CUMT", [4, S], F32)
    IW = k.dram("IW", [S, 8], F32)
    MOD = k.dram("MOD", [nlayers, 6 * D], F32)
    TD = k.dram("TD", [12 * 128, 384], F32)
    MTD = k.dram("MTD", [NST * 128, 32 * 512], BF16)
    WSETS = []
    for si in range(2):
        WSETS.append(dict(
            WIN=k.dram("WIN%d" % si, [D, 5120], BF16), WUQ=k.dram("WUQ%d" % si, [512, 768], BF16),
            WUKV=k.dram("WUKV%d" % si, [256, 1024], BF16), WG=k.dram("WG%d" % si, [4 * D, D], BF16),
            WB=k.dram("WB%d" % si, [4 * 512, D], BF16), WO=k.dram("WO%d" % si, [D, D], BF16),
            FW1=k.dram("FW1_%d" % si, [8 * D, 2816], BF16), FW3=k.dram("FW3_%d" % si, [8 * D, 2816], BF16),
            FW2=k.dram("FW2_%d" % si, [8 * 2816, D], BF16)))
    dbg = {}
    if debug:
        for nm, shp, dt in (("dbg_ft", [NFB * 128, S], BF16), ("dbg_vt", [S, NVT], BF16),
                            ("dbg_ot", [D, S], BF16), ("dbg_xa", [S, D], F32),
                            ("dbg_mod", [nlayers, 2048], F32), ("dbg_cum", [S, 4], F32)):
            dbg[nm] = k.dram(nm, shp, dt, "ExternalOutput")

    PB = [k.ps("pb%d" % i, [128, 512], F32) for i in range(8)]

    def pbf(i):
        return PB[i][:].bitcast(BF16)

    ident = k.sb("ident", [128, 128], BF16, True)
    ones_b = k.sb("ones_b", [128, 128], BF16, True)
    ones_f = k.sb("ones_f", [128, 512], F32, True)
    negcm = k.sb("negcm", [128, 4, 512], F32, True)
    negtri = k.sb("negtri", [128, 128], F32, True)
    tri = k.sb("tri", [128, 128], F32, True)
    ew2 = k.sb("ew2", [128, 4, 256], BF16, True)
    ebs = k.sb("ebs", [128, 2, 2, 512], BF16, True)
    smt = k.sb("smt", [128, NSM], F32, True)
    neg29 = k.sb("neg29", [128, 1], F32, True)

    dma("sp", ident, ident[:], ident_in, ident_in[:, :], ident)
    dma("sp", negcm, negcm[:].rearrange("p a b -> p (a b)"), negcm_in, negcm_in[:, :], negcm)
    dma("sp", negtri, negtri[:], negtri_in, negtri_in[:, :], negtri)
    dma("sp", tri, tri[:], tri_in, tri_in[:, :], tri)
    op("dve", lambda e: e.memset(ones_b[:], 1.0), w=(ones_b,))
    op("dve", lambda e: e.memset(ones_f[:], 1.0), w=(ones_f,))
    op("dve", lambda e: e.memset(neg29[:], -1e29), w=(neg29,))
    pw2 = k.sb("pw2", [128, 32], F32, True)
    for kk_ in range(32):
        op("pool", lambda e, kk_=kk_: e.memset(pw2[:, kk_:kk_ + 1], float(2.0 ** (-kk_))), w=(pw2,))

    rr = [0]

    def cast_eng():
        rr[0] += 1
        return ("act", "dve", "pool")[rr[0] % 3]

    def copy_op(eng, out_ap, in_ap, r, w):
        if eng == "act":
            op("act", lambda e: e.copy(out=out_ap, in_=in_ap), r=r, w=w)
        else:
            op(eng, lambda e: e.tensor_copy(out=out_ap, in_=in_ap), r=r, w=w)

    k.begin_phase()
    ct = sbt("ct", [128, 16], F32)
    sc = sbt("sc", [128, 16], F32)
    awt = [sbt("awt%d" % i, [128, 16, 512], F32) for i in range(2)]
    abt = [sbt("abt%d" % i, [1, 512], F32) for i in range(2)]
    mrow = [sbt("mrow%d" % i, [1, 512], F32) for i in range(2)]
    dma("sp", ct, ct[:], cT_in, cT_in[:, :], ct)
    op("act", lambda e: e.activation(out=sc[:], in_=ct[:], func=AF.Silu), r=(ct,), w=(sc,))
    it = 0
    for L in range(nlayers):
        for n in range(24):
            a, bt, mr = awt[it % 2], abt[it % 2], mrow[it % 2]
            src = ada_w[L * D:(L + 1) * D, n * 512:(n + 1) * 512].rearrange("(k p) n -> p k n", p=128)
            dma("sp", a, a[:], ada_w, src, a)
            dma("sp", bt, bt[:], ada_b, ada_b[L:L + 1, n * 512:(n + 1) * 512], bt)
            pz = PB[it % 2]
            mm(pz, pz[0:1, :], [(sc[:, kc:kc + 1], a[:, kc, :]) for kc in range(16)], r=(sc, a))
            op("dve", lambda e, mr=mr, pz=pz, bt=bt: e.tensor_tensor(out=mr[:], in0=pz[0:1, :], in1=bt[:], op=ALU.add),
               r=(pz, bt), w=(mr,))
            dma("pool", MOD, MOD[L:L + 1, n * 512:(n + 1) * 512], mr, mr[:], mr)
            it += 1
    k.end_phase()
    dsp.recycle()

    k.begin_phase()
    relt = sbt("relt", [32, 12], F32)
    oht = [sbt("ohd_t", [32, 384], F32), sbt("ohs_t", [32, 384], F32)]
    negw = sbt("negw", [128, 256], F32)
    tbc = [sbt("tbc%d" % i, [32, 128], F32) for i in range(2)]
    tdt = [sbt("tdt%d" % i, [128, 384], F32) for i in range(2)]
    w2t = [sbt("w2t%d" % i, [128, 256], F32) for i in range(2)]
    dma("sp", relt, relt[:], rel_bias, rel_bias[:, :], relt)
    dma("sp", oht[0], oht[0][:], ohd_in, ohd_in[:, :], oht[0])
    dma("sp", oht[1], oht[1][:], ohs_in, ohs_in[:, :], oht[1])
    dma("sp", negw, negw[:], negw_in, negw_in[:, :], negw)
    for h in range(12):
        tb, td, w2 = tbc[h % 2], tdt[h % 2], w2t[h % 2]
        oh = oht[0] if h < 4 else oht[1]
        op("dve", lambda e, tb=tb, h=h: e.tensor_copy(out=tb[:], in_=relt[:, h:h + 1].to_broadcast([32, 128])),
           r=(relt,), w=(tb,))
        pz = PB[h % 2]
        mm(pz, pz[:, 0:384], [(tb[:], oh[:])], r=(tb, oh))
        op("act", lambda e, td=td, pz=pz: e.copy(out=td[:], in_=pz[:, 0:384]), r=(pz,), w=(td,))
        dma("pool", TD, TD[h * 128:(h + 1) * 128, :], td, td[:], td)
        skew = bass.AP(TD.t.tensor, h * 128 * 384 + 127, [[383, 128], [1, 256]])
        dma("sp", w2, w2[:], TD, skew, w2)
        if h < 4:
            op("act", lambda e, w2=w2, h=h: e.activation(out=ew2[:, h, :], in_=w2[:], func=AF.Exp), r=(w2,), w=(ew2,))
        else:
            hh = h - 4
            g, hi = hh // 4, hh % 4
            op("dve", lambda e, w2=w2: e.tensor_tensor(out=w2[:], in0=w2[:], in1=negw[:], op=ALU.add), r=(w2, negw), w=(w2,))
            for rel in range(2):
                cs = slice(128, 256) if rel == 0 else slice(0, 128)
                op("act", lambda e, w2=w2, g=g, rel=rel, hi=hi, cs=cs: e.activation(
                    out=ebs[:, g, rel, hi * 128:(hi + 1) * 128], in_=w2[:, cs], func=AF.Exp), r=(w2,), w=(ebs,))
    k.end_phase()
    dsp.recycle()

    BGW = 1024
    bgf = [k.sb("bgf%d" % i, [128, BGW], F32, True) for i in range(2)]
    bgb = [k.sb("bgb%d" % i, [128, BGW], BF16, True) for i in range(2)]

    def precast_items(L, BGW=BGW):
        W = WSETS[L % 2]
        j2 = L // 2
        items = []

        def add(src_b, src_ap, dst_b, dst_ap, R, C):
            ncb = -(-C // BGW)
            while C % ncb:
                ncb += 1
            cb = C // ncb
            nr = max(1, BGW // cb)
            nblk = R // 128
            for c in range(ncb):
                for b0 in range(0, nblk, nr):
                    n = min(nr, nblk - b0)
                    sv = src_ap[b0 * 128:(b0 + n) * 128, c * cb:(c + 1) * cb].rearrange("(n p) c -> p n c", p=128)
                    dv = dst_ap[b0 * 128:(b0 + n) * 128, c * cb:(c + 1) * cb].rearrange("(n p) c -> p n c", p=128)
                    items.append((src_b, sv, dst_b, dv, n, cb))
        add(w_in, w_in[L * D:(L + 1) * D, :], W["WIN"], W["WIN"][:, :], D, 5120)
        add(w_uq, w_uq[L * 512:(L + 1) * 512, :], W["WUQ"], W["WUQ"][:, :], 512, 768)
        add(w_ukv, w_ukv[L * 256:(L + 1) * 256, :], W["WUKV"], W["WUKV"][:, :], 256, 1024)
        add(w_gate, w_gate[L * 4 * D:(L + 1) * 4 * D, :], W["WG"], W["WG"][:, :], 4 * D, D)
        add(w_branch, w_branch[L * 2048:(L + 1) * 2048, :], W["WB"], W["WB"][:, :], 2048, D)
        add(w_out, w_out[L * D:(L + 1) * D, :], W["WO"], W["WO"][:, :], D, D)
        if L % 2 == 1:
            add(moe_w1, moe_w1[j2 * 8 * D:(j2 + 1) * 8 * D, :], W["FW1"], W["FW1"][:, :], 8 * D, 2816)
            add(moe_w3, moe_w3[j2 * 8 * D:(j2 + 1) * 8 * D, :], W["FW3"], W["FW3"][:, :], 8 * D, 2816)
            add(moe_w2, moe_w2[j2 * 8 * 2816:(j2 + 1) * 8 * 2816, :], W["FW2"], W["FW2"][:, :], 8 * 2816, D)
        else:
            for e_ in range(2):
                add(ffn_w1, ffn_w1[j2 * D:(j2 + 1) * D, e_ * 2816:(e_ + 1) * 2816], W["FW1"], W["FW1"][e_ * D:(e_ + 1) * D, :], D, 2816)
                add(ffn_w3, ffn_w3[j2 * D:(j2 + 1) * D, e_ * 2816:(e_ + 1) * 2816], W["FW3"], W["FW3"][e_ * D:(e_ + 1) * D, :], D, 2816)
            add(ffn_w2, ffn_w2[j2 * 5632:(j2 + 1) * 5632, :], W["FW2"], W["FW2"][0:5632, :], 5632, D)
        return items

    class Bg:
        def __init__(self):
            self.gen = None
            self.rate = 0.0
            self.credit = 0.0

        def start(self, L, hooks, fg=False):
            if fg:
                self.tf = [sbt("pcf%d" % i, [128, 4096], F32) for i in range(2)]
                self.tb = [sbt("pcb%d" % i, [128, 4096], BF16) for i in range(2)]
                items = precast_items(L, 4096)
            else:
                self.tf, self.tb = bgf, bgb
                items = precast_items(L)
            self.gen = self._run(items, fg)
            self.rate = len(items) / float(hooks)
            self.credit = 0.0

        def _run(self, items, fg=False):
            q = "sp" if fg else "pool"

            def load(t):
                src_b, sv, dst_b, dv, n, cb = items[t]
                ft = self.tf[t % 2]
                dma(q, ft, ft[:, 0:n * cb].rearrange("p (n c) -> p n c", n=n), src_b, sv, ft)
            load(0)
            for t in range(len(items)):
                src_b, sv, dst_b, dv, n, cb = items[t]
                ft, bt = self.tf[t % 2], self.tb[t % 2]
                if t + 1 < len(items):
                    load(t + 1)
                copy_op(("act", "dve", "pool")[t % 3] if fg else "pool", bt[:, 0:n * cb], ft[:, 0:n * cb], (ft,), (bt,))
                dma("pool", dst_b, dv, bt, bt[:, 0:n * cb].rearrange("p (n c) -> p n c", n=n), bt)
                yield

        def tick(self, w=1.0):
            if self.gen is None:
                return
            self.credit += self.rate * w
            while self.credit >= 1.0 and self.gen is not None:
                self.credit -= 1.0
                self._step()

        def _step(self):
            try:
                next(self.gen)
            except StopIteration:
                self.gen = None

        def finish(self):
            while self.gen is not None:
                self._step()

    bg = Bg()

    def rstd_from_ssq(ssq_ap, out_ap, d, bufs):
        op("act", lambda e: e.activation(out=out_ap, in_=ssq_ap, func=AF.Sqrt, scale=1.0 / d, bias=EPS), r=bufs, w=bufs)
        op("dve", lambda e: e.reciprocal(out=out_ap, in_=out_ap), r=bufs, w=bufs)

    def load_bcast(tile_b, tile_ap, src_b, row_ap):
        dma("sp", tile_b, tile_ap, src_b, row_ap.partition_broadcast(128), tile_b)

    def finish():
        if debug:
            k.begin_phase()
            cp = [sbt("cp%d" % i, [128, 4096], BF16) for i in range(2)]
            cpf = [sbt("cpf%d" % i, [128, 2048], F32) for i in range(2)]
            n = 0
            for src, dst, rows in ((FT, dbg["dbg_ft"], NFB * 128), (OT, dbg["dbg_ot"], D)):
                for r0 in range(0, rows, 128):
                    t = cp[n % 2]
                    n += 1
                    dma("sp", t, t[:], src, src[r0:r0 + 128, :], t)
                    dma("pool", dst, dst[r0:r0 + 128, :], t, t[:], t)
            for r0 in range(0, S, 128):
                t = cp[n % 2]
                n += 1
                dma("sp", t, t[:, 0:NVT], VT, VT[r0:r0 + 128, :], t)
                dma("pool", dbg["dbg_vt"], dbg["dbg_vt"][r0:r0 + 128, :], t, t[:, 0:NVT], t)
                t = cpf[n % 2]
                dma("sp", t, t[:], XA, XA[r0:r0 + 128, :], t)
                dma("pool", dbg["dbg_xa"], dbg["dbg_xa"][r0:r0 + 128, :], t, t[:], t)
                t = cpf[(n + 1) % 2]
                dma("sp", t, t[:, 0:4], CUML, CUML[r0:r0 + 128, :], t)
                dma("pool", dbg["dbg_cum"], dbg["dbg_cum"][r0:r0 + 128, :], t, t[:, 0:4], t)
            t = cpf[0]
            dma("sp", t, t[0:nlayers, :], MOD, MOD[:, 0:2048], t)
            dma("pool", dbg["dbg_mod"], dbg["dbg_mod"][:, :], t, t[0:nlayers, :], t)
            k.end_phase()
        k.barrier()
        k.es.close()
        return nc

    xcur = x_in
    for L in range(nlayers):
        moe = (L % 2 == 1)
        j2 = L // 2
        NE = 8 if moe else 2
        last = (L == nlayers - 1)
        xmid = XA
        xnext = y_out if last else XB

        if L == 0:
            k.begin_phase()
            bg.start(0, 1, fg=True)
            bg.finish()
            k.end_phase()
            dsp.recycle()
        bg.finish()
        k.barrier()
        WS = WSETS[L % 2]
        WIN, WUQ, WUKV, WG, WB, WO, FW1, FW3, FW2 = (WS[n_] for n_ in ("WIN", "WUQ", "WUKV", "WG", "WB", "WO", "FW1", "FW3", "FW2"))
        if not last:
            bg.start(L + 1, 5200 if moe else 4300)

        k.begin_phase()
        load_bcast(smt, smt[:], small, small[L:L + 1, :])
        gm = sbt("gm", [128, D], F32)
        sh = sbt("sh", [128, D], F32)
        tmpD = sbt("tmpD", [128, D], F32)
        load_bcast(gm, gm[:], MOD, MOD[L:L + 1, D:2 * D])
        load_bcast(tmpD, tmpD[:], norm_mix, norm_mix[L:L + 1, :])
        op("dve", lambda e: e.scalar_tensor_tensor(out=gm[:], in0=gm[:], scalar=1.0, in1=tmpD[:], op0=ALU.add, op1=ALU.mult),
           r=(gm, tmpD), w=(gm,))
        load_bcast(sh, sh[:], MOD, MOD[L:L + 1, 0:D])
        xt = [sbt("xt%d" % i, [128, D], F32) for i in range(2)]
        hb = sbt("hb", [128, D], BF16)
        hts = sbt("hts", [128, 16, 512], BF16)
        wint = [sbt("wint%d" % i, [128, 16, 512], BF16) for i in range(2)]
        wuqt = sbt("wuqt", [128, 4, 768], BF16)
        wukvt = sbt("wukvt", [128, 2, 1024], BF16)
        fts = sbt("fts", [128, NFB, 512], BF16)
        vts = [sbt("vts%d" % i, [128, NVT], BF16) for i in range(4)]
        zc = sbt("zc", [128, 1024], F32)
        sq = sbt("sq", [128, 1024], F32)
        nb = sbt("nb", [128, 1024], BF16)
        nT = sbt("nT", [128, 4, 128], BF16)
        st8 = sbt("st8", [128, 16], F32)
        rot = sbt("rot", [128, 4, 64], F32)
        rt = sbt("rt", [128, 4, 4, 32], F32)
        cst = [sbt("cst%d" % i, [128, 64], F32) for i in range(4)]
        iwt = sbt("iwt", [128, 8], F32)
        lft = sbt("lft", [128, 4], F32)
        cumt = sbt("cumt", [128, 4], F32)
        runt = sbt("runt", [128, 4], F32)
        cumTt = sbt("cumTt", [4, 128], F32)
        identf = sbt("identf", [128, 128], F32)
        dma("sp", wuqt, wuqt[:], WUQ, WUQ[:, :].rearrange("(k p) n -> p k n", p=128), wuqt)
        dma("sp", wukvt, wukvt[:], WUKV, WUKV[:, :].rearrange("(k p) n -> p k n", p=128), wukvt)
        op("dve", lambda e: e.memset(runt[:], 0.0), w=(runt,))
        op("dve", lambda e: e.tensor_copy(out=identf[:], in_=ident[:]), r=(ident,), w=(identf,))
        op("dve", lambda e: e.memset(fts[:, B_IK, :], 0.0), w=(fts,))

        def sm(name):
            o, w_ = SM_OFF[name]
            return smt[:, o:o + w_]

        def hnorm(src3, H, d, gain_ap, out3, extra=None):
            sq3 = sq[:, 0:H * d].rearrange("p (h d) -> p h d", h=H)
            op("dve", lambda e: e.tensor_tensor(out=sq3, in0=src3, in1=src3, op=ALU.mult), r=(zc,), w=(sq,))
            op("dve", lambda e: e.tensor_reduce(out=st8[:, 0:H], in_=sq3, axis=AX.X, op=ALU.add), r=(sq,), w=(st8,))
            rstd_from_ssq(st8[:, 0:H], st8[:, 0:H], d, (st8,))
            op("dve", lambda e: e.tensor_tensor(out=sq3, in0=src3, in1=st8[:, 0:H].unsqueeze(2).to_broadcast([128, H, d]), op=ALU.mult),
               r=(zc, st8), w=(sq,))
            op("dve", lambda e: e.tensor_tensor(out=out3, in0=sq3, in1=gain_ap.unsqueeze(1).to_broadcast([128, H, d]), op=ALU.mult),
               r=(sq, smt), w=(nb,))

        def transposes(src_b, blocks, bank, dst_b, dst_ap, rows=128):
            n = len(blocks)
            pv = pbf(bank)
            for i, bap in enumerate(blocks):
                op("pe", lambda e, i=i, bap=bap: e.transpose(out=pv[0:rows, i * 128:(i + 1) * 128], in_=bap, identity=ident[:]),
                   r=(src_b, ident) if i == 0 else (), w=(PB[bank],), sig=(i == n - 1))
            op("act", lambda e: e.copy(out=dst_ap, in_=pv[0:rows, 0:n * 128].rearrange("p (n t) -> p n t", n=n)),
               r=(PB[bank],), w=(dst_b,))

        def rope_apply(src3, out3, cs, H):
            cosb = cs[:, 0:32].unsqueeze(1).to_broadcast([128, H, 32])
            sinb = cs[:, 32:64].unsqueeze(1).to_broadcast([128, H, 32])
            x1, x2 = src3[:, :, 0:32], src3[:, :, 32:64]
            op("dve", lambda e: e.tensor_tensor(out=rt[:, 0], in0=x1, in1=cosb, op=ALU.mult), r=(rot, cst_b[0]), w=(rt,))
            op("dve", lambda e: e.tensor_tensor(out=rt[:, 1], in0=x2, in1=sinb, op=ALU.mult), r=(rot, cst_b[0]), w=(rt,))
            op("dve", lambda e: e.tensor_tensor(out=rt[:, 2], in0=x1, in1=sinb, op=ALU.mult), r=(rot, cst_b[0]), w=(rt,))
            op("dve", lambda e: e.tensor_tensor(out=rt[:, 3], in0=x2, in1=cosb, op=ALU.mult), r=(rot, cst_b[0]), w=(rt,))
            op("dve", lambda e: e.tensor_tensor(out=out3[:, :, 0:32], in0=rt[:, 0], in1=rt[:, 1], op=ALU.subtract), r=(rt,), w=(nb,))
            op("dve", lambda e: e.tensor_tensor(out=out3[:, :, 32:64], in0=rt[:, 2], in1=rt[:, 3], op=ALU.add), r=(rt,), w=(nb,))

        cst_b = [None]
        for st in range(NST):
            for tt in range(4):
                ti = st * 4 + tt
                xtile = xt[ti % 2]
                dma("sp", xtile, xtile[:], xcur, xcur[ti * 128:(ti + 1) * 128, :], xtile)
                dma("sp", cst[tt], cst[tt][:], cs_in, cs_in[ti * 128:(ti + 1) * 128, :], cst[tt])
                op("act", lambda e: e.activation(out=tmpD[:], in_=xtile[:], func=AF.Square, accum_out=st8[:, 8:9]),
                   r=(xtile,), w=(tmpD, st8))
                rstd_from_ssq(st8[:, 8:9], st8[:, 8:9], D, (st8,))
                op("dve", lambda e: e.scalar_tensor_tensor(out=tmpD[:], in0=xtile[:], scalar=st8[:, 8:9], in1=gm[:], op0=ALU.mult, op1=ALU.mult),
                   r=(xtile, st8, gm), w=(tmpD,))
                op("pool", lambda e: e.tensor_tensor(out=hb[:], in0=tmpD[:], in1=sh[:], op=ALU.add), r=(tmpD, sh), w=(hb,))
                for half in range(2):
                    transposes(hb, [hb[:, (half * 8 + i) * 128:(half * 8 + i + 1) * 128] for i in range(8)], 6 + half,
                               hts, hts[:, half * 8:half * 8 + 8, tt * 128:(tt + 1) * 128])
            for half in range(2):
                dma("pool", HT, HT[half * 1024:(half + 1) * 1024, st * 512:(st + 1) * 512].rearrange("(k p) t -> p k t", p=128),
                    hts, hts[:, half * 8:(half + 1) * 8, :], hts)
            for c in range(10):
                wt = wint[c % 2]
                ncols = {1: 320, 6: 332, 9: 256}.get(c, 512)
                dma("sp", wt, wt[:], WIN, WIN[:, c * 512:(c + 1) * 512].rearrange("(k p) n -> p k n", p=128), wt)
                for tt in range(4):
                    pz = PB[tt % 4]
                    ts_ = slice(tt * 128, (tt + 1) * 128)
                    mm(pz, pz[:, 0:ncols], [(hts[:, kc, ts_], wt[:, kc, 0:ncols]) for kc in range(16)], r=(hts, wt))
                for tt in range(4):
                    ti = st * 4 + tt
                    bg.tick()
                    cst_b[0] = cst[tt]
                    cs = cst[tt]
                    pz = PB[tt % 4]
                    ts_ = slice(tt * 128, (tt + 1) * 128)
                    vt = vts[tt]
                    if c in (4,):
                        op("act", lambda e: e.copy(out=vt[:, V_FOX:V_FOX + 512], in_=pz[:, 0:512]), r=(pz,), w=(vt,))
                        continue
                    op("act", lambda e: e.copy(out=zc[:, 0:ncols], in_=pz[:, 0:ncols]), r=(pz,), w=(zc,))
                    if c == 0:
                        hnorm(zc[:, 0:512].rearrange("p (h d) -> p h d", h=1), 1, 512, sm("cq"), nb[:, 0:512].rearrange("p (h d) -> p h d", h=1))
                        transposes(nb, [nb[:, i * 128:(i + 1) * 128] for i in range(4)], 6, nT, nT[:, 0:4, :])
                        p0, p1 = PB[4], PB[5]
                        mm(p0, p0[:, 0:512], [(nT[:, kc, :], wuqt[:, kc, 0:512]) for kc in range(4)], r=(nT, wuqt))
                        mm(p1, p1[:, 0:256], [(nT[:, kc, :], wuqt[:, kc, 512:768]) for kc in range(4)], r=(nT, wuqt))
                        op("act", lambda e: e.copy(out=zc[:, 0:512], in_=p0[:, 0:512]), r=(p0,), w=(zc,))
                        op("act", lambda e: e.copy(out=zc[:, 512:768], in_=p1[:, 0:256]), r=(p1,), w=(zc,))
                        op("dve", lambda e: e.tensor_tensor(out=sq[:, 0:768], in0=zc[:, 0:768], in1=zc[:, 0:768], op=ALU.mult), r=(zc,), w=(sq,))
                        op("dve", lambda e: e.tensor_reduce(out=st8[:, 0:4], in_=sq[:, 0:512].rearrange("p (h d) -> p h d", h=4), axis=AX.X, op=ALU.add), r=(sq,), w=(st8,))
                        op("dve", lambda e: e.tensor_reduce(out=st8[:, 4:8], in_=sq[:, 512:768].rearrange("p (h d) -> p h d", h=4), axis=AX.X, op=ALU.add), r=(sq,), w=(st8,))
                        op("dve", lambda e: e.tensor_tensor(out=st8[:, 0:4], in0=st8[:, 0:4], in1=st8[:, 4:8], op=ALU.add), r=(st8,), w=(st8,))
                        rstd_from_ssq(st8[:, 0:4], st8[:, 0:4], 192, (st8,))
                        o_, _w = SM_OFF["mq"]
                        gq_n, gq_r = smt[:, o_:o_ + 128], smt[:, o_ + 128:o_ + 192]
                        z3 = zc[:, 0:512].rearrange("p (h d) -> p h d", h=4)
                        s3 = sq[:, 0:512].rearrange("p (h d) -> p h d", h=4)
                        op("dve", lambda e: e.tensor_tensor(out=s3, in0=z3, in1=st8[:, 0:4].unsqueeze(2).to_broadcast([128, 4, 128]), op=ALU.mult), r=(zc, st8), w=(sq,))
                        op("dve", lambda e: e.tensor_tensor(out=nb[:, 0:512].rearrange("p (h d) -> p h d", h=4), in0=s3, in1=gq_n.unsqueeze(1).to_broadcast([128, 4, 128]), op=ALU.mult), r=(sq, smt), w=(nb,))
                        r3 = zc[:, 512:768].rearrange("p (h d) -> p h d", h=4)
                        op("dve", lambda e: e.tensor_tensor(out=rot[:], in0=r3, in1=st8[:, 0:4].unsqueeze(2).to_broadcast([128, 4, 64]), op=ALU.mult), r=(zc, st8), w=(rot,))
                        op("dve", lambda e: e.tensor_tensor(out=rot[:], in0=rot[:], in1=gq_r.unsqueeze(1).to_broadcast([128, 4, 64]), op=ALU.mult), r=(rot, smt), w=(rot,))
                        rope_apply(rot, nb[:, 512:768].rearrange("p (h d) -> p h d", h=4), cs, 4)
                        transposes(nb, [nb[:, i * 128:(i + 1) * 128] for i in range(6)], 7, fts, fts[:, B_QN:B_QN + 6, ts_])
                    elif c == 1:
                        hnorm(zc[:, 0:256].rearrange("p (h d) -> p h d", h=1), 1, 256, sm("ckv"), nb[:, 0:256].rearrange("p (h d) -> p h d", h=1))
                        transposes(nb, [nb[:, i * 128:(i + 1) * 128] for i in range(2)], 6, nT, nT[:, 0:2, :])
                        p0, p1 = PB[4], PB[5]
                        mm(p0, p0[:, 0:512], [(nT[:, kc, :], wukvt[:, kc, 0:512]) for kc in range(2)], r=(nT, wukvt))
                        mm(p1, p1[:, 0:512], [(nT[:, kc, :], wukvt[:, kc, 512:1024]) for kc in range(2)], r=(nT, wukvt))
                        op("act", lambda e: e.copy(out=vt[:, V_MLA:V_MLA + 512], in_=p1[:, 0:512]), r=(p1,), w=(vt,))
                        op("act", lambda e: e.copy(out=zc[:, 512:1024], in_=p0[:, 0:512]), r=(p0,), w=(zc,))
                        op("dve", lambda e: e.tensor_tensor(out=sq[:, 0:512], in0=zc[:, 512:1024], in1=zc[:, 512:1024], op=ALU.mult), r=(zc,), w=(sq,))
                        op("dve", lambda e: e.tensor_reduce(out=st8[:, 0:4], in_=sq[:, 0:512].rearrange("p (h d) -> p h d", h=4), axis=AX.X, op=ALU.add), r=(sq,), w=(st8,))
                        op("dve", lambda e: e.tensor_tensor(out=sq[:, 512:576], in0=zc[:, 256:320], in1=zc[:, 256:320], op=ALU.mult), r=(zc,), w=(sq,))
                        op("dve", lambda e: e.tensor_reduce(out=st8[:, 4:5], in_=sq[:, 512:576], axis=AX.X, op=ALU.add), r=(sq,), w=(st8,))
                        op("dve", lambda e: e.tensor_scalar(out=st8[:, 0:4], in0=st8[:, 0:4], scalar1=st8[:, 4:5], scalar2=None, op0=ALU.add), r=(st8,), w=(st8,))
                        rstd_from_ssq(st8[:, 0:4], st8[:, 0:4], 192, (st8,))
                        o_, _w = SM_OFF["mk"]
                        gk_n, gk_r = smt[:, o_:o_ + 128], smt[:, o_ + 128:o_ + 192]
                        z3 = zc[:, 512:1024].rearrange("p (h d) -> p h d", h=4)
                        s3 = sq[:, 0:512].rearrange("p (h d) -> p h d", h=4)
                        op("dve", lambda e: e.tensor_tensor(out=s3, in0=z3, in1=st8[:, 0:4].unsqueeze(2).to_broadcast([128, 4, 128]), op=ALU.mult), r=(zc, st8), w=(sq,))
                        op("dve", lambda e: e.tensor_tensor(out=nb[:, 0:512].rearrange("p (h d) -> p h d", h=4), in0=s3, in1=gk_n.unsqueeze(1).to_broadcast([128, 4, 128]), op=ALU.mult), r=(sq, smt), w=(nb,))
                        krb = zc[:, 256:320].unsqueeze(1).to_broadcast([128, 4, 64])
                        op("dve", lambda e: e.tensor_tensor(out=rot[:], in0=krb, in1=st8[:, 0:4].unsqueeze(2).to_broadcast([128, 4, 64]), op=ALU.mult), r=(zc, st8), w=(rot,))
                        op("dve", lambda e: e.tensor_tensor(out=rot[:], in0=rot[:], in1=gk_r.unsqueeze(1).to_broadcast([128, 4, 64]), op=ALU.mult), r=(rot, smt), w=(rot,))
                        rope_apply(rot, nb[:, 512:768].rearrange("p (h d) -> p h d", h=4), cs, 4)
                        transposes(nb, [nb[:, i * 128:(i + 1) * 128] for i in range(6)], 7, fts, fts[:, B_KN:B_KN + 6, ts_])
                    elif c in (2, 3, 5):
                        gname, blk = {2: ("fq", B_FQ), 3: ("fk", B_FK), 5: ("dq", B_DQ)}[c]
                        hnorm(zc[:, 0:512].rearrange("p (h d) -> p h d", h=4), 4, 128, sm(gname), nb[:, 0:512].rearrange("p (h d) -> p h d", h=4))
                        transposes(nb, [nb[:, i * 128:(i + 1) * 128] for i in range(4)], 6 + (c % 2), fts, fts[:, blk:blk + 4, ts_])
                    elif c == 6:
                        hnorm(zc[:, 0:128].rearrange("p (h d) -> p h d", h=1), 1, 128, sm("dk"), nb[:, 0:128].rearrange("p (h d) -> p h d", h=1))
                        op("pool", lambda e: e.tensor_copy(out=vt[:, V_DSA:V_DSA + 128], in_=zc[:, 128:256]), r=(zc,), w=(vt,))
                        op("pool", lambda e: e.tensor_copy(out=nb[:, 128:192], in_=zc[:, 256:320]), r=(zc,), w=(nb,))
                        transposes(nb, [nb[:, 0:128]], 6, fts, fts[:, B_DK:B_DK + 1, ts_])
                        transposes(nb, [nb[:, 128:192]], 7, fts, fts[0:64, B_IK:B_IK + 1, ts_], rows=64)
                        op("dve", lambda e: e.tensor_scalar(out=iwt[:], in0=zc[:, 320:328], scalar1=float(8 ** -0.5 * 64 ** -0.5), scalar2=None, op0=ALU.mult), r=(zc,), w=(iwt,))
                        dma("pool", IW, IW[ti * 128:(ti + 1) * 128, :], iwt, iwt[:], iwt)
                        op("dve", lambda e: e.tensor_tensor(out=lft[:], in0=zc[:, 328:332], in1=sm("fb"), op=ALU.add), r=(zc, smt), w=(lft,))
                        op("act", lambda e: e.activation(out=lft[:], in_=lft[:], func=AF.Exp, scale=-1.0), r=(lft,), w=(lft,))
                        op("act", lambda e: e.activation(out=lft[:], in_=lft[:], func=AF.Ln, bias=1.0), r=(lft,), w=(lft,))
                        p0 = PB[4]
                        mm(p0, p0[:, 0:4], [(tri[:], lft[:])], r=(tri, lft))
                        mm(p0, p0[:, 4:8], [(ones_f[:, 0:128], lft[:])], r=(ones_f, lft))
                        op("dve", lambda e: e.tensor_tensor(out=cumt[:], in0=p0[:, 0:4], in1=runt[:], op=ALU.add), r=(p0, runt), w=(cumt,))
                        op("dve", lambda e: e.tensor_tensor(out=runt[:], in0=p0[:, 4:8], in1=runt[:], op=ALU.add), r=(p0, runt), w=(runt,))
                        dma("pool", CUML, CUML[ti * 128:(ti + 1) * 128, :], cumt, cumt[:], cumt)
                        p1 = PB[5]
                        op("pe", lambda e: e.transpose(out=p1[0:4, 0:128], in_=cumt[:], identity=identf[:]), r=(cumt, identf), w=(p1,))
                        op("act", lambda e: e.copy(out=cumTt[:], in_=p1[0:4, 0:128]), r=(p1,), w=(cumTt,))
                        dma("pool", CUMT, CUMT[:, ti * 128:(ti + 1) * 128], cumTt, cumTt[:], cumTt)
                    elif c == 7:
                        op("pool", lambda e: e.tensor_copy(out=nb[:, 0:512], in_=zc[:, 0:512]), r=(zc,), w=(nb,))
                        transposes(nb, [nb[:, i * 128:(i + 1) * 128] for i in range(4)], 7, fts, fts[:, B_IQ:B_IQ + 4, ts_])
                    elif c == 8:
                        hnorm(zc[:, 0:512].rearrange("p (h d) -> p h d", h=8), 8, 64, sm("sq"), nb[:, 0:512].rearrange("p (h d) -> p h d", h=8))
                        transposes(nb, [nb[:, i * 128:(i + 1) * 128] for i in range(4)], 6, fts, fts[:, B_SQ:B_SQ + 4, ts_])
                    elif c == 9:
                        hnorm(zc[:, 0:128].rearrange("p (h d) -> p h d", h=2), 2, 64, sm("sk"), nb[:, 0:128].rearrange("p (h d) -> p h d", h=2))
                        op("pool", lambda e: e.tensor_copy(out=vt[:, V_SWA:V_SWA + 128], in_=zc[:, 128:256]), r=(zc,), w=(vt,))
                        transposes(nb, [nb[:, 0:128]], 7, fts, fts[:, B_SK:B_SK + 1, ts_])
                        dma("pool", VT, VT[ti * 128:(ti + 1) * 128, :], vt, vt[:], vt)
            for b0 in range(0, NFB, 7):
                dma("pool", FT, FT[b0 * 128:(b0 + 7) * 128, st * 512:(st + 1) * 512].rearrange("(b p) t -> p b t", p=128),
                    fts, fts[:, b0:b0 + 7, :], fts)
        k.end_phase()
        dsp.recycle()

        if stop == "p1":
            return finish()
        def attn_pair(psS, pairs, r, pT, exp_scale, bias_ap=None, bias_b=None, addmask=None, pre=None):
            mm(psS, psS[:, 0:512], pairs, r=r)
            if pre is not None:
                pre(psS)
            if addmask is not None:
                op("dve", lambda e: e.tensor_tensor(out=mtmp[:], in0=psS[:, 0:512], in1=addmask, op=ALU.add), r=(psS, negcm), w=(mtmp,))
                op("act", lambda e: e.activation(out=pT[:], in_=mtmp[:], func=AF.Exp, scale=exp_scale), r=(mtmp,), w=(pT,))
            else:
                op("act", lambda e: e.activation(out=pT[:], in_=psS[:, 0:512], func=AF.Exp, scale=exp_scale), r=(psS,), w=(pT,))

        def finalize(psO, psD, rows, out_b, rdt, extra_add=None):
            if extra_add is not None:
                op("dve", lambda e: e.tensor_tensor(out=rdt[0:rows, :], in0=psD[0:rows, :], in1=extra_add, op=ALU.add), r=(psD, est), w=(rdt,))
                op("dve", lambda e: e.reciprocal(out=rdt[0:rows, :], in_=rdt[0:rows, :]), r=(rdt,), w=(rdt,))
            else:
                op("dve", lambda e: e.reciprocal(out=rdt[0:rows, :], in_=psD[0:rows, :]), r=(psD,), w=(rdt,))
            op("dve", lambda e: e.tensor_tensor(out=out_b[0:rows, :], in0=psO[0:rows, :], in1=rdt[0:rows, :], op=ALU.mult), r=(psO, rdt), w=(out_b,))

        k.begin_phase()
        kn = [sbt("kn%d" % i, [128, S], BF16) for i in range(2)]
        kr = [sbt("kr%d" % i, [64, S], BF16) for i in range(2)]
        vv = [sbt("vv%d" % i, [128, 32, 128], BF16) for i in range(2)]
        qn = [sbt("qn%d" % i, [128, 512], BF16) for i in range(2)]
        qr = [sbt("qr%d" % i, [64, 512], BF16) for i in range(2)]
        pTa = [sbt("pTa%d" % i, [128, 512], BF16) for i in range(2)]
        mtmp = sbt("mtmp", [128, 512], F32)
        rdta = sbt("rdta", [128, 512], F32)
        otla = [sbt("otla%d" % i, [128, 512], BF16) for i in range(2)]
        ca = sbt("ca", [1, S], F32)
        cnq = [sbt("cnq%d" % i, [1, 512], F32) for i in range(2)]
        ikt = sbt("ikt", [64, S], BF16)
        iqt = [sbt("iqt%d" % i, [64, 8, 128], BF16) for i in range(2)]
        iwq = [sbt("iwq%d" % i, [128, 8], F32) for i in range(2)]
        Itl = [sbt("ItA", [128, S], F32), sbt("ItB", [128, S], F32)]
        bsl = [sbt("bsA", [128, 8], F32), sbt("bsB", [128, 8], F32)]
        stl = [sbt("stA", [128, 32], F32), sbt("stB", [128, 32], F32)]
        Mt = sbt("Mt", [128, S], BF16)
        MT = sbt("MT", [128, 32, 512], BF16)
        rl = [sbt("rl%d" % i, [128, 512], F32) for i in range(2)]
        m8 = sbt("m8", [128, 8], F32)

        def gen_mla_fox():
            cnt = 0
            for mixer in ("mla", "fox"):
                scale = (192 ** -0.5) if mixer == "mla" else (128 ** -0.5)
                qblk, kblk, vbase, obase = (B_QN, B_KN, V_MLA, 0) if mixer == "mla" else (B_FQ, B_FK, V_FOX, 512)
                for h in range(4):
                    knh, vvh = kn[h % 2], vv[h % 2]
                    dma("sp", knh, knh[:], FT, FT[(kblk + h) * 128:(kblk + h + 1) * 128, :], knh)
                    for q4 in range(4):
                        dma("sp", vvh, vvh[:, q4 * 8:(q4 + 1) * 8, :], VT,
                            VT[q4 * 1024:(q4 + 1) * 1024, vbase + h * 128:vbase + (h + 1) * 128].rearrange("(kb p) d -> p kb d", p=128), vvh)
                    if mixer == "mla":
                        krh = kr[h % 2]
                        dma("sp", krh, krh[:], FT, FT[B_KR * 128 + h * 64:B_KR * 128 + (h + 1) * 64, :], krh)
                    else:
                        dma("sp", ca, ca[:], CUMT, CUMT[h:h + 1, :], ca)
                        op("pool", lambda e: e.tensor_scalar(out=ca[:], in0=ca[:], scalar1=float(1.0 / scale), scalar2=None, op0=ALU.mult), r=(ca,), w=(ca,))
                    yield 0.5
                    for j in range(NST):
                        qs = slice(j * 512, (j + 1) * 512)
                        qnj = qn[(h * NST + j) % 2]
                        dma("sp", qnj, qnj[:], FT, FT[(qblk + h) * 128:(qblk + h + 1) * 128, qs], qnj)
                        if mixer == "mla":
                            qrj = qr[(h * NST + j) % 2]
                            dma("sp", qrj, qrj[:], FT, FT[B_QR * 128 + h * 64:B_QR * 128 + (h + 1) * 64, qs], qrj)
                        else:
                            cnj = cnq[(h * NST + j) % 2]
                            op("pool", lambda e: e.tensor_scalar(out=cnj[:], in0=ca[0:1, qs], scalar1=-1.0, scalar2=None, op0=ALU.mult), r=(ca,), w=(cnj,))
                        psO, psD = PB[4], PB[5]
                        nkb = 4 * j + 4
                        def qk(kb_, c_):
                            ks_ = slice(kb_ * 128, (kb_ + 1) * 128)
                            ps_ = PB[c_ % 2]
                            if mixer == "mla":
                                pairs = [(knh[:, ks_], qnj[:]), (krh[0:64, ks_], qrj[0:64, :])]
                                r = (knh, krh, qnj, qrj)
                            else:
                                pairs = [(knh[:, ks_], qnj[:]), (ca[0:1, ks_], ones_f[0:1, 0:512]), (ones_f[0:1, 0:128], cnj[0:1, :])]
                                r = (knh, qnj, ca, cnj, ones_f)
                            mm(ps_, ps_[:, 0:512], pairs, r=r)
                        qk(0, cnt)
                        for kb in range(nkb):
                            psS = PB[cnt % 2]
                            pT = pTa[cnt % 2]
                            cnt += 1
                            if kb + 1 < nkb:
                                qk(kb + 1, cnt)
                            if kb >= 4 * j:
                                op("dve", lambda e: e.tensor_tensor(out=mtmp[:], in0=psS[:, 0:512], in1=negcm[:, kb - 4 * j, :], op=ALU.add), r=(psS, negcm), w=(mtmp,))
                                op("act", lambda e: e.activation(out=pT[:], in_=mtmp[:], func=AF.Exp, scale=scale), r=(mtmp,), w=(pT,))
                            else:
                                op("act", lambda e: e.activation(out=pT[:], in_=psS[:, 0:512], func=AF.Exp, scale=scale), r=(psS,), w=(pT,))
                            mm(psO, psO[:, 0:512], [(vvh[:, kb, :], pT[:])], r=(vvh, pT), start=(kb == 0), stop=(kb == nkb - 1))
                            mm(psD, psD[:, 0:512], [(ones_b[:], pT[:])], r=(ones_b, pT), start=(kb == 0), stop=(kb == nkb - 1))
                            yield (3.5 if mixer == "mla" else 7.0)
                        ot = otla[j % 2]
                        finalize(psO, psD, 128, ot, rdta)
                        dma("pool", OT, OT[obase + h * 128:obase + (h + 1) * 128, qs], ot, ot[:], ot)
                        yield 3.0

        NIT = 30

        def gen_indexer():
            cnt = 0
            dma("sp", ikt, ikt[:], FT, FT[B_IK * 128:B_IK * 128 + 64, :], ikt)
            for j in range(NST):
                op("pool", lambda e: e.memset(MT[:, 4 * j:4 * j + 4, :], 0.0), w=(MT,))
                for up in range(2):
                    blks = []
                    for ui in range(2):
                        u = 2 * up + ui
                        i = 4 * j + u
                        nk = (i + 1) * 128
                        It, bs, stp = Itl[ui], bsl[ui], stl[ui]
                        blks.append((u, i, nk, It, bs, stp))
                        iq_, iw_ = iqt[i % 2], iwq[i % 2]
                        dma("sp", iq_, iq_[:], FT, FT[B_IQ * 128:(B_IQ + 4) * 128, i * 128:(i + 1) * 128].rearrange("(jj p) t -> p jj t", p=64), iq_)
                        dma("sp", iw_, iw_[:], IW, IW[i * 128:(i + 1) * 128, :], iw_)
                        for c0 in range(0, nk, 512):
                            cw = min(512, nk - c0)
                            for jj in range(8):
                                psS = PB[2 + cnt % 2]
                                rlt = rl[cnt % 2]
                                cnt += 1
                                mm(psS, psS[:, 0:cw], [(iq_[0:64, jj, :], ikt[0:64, c0:c0 + cw])], r=(iq_, ikt))
                                op("act", lambda e: e.activation(out=rlt[:, 0:cw], in_=psS[:, 0:cw], func=AF.Relu), r=(psS,), w=(rlt,))
                                if jj == 0:
                                    op("dve", lambda e: e.tensor_scalar(out=It[:, c0:c0 + cw], in0=rlt[:, 0:cw], scalar1=iw_[:, 0:1], scalar2=None, op0=ALU.mult),
                                       r=(rlt, iw_), w=(It,))
                                else:
                                    op("dve", lambda e: e.scalar_tensor_tensor(out=It[:, c0:c0 + cw], in0=rlt[:, 0:cw], scalar=iw_[:, jj:jj + 1], in1=It[:, c0:c0 + cw],
                                                                                 op0=ALU.mult, op1=ALU.add), r=(rlt, iw_, It), w=(It,))
                                if jj % 2 == 1:
                                    yield 2.0 * cw / 960.0 + 0.2
                        if nk > 256:
                            op("dve", lambda e: e.tensor_reduce(out=bs[:, 5:6], in_=It[:, 0:nk], axis=AX.X, op=ALU.max, apply_absolute_value=True), r=(It,), w=(bs,))
                            op("dve", lambda e: e.tensor_scalar(out=bs[:, 5:6], in0=bs[:, 5:6], scalar1=1.001, scalar2=1e-3, op0=ALU.mult, op1=ALU.add), r=(bs,), w=(bs,))
                            op("dve", lambda e: e.tensor_scalar(out=stp[:], in0=pw2[:], scalar1=bs[:, 5:6], scalar2=None, op0=ALU.mult), r=(pw2, bs), w=(stp,))
                            op("dve", lambda e: e.tensor_scalar(out=bs[:, 0:1], in0=bs[:, 5:6], scalar1=-1.0, scalar2=None, op0=ALU.mult), r=(bs,), w=(bs,))
                            op("dve", lambda e: e.memset(bs[:, 2:3], 0.0), w=(bs,))
                        op("dve", lambda e: e.tensor_tensor(out=It[:, i * 128:(i + 1) * 128], in0=It[:, i * 128:(i + 1) * 128], in1=negtri[:], op=ALU.add), r=(It, negtri), w=(It,))
                        yield 1.0
                    if blks[0][2] > 256:
                        for kk in range(NIT):
                            for (u, i, nk, It, bs, stp) in blks:
                                op("act", lambda e: e.activation(out=Mt[:, 0:nk], in_=It[:, 0:nk], func=AF.Sign, bias=bs[:, 2:3], scale=-1.0, accum_out=bs[:, 3:4]),
                                   r=(It, bs), w=(Mt, bs))
                                op("dve", lambda e: e.scalar_tensor_tensor(out=bs[:, 4:5], in0=bs[:, 3:4], scalar=float(nk - 512) + 0.5, in1=stp[:, kk:kk + 1],
                                                                             op0=ALU.is_le, op1=ALU.mult), r=(bs, stp), w=(bs,))
                                op("dve", lambda e: e.scalar_tensor_tensor(out=bs[:, 2:3], in0=bs[:, 4:5], scalar=stp[:, kk + 1:kk + 2], in1=bs[:, 0:1],
                                                                             op0=ALU.add, op1=ALU.add), r=(bs, stp), w=(bs,))
                                op("dve", lambda e: e.tensor_tensor(out=bs[:, 0:1], in0=bs[:, 0:1], in1=bs[:, 4:5], op=ALU.add), r=(bs,), w=(bs,))
                            yield (blks[0][2] + blks[1][2]) / 1400.0 + 1.0
                    for (u, i, nk, It, bs, stp) in blks:
                        if nk > 256:
                            thr, thr_b = bs[:, 0:1], bs
                        else:
                            thr, thr_b = neg29[:, 0:1], neg29
                        op("dve", lambda e: e.tensor_scalar(out=Mt[:, 0:nk], in0=It[:, 0:nk], scalar1=thr, scalar2=None, op0=ALU.is_ge), r=(It, thr_b), w=(Mt,))
                        for kb0 in range(0, i + 1, 8):
                            n = min(8, i + 1 - kb0)
                            tb_ = 6 + (kb0 // 8) % 2
                            pv = pbf(tb_)
                            for t_ in range(n):
                                kb = kb0 + t_
                                op("pe", lambda e: e.transpose(out=pv[:, t_ * 128:(t_ + 1) * 128], in_=Mt[:, kb * 128:(kb + 1) * 128], identity=ident[:]),
                                   r=(Mt, ident) if t_ == 0 else (), w=(PB[tb_],), sig=(t_ == n - 1))
                            op("act", lambda e: e.copy(out=MT[:, kb0:kb0 + n, u * 128:(u + 1) * 128], in_=pv[:, 0:n * 128].rearrange("p (n t) -> p n t", n=n)),
                               r=(PB[tb_],), w=(MT,))
                            yield 1.0
                nkb = 4 * j + 4
                dma("pool", MTD, MTD[j * 128:(j + 1) * 128, 0:nkb * 512], MT, MT[:, 0:nkb, :].rearrange("p a b -> p (a b)"), MT)
                yield 1.0

        gens = [[gen_mla_fox(), 0.0, 1.0], [gen_indexer(), 0.0, 2.0]]
        while gens:
            g_ = min(gens, key=lambda x: x[1])
            try:
                g_[1] += next(g_[0]) * g_[2]
            except StopIteration:
                gens.remove(g_)
            bg.tick()
        k.end_phase()
        dsp.recycle()

        k.begin_phase()
        dkt = sbt("dkt", [128, S], BF16)
        dvv = sbt("dvv", [128, 32, 128], BF16)
        mtj = [sbt("mtj%d" % i, [128, 32, 512], BF16) for i in range(2)]
        dq = [sbt("dq%d" % i, [128, 512], BF16) for i in range(2)]
        pTt = [sbt("pT%d" % i, [128, 512], BF16) for i in range(3)]
        rdt = sbt("rdt", [128, 512], F32)
        otl = [sbt("otl%d" % i, [128, 512], BF16) for i in range(2)]
        dma("sp", dkt, dkt[:], FT, FT[B_DK * 128:(B_DK + 1) * 128, :], dkt)
        for q4 in range(4):
            dma("sp", dvv, dvv[:, q4 * 8:(q4 + 1) * 8, :], VT,
                VT[q4 * 1024:(q4 + 1) * 1024, V_DSA:V_DSA + 128].rearrange("(kb p) d -> p kb d", p=128), dvv)
        dscale = 128 ** -0.5
        cnt = 0
        for j in range(NST):
            qs = slice(j * 512, (j + 1) * 512)
            nkb = 4 * j + 4
            MTj = mtj[j % 2]
            dma("sp", MTj, MTj[:, 0:nkb, :].rearrange("p a b -> p (a b)"), MTD, MTD[j * 128:(j + 1) * 128, 0:nkb * 512], MTj)
            for h in range(4):
                dqh = dq[h % 2]
                dma("sp", dqh, dqh[:], FT, FT[(B_DQ + h) * 128:(B_DQ + h + 1) * 128, qs], dqh)
                psO, psD = PB[4 + (h % 2) * 2], PB[5 + (h % 2) * 2]
                mm(PB[cnt % 3], PB[cnt % 3][:, 0:512], [(dkt[:, 0:128], dqh[:])], r=(dkt, dqh))
                for kb in range(nkb):
                    psS = PB[cnt % 3]
                    pT = pTt[cnt % 3]
                    cnt += 1
                    bg.tick()
                    if kb + 1 < nkb:
                        mm(PB[cnt % 3], PB[cnt % 3][:, 0:512], [(dkt[:, (kb + 1) * 128:(kb + 2) * 128], dqh[:])], r=(dkt, dqh))
                    op("act", lambda e: e.activation(out=pT[:], in_=psS[:, 0:512], func=AF.Exp, scale=dscale), r=(psS,), w=(pT,))
                    op("dve", lambda e: e.tensor_tensor(out=pT[:], in0=pT[:], in1=MTj[:, kb, :], op=ALU.mult), r=(pT, MTj), w=(pT,))
                    rb = kb - 4 * j
                    if rb >= -1:
                        lo, hi = max(0, rb * 128), min(512, rb * 128 + 256)
                        op("dve", lambda e: e.tensor_tensor(out=pT[:, lo:hi], in0=pT[:, lo:hi], in1=ew2[:, h, lo - rb * 128:hi - rb * 128], op=ALU.mult),
                           r=(pT, ew2), w=(pT,))
                    mm(psO, psO[:, 0:512], [(dvv[:, kb, :], pT[:])], r=(dvv, pT), start=(kb == 0), stop=(kb == nkb - 1))
                    mm(psD, psD[:, 0:512], [(ones_b[:], pT[:])], r=(ones_b, pT), start=(kb == 0), stop=(kb == nkb - 1))
                ot = otl[h % 2]
                finalize(psO, psD, 128, ot, rdt)
                dma("pool", OT, OT[1024 + h * 128:1024 + (h + 1) * 128, qs], ot, ot[:], ot)
        k.end_phase()
        dsp.recycle()

        k.begin_phase()
        skt = sbt("skt", [64, 2, S], BF16)
        svv = sbt("svv", [128, 32, 128], BF16)
        sqt = [sbt("sqt%d" % i, [64, 8, 128], BF16) for i in range(2)]
        pTt = [sbt("pT%d" % i, [128, 512], BF16) for i in range(2)]
        rdt = sbt("rdt", [128, 512], F32)
        otl = [sbt("otl%d" % i, [64, 512], BF16) for i in range(2)]
        es8 = sbt("es8", [128, 8], F32)
        est = sbt("est", [64, 2, 512], F32)
        dma("sp", skt, skt[:], FT, FT[B_SK * 128:(B_SK + 1) * 128, :].rearrange("(g p) t -> p g t", p=64), skt)
        for q4 in range(4):
            dma("sp", svv, svv[:, q4 * 8:(q4 + 1) * 8, :], VT,
                VT[q4 * 1024:(q4 + 1) * 1024, V_SWA:V_SWA + 128].rearrange("(kb p) d -> p kb d", p=128), svv)
        o_, _w = SM_OFF["sink"]
        op("act", lambda e: e.activation(out=es8[:], in_=smt[:, o_:o_ + 8], func=AF.Exp), r=(smt,), w=(es8,))
        for hh in range(8):
            op("dve", lambda e: e.tensor_copy(out=est[:, hh // 4, (hh % 4) * 128:(hh % 4 + 1) * 128], in_=es8[0:64, hh:hh + 1].to_broadcast([64, 128])),
               r=(es8,), w=(est,))
        sscale = 64 ** -0.5
        cnt = 0
        for i in range(NT):
            sq_ = sqt[i % 2]
            dma("sp", sq_, sq_[:], FT, FT[B_SQ * 128:(B_SQ + 4) * 128, i * 128:(i + 1) * 128].rearrange("(hh p) t -> p hh t", p=64), sq_)
            for g in range(2):
                psO, psD = PB[4 + (g % 2) * 2], PB[5 + (g % 2) * 2]
                rels = [(1, i)] if i == 0 else [(0, i - 1), (1, i)]
                bg.tick()
                for ri, (rel, kb) in enumerate(rels):
                    psS = PB[cnt % 2]
                    pT = pTt[cnt % 2]
                    cnt += 1
                    mm(psS, psS[:, 0:512], [(skt[0:64, g, kb * 128:(kb + 1) * 128], sq_[0:64, 4 * g:4 * g + 4, :])], r=(skt, sq_))
                    op("act", lambda e: e.activation(out=pT[:], in_=psS[:, 0:512], func=AF.Exp, scale=sscale), r=(psS,), w=(pT,))
                    op("dve", lambda e: e.tensor_tensor(out=pT[:], in0=pT[:], in1=ebs[:, g, rel, :], op=ALU.mult), r=(pT, ebs), w=(pT,))
                    mm(psO, psO[0:64, 0:512], [(svv[:, kb, g * 64:(g + 1) * 64], pT[:])], r=(svv, pT), start=(ri == 0), stop=(ri == len(rels) - 1))
                    mm(psD, psD[0:64, 0:512], [(ones_b[:, 0:64], pT[:])], r=(ones_b, pT), start=(ri == 0), stop=(ri == len(rels) - 1))
                ot = otl[g % 2]
                finalize(psO, psD, 64, ot, rdt, extra_add=est[:, g, :])
                dma("pool", OT, OT[1536 + 256 * g:1536 + 256 * (g + 1), i * 128:(i + 1) * 128].rearrange("(hi d) t -> d hi t", d=64),
                    ot, ot[:].rearrange("p (hi t) -> p hi t", hi=4), ot)
        k.end_phase()
        dsp.recycle()
        if stop == "attn":
            return finish()

        k.begin_phase()
        g1 = sbt("g1", [128, D], F32)
        load_bcast(g1, g1[:], MOD, MOD[L:L + 1, 2 * D:3 * D])
        hts = sbt("hts", [128, 16, 512], BF16)
        ots = sbt("ots", [128, 16, 512], BF16)
        wgt = [sbt("wgt%d" % i, [128, 16, 256], BF16) for i in range(4)]
        wbt = [sbt("wbt%d" % i, [128, 4, 256], BF16) for i in range(4)]
        mT = sbt("mT", [128, 16, 512], BF16)
        sg = [sbt("sg%d" % i, [128, 512], F32) for i in range(2)]
        tm = [sbt("tm%d" % i, [128, 512], F32) for i in range(2)]
        acc = sbt("acc", [128, 512], F32)
        wot = [sbt("wot%d" % i, [128, 16, 256], BF16) for i in range(2)]
        xt4 = [sbt("xt4_%d" % i, [128, D], F32) for i in range(4)]
        cnt = 0
        for st in range(NST):
            tsl = slice(st * 512, (st + 1) * 512)
            for half in range(2):
                dma("sp", hts, hts[:, half * 8:(half + 1) * 8, :], HT, HT[half * 1024:(half + 1) * 1024, tsl].rearrange("(k p) t -> p k t", p=128), hts)
                dma("sp", ots, ots[:, half * 8:(half + 1) * 8, :], OT, OT[half * 1024:(half + 1) * 1024, tsl].rearrange("(k p) t -> p k t", p=128), ots)
            for tt in range(4):
                ti = st * 4 + tt
                dma("sp", xt4[tt], xt4[tt][:], xcur, xcur[ti * 128:(ti + 1) * 128, :], xt4[tt])
            for mg in range(8):
                for i in range(4):
                    for half in range(2):
                        dma("sp", wgt[i], wgt[i][:, half * 8:(half + 1) * 8, :], WG,
                            WG[i * D + half * 1024:i * D + (half + 1) * 1024, mg * 256:(mg + 1) * 256].rearrange("(k p) n -> p k n", p=128), wgt[i])
                    dma("sp", wbt[i], wbt[i][:], WB, WB[i * 512:(i + 1) * 512, mg * 256:(mg + 1) * 256].rearrange("(k p) n -> p k n", p=128), wbt[i])
                for m2 in range(2):
                    m = 2 * mg + m2
                    ms = slice(m2 * 128, (m2 + 1) * 128)
                    for i in range(4):
                        psG, psB = PB[cnt % 2], PB[2 + cnt % 2]
                        sgt, tmt = sg[cnt % 2], tm[cnt % 2]
                        cnt += 1
                        bg.tick()
                        mm(psG, psG[:, 0:512], [(wgt[i][:, kc, ms], hts[:, kc, :]) for kc in range(16)], r=(wgt[i], hts))
                        mm(psB, psB[:, 0:512], [(wbt[i][:, kc, ms], ots[:, 4 * i + kc, :]) for kc in range(4)], r=(wbt[i], ots))
                        op("act", lambda e: e.activation(out=sgt[:], in_=psG[:, 0:512], func=AF.Sigmoid), r=(psG,), w=(sgt,))
                        if i == 0:
                            op("dve", lambda e: e.tensor_tensor(out=acc[:], in0=psB[:, 0:512], in1=sgt[:], op=ALU.mult), r=(psB, sgt), w=(acc,))
                        else:
                            op("dve", lambda e: e.tensor_tensor(out=tmt[:], in0=psB[:, 0:512], in1=sgt[:], op=ALU.mult), r=(psB, sgt), w=(tmt,))
                            if i < 3:
                                op("pool", lambda e: e.tensor_tensor(out=acc[:], in0=acc[:], in1=tmt[:], op=ALU.add), r=(acc, tmt), w=(acc,))
                            else:
                                op("pool", lambda e: e.tensor_tensor(out=mT[:, m, :], in0=acc[:], in1=tmt[:], op=ALU.add), r=(acc, tmt), w=(mT,))
            for n in range(8):
                wo = wot[n % 2]
                ns = slice(n * 256, (n + 1) * 256)
                for half in range(2):
                    dma("sp", wo, wo[:, half * 8:(half + 1) * 8, :], WO, WO[half * 1024:(half + 1) * 1024, ns].rearrange("(k p) n -> p k n", p=128), wo)
                for tt in range(4):
                    bg.tick()
                    pso = PB[4 + tt]
                    mm(pso, pso[:, 0:256], [(mT[:, kc, tt * 128:(tt + 1) * 128], wo[:, kc, :]) for kc in range(16)], r=(mT, wo))
                    op("dve", lambda e: e.tensor_tensor(out=tm[tt % 2][:, 0:256], in0=pso[:, 0:256], in1=g1[:, ns], op=ALU.mult), r=(pso, g1), w=(tm[tt % 2],))
                    op("pool", lambda e: e.tensor_tensor(out=xt4[tt][:, ns], in0=xt4[tt][:, ns], in1=tm[tt % 2][:, 0:256], op=ALU.add), r=(xt4[tt], tm[tt % 2]), w=(xt4[tt],))
            for tt in range(4):
                ti = st * 4 + tt
                dma("pool", xmid, xmid[ti * 128:(ti + 1) * 128, :], xt4[tt], xt4[tt][:], xt4[tt])
        k.end_phase()
        dsp.recycle()

        if stop == "p3":
            return finish()
        k.begin_phase()
        gm = sbt("gm2", [128, D], F32)
        sh = sbt("sh2", [128, D], F32)
        g2 = sbt("g2", [128, D], F32)
        tmpD = sbt("tmpD2", [128, D], F32)
        load_bcast(gm, gm[:], MOD, MOD[L:L + 1, 4 * D:5 * D])
        load_bcast(tmpD, tmpD[:], norm_ffn, norm_ffn[L:L + 1, :])
        op("dve", lambda e: e.scalar_tensor_tensor(out=gm[:], in0=gm[:], scalar=1.0, in1=tmpD[:], op0=ALU.add, op1=ALU.mult), r=(gm, tmpD), w=(gm,))
        load_bcast(sh, sh[:], MOD, MOD[L:L + 1, 3 * D:4 * D])
        load_bcast(g2, g2[:], MOD, MOD[L:L + 1, 5 * D:6 * D])
        xt4 = [sbt("xt4_%d" % i, [128, D], F32) for i in range(4)]
        hb = sbt("hb2", [128, D], BF16)
        h2T = sbt("h2T", [128, 16, 512], BF16)
        at = sbt("at", [128, 22, 512], BF16)
        w1t = [sbt("w1t%d" % i, [128, 16, 256], BF16) for i in range(2)]
        w3t = [sbt("w3t%d" % i, [128, 16, 256], BF16) for i in range(2)]
        w2t = [sbt("w2t%d" % i, [128, 11, 512], BF16) for i in range(2)]
        sg = [sbt("sgf%d" % i, [128, 512], F32) for i in range(2)]
        tm = [sbt("tmf%d" % i, [128, 512], F32) for i in range(2)]
        st8 = sbt("st8f", [128, 16], F32)
        if moe:
            rtf = sbt("rtf", [128, 16, 8], F32)
            rtb = sbt("rtb", [128, 16, 8], BF16)
            lg = sbt("lg", [128, 8], F32)
            m8 = sbt("m8f", [128, 8], F32)
            gt = [sbt("gt%d" % i, [128, 8], F32) for i in range(4)]
            e1 = sbt("e1", [128, 8], F32)
            dma("sp", rtf, rtf[:], moe_router, moe_router[j2 * D:(j2 + 1) * D, :].rearrange("(k p) n -> p k n", p=128), rtf)
            op("dve", lambda e: e.tensor_copy(out=rtb[:], in_=rtf[:]), r=(rtf,), w=(rtb,))
        cnt = 0
        wc = 0
        for st in range(NST):
            for tt in range(4):
                ti = st * 4 + tt
                xtile = xt4[tt]
                dma("sp", xtile, xtile[:], xmid, xmid[ti * 128:(ti + 1) * 128, :], xtile)
                op("act", lambda e: e.activation(out=tmpD[:], in_=xtile[:], func=AF.Square, accum_out=st8[:, 8:9]), r=(xtile,), w=(tmpD, st8))
                rstd_from_ssq(st8[:, 8:9], st8[:, 8:9], D, (st8,))
                op("dve", lambda e: e.scalar_tensor_tensor(out=tmpD[:], in0=xtile[:], scalar=st8[:, 8:9], in1=gm[:], op0=ALU.mult, op1=ALU.mult), r=(xtile, st8, gm), w=(tmpD,))
                op("pool", lambda e: e.tensor_tensor(out=hb[:], in0=tmpD[:], in1=sh[:], op=ALU.add), r=(tmpD, sh), w=(hb,))
                for half in range(2):
                    bank = 6 + half
                    pv = pbf(bank)
                    for i_ in range(8):
                        kc = half * 8 + i_
                        op("pe", lambda e: e.transpose(out=pv[:, i_ * 128:(i_ + 1) * 128], in_=hb[:, kc * 128:(kc + 1) * 128], identity=ident[:]),
                           r=(hb, ident) if i_ == 0 else (), w=(PB[bank],), sig=(i_ == 7))
                    op("act", lambda e: e.copy(out=h2T[:, half * 8:half * 8 + 8, tt * 128:(tt + 1) * 128], in_=pv[:, 0:1024].rearrange("p (n t) -> p n t", n=8)),
                       r=(PB[bank],), w=(h2T,))
                if moe:
                    pz = PB[4]
                    mm(pz, pz[:, 0:8], [(h2T[:, kc, tt * 128:(tt + 1) * 128], rtb[:, kc, :]) for kc in range(16)], r=(h2T, rtb))
                    op("dve", lambda e: e.tensor_copy(out=lg[:], in_=pz[:, 0:8]), r=(pz,), w=(lg,))
                    op("dve", lambda e: e.max(out=m8[:], in_=lg[:]), r=(lg,), w=(m8,))
                    op("dve", lambda e: e.tensor_tensor(out=st8[:, 0:1], in0=m8[:, 0:1], in1=m8[:, 1:2], op=ALU.subtract), r=(m8,), w=(st8,))
                    op("act", lambda e: e.activation(out=st8[:, 1:2], in_=st8[:, 0:1], func=AF.Sigmoid), r=(st8,), w=(st8,))
                    op("dve", lambda e: e.tensor_scalar(out=st8[:, 2:3], in0=st8[:, 1:2], scalar1=-1.0, scalar2=1.0, op0=ALU.mult, op1=ALU.add), r=(st8,), w=(st8,))
                    op("dve", lambda e: e.tensor_scalar(out=e1[:], in0=lg[:], scalar1=m8[:, 0:1], scalar2=st8[:, 1:2], op0=ALU.is_equal, op1=ALU.mult), r=(lg, m8, st8), w=(e1,))
                    op("dve", lambda e: e.tensor_scalar(out=gt[tt][:], in0=lg[:], scalar1=m8[:, 1:2], scalar2=st8[:, 2:3], op0=ALU.is_equal, op1=ALU.mult), r=(lg, m8, st8), w=(gt[tt],))
                    op("dve", lambda e: e.tensor_tensor(out=gt[tt][:], in0=gt[tt][:], in1=e1[:], op=ALU.add), r=(gt[tt], e1), w=(gt[tt],))
            for ex in range(NE):
                for fg in range(11):
                    w1, w3 = w1t[wc % 2], w3t[wc % 2]
                    wc += 1
                    for half in range(2):
                        dma("sp", w1, w1[:, half * 8:(half + 1) * 8, :], FW1,
                            FW1[ex * D + half * 1024:ex * D + (half + 1) * 1024, fg * 256:(fg + 1) * 256].rearrange("(k p) n -> p k n", p=128), w1)
                        dma("sp", w3, w3[:, half * 8:(half + 1) * 8, :], FW3,
                            FW3[ex * D + half * 1024:ex * D + (half + 1) * 1024, fg * 256:(fg + 1) * 256].rearrange("(k p) n -> p k n", p=128), w3)
                    for fc in range(2):
                        f = 2 * fg + fc
                        fs = slice(fc * 128, (fc + 1) * 128)
                        psG, psU = PB[cnt % 2], PB[2 + cnt % 2]
                        sgt = sg[cnt % 2]
                        cnt += 1
                        bg.tick()
                        mm(psG, psG[:, 0:512], [(w1[:, kc, fs], h2T[:, kc, :]) for kc in range(16)], r=(w1, h2T))
                        mm(psU, psU[:, 0:512], [(w3[:, kc, fs], h2T[:, kc, :]) for kc in range(16)], r=(w3, h2T))
                        op("act", lambda e: e.activation(out=sgt[:], in_=psG[:, 0:512], func=AF.Silu), r=(psG,), w=(sgt,))
                        op("dve", lambda e: e.tensor_tensor(out=at[:, f, :], in0=psU[:, 0:512], in1=sgt[:], op=ALU.mult), r=(psU, sgt), w=(at,))
                for n in range(4):
                    ns = slice(n * 512, (n + 1) * 512)
                    for f2 in range(2):
                        w2 = w2t[wc % 2]
                        wc += 1
                        bg.tick()
                        dma("sp", w2, w2[:], FW2, FW2[ex * 2816 + f2 * 1408:ex * 2816 + (f2 + 1) * 1408, ns].rearrange("(f p) n -> p f n", p=128), w2)
                        for tt in range(4):
                            psY = PB[4 + tt]
                            mm(psY, psY[:, 0:512], [(at[:, f2 * 11 + fl, tt * 128:(tt + 1) * 128], w2[:, fl, :]) for fl in range(11)], r=(at, w2),
                               start=(f2 == 0), stop=(f2 == 1))
                    for tt in range(4):
                        psY = PB[4 + tt]
                        tmt = tm[tt % 2]
                        if moe:
                            op("dve", lambda e: e.scalar_tensor_tensor(out=tmt[:], in0=psY[:, 0:512], scalar=gt[tt][:, ex:ex + 1], in1=g2[:, ns], op0=ALU.mult, op1=ALU.mult),
                               r=(psY, gt[tt], g2), w=(tmt,))
                        else:
                            op("dve", lambda e: e.tensor_tensor(out=tmt[:], in0=psY[:, 0:512], in1=g2[:, ns], op=ALU.mult), r=(psY, g2), w=(tmt,))
                        op("pool", lambda e: e.tensor_tensor(out=xt4[tt][:, ns], in0=xt4[tt][:, ns], in1=tmt[:], op=ALU.add), r=(xt4[tt], tmt), w=(xt4[tt],))
            for tt in range(4):
                ti = st * 4 + tt
                dma("pool", xnext, xnext[ti * 128:(ti + 1) * 128, :], xt4[tt], xt4[tt][:], xt4[tt])
        k.end_phase()
        dsp.recycle()
        xcur = xnext

    return finish()


def _rel_bucket_np(n):
    n = np.maximum(n, 0)
    nf = np.maximum(n, 1).astype(np.float32)
    large = 16 + (np.log(nf / 16) / math.log(128 / 16) * 16).astype(np.int32)
    large = np.minimum(large, 31)
    return np.where(n < 16, n, large)


def _constants():
    c = {}
    c["ident"] = np.eye(128, dtype=np.float32).astype(NPBF)
    half = 32
    freqs = (10000.0 ** (-np.arange(half, dtype=np.float32) / half)).astype(np.float32)
    ang = np.arange(S, dtype=np.float32)[:, None] * freqs[None, :]
    c["cs_tab"] = np.concatenate([np.cos(ang), np.sin(ang)], axis=1).astype(np.float32)
    s_ = np.arange(128)[:, None]
    t_ = np.arange(512)[None, :]
    c["negcm"] = np.concatenate([np.where(r * 128 + s_ <= t_, 0.0, NEG) for r in range(4)], axis=1).astype(np.float32)
    qq = np.arange(128)[:, None]
    kk = np.arange(128)[None, :]
    c["negtri"] = np.where(kk <= qq, 0.0, -1e30).astype(np.float32)
    c["tri"] = (np.arange(128)[:, None] <= np.arange(128)[None, :]).astype(np.float32)
    dist = np.arange(384) - 127
    bk = _rel_bucket_np(dist)
    oh = np.zeros((32, 384), np.float32)
    for j in range(384):
        if dist[j] >= 0:
            oh[bk[j], j] = 1.0
    c["ohs"] = oh.copy()
    ohd = oh.copy()
    ohd[31, dist >= 0] -= 1.0
    c["ohd"] = ohd
    tt = np.arange(256)[None, :]
    dd = tt - s_
    c["negw"] = np.where((dd >= 0) & (dd < 128), 0.0, NEG).astype(np.float32)
    return c


def _prep_inputs(inp, nl=DEPTH, cores=8):
    f = lambda a: np.ascontiguousarray(np.asarray(a, dtype=np.float32))
    nf_, nm_ = (nl + 1) // 2, nl // 2
    w_in = f(inp["w_in"][:nl])
    offs = np.cumsum([0, 512, 256, 64, 512, 512, 512, 4, 512, 128, 128, 512, 64, 8, 512, 128, 128])
    (cq, ckv, kr, fq, fk, fv, fg, dq, dk, dv, iq, ik, iw, sq, sk, sv) = [slice(offs[i], offs[i + 1]) for i in range(16)]
    wp = np.zeros((nl, D, 5120), np.float32)

    def put(c, o, sl):
        wp[:, :, c * 512 + o:c * 512 + o + (sl.stop - sl.start)] = w_in[:, :, sl]
    put(0, 0, cq); put(1, 0, ckv); put(1, 256, kr); put(2, 0, fq); put(3, 0, fk); put(4, 0, fv); put(5, 0, dq)
    put(6, 0, dk); put(6, 128, dv); put(6, 256, ik); put(6, 320, iw); put(6, 328, fg)
    put(7, 0, iq); put(8, 0, sq); put(9, 0, sk); put(9, 128, sv)
    uq = f(inp["mla_w_uq"][:nl]).reshape(nl, 512, 4, 192)
    uqp = np.concatenate([uq[..., :128].reshape(nl, 512, 512), uq[..., 128:].reshape(nl, 512, 256)], axis=-1)
    ukv = f(inp["mla_w_ukv"][:nl]).reshape(nl, 256, 4, 256)
    ukvp = np.concatenate([ukv[..., :128].reshape(nl, 256, 512), ukv[..., 128:].reshape(nl, 256, 512)], axis=-1)
    small = np.concatenate([f(inp[n][:nl]) for n in ("mla_cq_norm", "mla_ckv_norm", "mla_q_norm", "mla_k_norm", "fox_q_norm", "fox_k_norm",
                                                      "fox_f_bias", "dsa_q_norm", "dsa_k_norm", "swa_q_norm", "swa_k_norm", "swa_sinks")], axis=1)
    assert small.shape == (nl, NSM)
    shared = {
        "ada_w": f(inp["ada_w"][:nl]).reshape(nl * D, 6 * D), "ada_b": f(inp["ada_b"][:nl]),
        "norm_mix": f(inp["norm_mix"][:nl]), "norm_ffn": f(inp["norm_ffn"][:nl]), "small": np.ascontiguousarray(small),
        "w_in_p": wp.reshape(nl * D, 5120), "w_uq_p": np.ascontiguousarray(uqp).reshape(nl * 512, 768),
        "w_ukv_p": np.ascontiguousarray(ukvp).reshape(nl * 256, 1024), "rel_bias": f(inp["rel_bias"]),
        "w_branch": f(inp["w_branch"][:nl]).reshape(nl * 4 * 512, D), "w_gate": f(inp["w_gate"][:nl]).reshape(nl * 4 * D, D),
        "w_out": f(inp["w_out"][:nl]).reshape(nl * D, D),
        "ffn_w1": f(inp["ffn_w1"][:nf_]).reshape(nf_ * D, 5632), "ffn_w3": f(inp["ffn_w3"][:nf_]).reshape(nf_ * D, 5632),
        "ffn_w2": f(inp["ffn_w2"][:nf_]).reshape(nf_ * 5632, D),
    }
    if nm_:
        shared.update({
            "moe_router": f(inp["moe_router"][:nm_]).reshape(nm_ * D, 8),
            "moe_w1": f(inp["moe_w1"][:nm_]).reshape(nm_ * 8 * D, 2816), "moe_w3": f(inp["moe_w3"][:nm_]).reshape(nm_ * 8 * D, 2816),
            "moe_w2": f(inp["moe_w2"][:nm_]).reshape(nm_ * 8 * 2816, D),
        })
    shared.update(_constants())
    maps = []
    for b in range(cores):
        m = dict(shared)
        m["x"] = f(inp["x"][b])
        m["cT"] = np.ascontiguousarray(f(inp["c"][b]).reshape(16, 128).T)
        maps.append(m)
    return maps


def kernel(**inputs):
    maps = _prep_inputs(inputs)
    nc = build_program()
    res = run_bass_kernel_spmd(nc, maps, core_ids=list(range(8)))
    return np.stack([np.asarray(r["y"], dtype=np.float32) for r in res.results], axis=0)
```

```python
import contextlib
import math
import numpy as np
import ml_dtypes
import concourse.bass as bass
import concourse.mybir as mybir
from concourse.bass_utils import run_bass_kernel_spmd

F32 = mybir.dt.float32
BF16 = mybir.dt.bfloat16
AF = mybir.ActivationFunctionType
ALU = mybir.AluOpType
AX = mybir.AxisListType
NPBF = ml_dtypes.bfloat16

S = 4096
D = 2048
NT = 32
NST = 8
DEPTH = 4
EPS = 1e-6
NEG = -30000.0

SM_OFF = {}
_o = 0
for _n, _w in (("cq", 512), ("ckv", 256), ("mq", 192), ("mk", 192), ("fq", 128), ("fk", 128),
               ("fb", 4), ("dq", 128), ("dk", 128), ("sq", 64), ("sk", 64), ("sink", 8)):
    SM_OFF[_n] = (_o, _w)
    _o += _w
NSM = _o

B_QN, B_QR, B_KN, B_KR, B_FQ, B_FK, B_DQ, B_DK, B_IK, B_IQ, B_SQ, B_SK = 0, 4, 6, 10, 12, 16, 20, 24, 25, 26, 30, 34
NFB = 35
V_MLA, V_FOX, V_DSA, V_SWA, NVT = 0, 512, 1024, 1152, 1280


class Sem:
    def __init__(self, h, name):
        self.h = h
        self.name = name


class Buf:
    def __init__(self, t, name):
        self.t = t
        self.name = name
        self.w = {}
        self.r = {}
        self.ds = None
        self.persist = False

    def __getitem__(self, k):
        return self.t[k]


class Eng:
    def __init__(self, name, e, sem):
        self.name = name
        self.e = e
        self.sem = sem
        self.cnt = 0
        self.seen = {}

    def waitd(self, d):
        for sem, val in d.items():
            if sem is self.sem and val > self.cnt:
                continue
            if self.seen.get(sem, 0) >= val:
                continue
            self.e.wait_ge(sem.h, val)
            self.seen[sem] = val


class K:
    def __init__(self):
        self.nc = bass.Bass("TRN2", target_bir_lowering=False)
        self.es = contextlib.ExitStack()
        self.sems = []
        self.bufs = []
        nc = self.nc
        self.E = {}
        for n, e in (("pe", nc.tensor), ("act", nc.scalar), ("dve", nc.vector),
                     ("pool", nc.gpsimd), ("sp", nc.sync)):
            self.E[n] = Eng(n, e, self.sem("prog_" + n))
        self.phase = None
        self.uid = 0
        self.dsp = DsPool(self)

    def sem(self, name):
        s = Sem(self.es.enter_context(self.nc.semaphore(name)), name)
        self.sems.append(s)
        return s

    def begin_phase(self):
        self.phase = contextlib.ExitStack()

    def end_phase(self):
        self.barrier()
        self.phase.close()
        self.phase = None

    def _reg(self, t, name):
        b = Buf(t, name)
        self.bufs.append(b)
        return b

    def sb(self, name, shape, dt, persist=False):
        st = self.es if (persist or self.phase is None) else self.phase
        self.uid += 1
        t = st.enter_context(self.nc.sbuf_tensor("%s_%d" % (name, self.uid), list(shape), dt))
        b = self._reg(t, name)
        b.persist = persist or self.phase is None
        return b

    def ps(self, name, shape, dt):
        t = self.es.enter_context(self.nc.psum_tensor(name, list(shape), dt))
        return self._reg(t, name)

    def dram(self, name, shape, dt, kind="Internal"):
        t = self.nc.dram_tensor(name, list(shape), dt, kind=kind)
        return self._reg(t.ap(), name)

    def op(self, eng, fn, r=(), w=(), sig=True):
        E = self.E[eng]
        for b in r:
            E.waitd(b.w)
        for b in w:
            E.waitd(b.w)
            E.waitd(b.r)
        ins = fn(E.e)
        if sig:
            E.cnt += 1
            ins.then_inc(E.sem.h, 1)
            val = E.cnt
        else:
            val = E.cnt + 1
        for b in r:
            b.r[E.sem] = max(b.r.get(E.sem, 0), val)
        for b in w:
            b.w[E.sem] = max(b.w.get(E.sem, 0), val)
        return ins

    def mm(self, out_b, out_ap, pairs, r, start=True, stop=True):
        n = len(pairs)
        for i, (l, rh) in enumerate(pairs):
            self.op("pe", lambda e, l=l, rh=rh, i=i: e.matmul(
                out_ap, lhsT=l, rhs=rh, start=(start and i == 0), stop=(stop and i == n - 1)),
                r=r if i == 0 else (), w=(out_b,), sig=(i == n - 1))

    def dma(self, q, out_b, out_ap, in_b, in_ap, side):
        E = self.E[q]
        E.waitd(in_b.w)
        E.waitd(out_b.w)
        E.waitd(out_b.r)
        if side.ds is None:
            side.ds = self.dsp.get(side.persist)
        ins = E.e.dma_start(out=out_ap, in_=in_ap)
        side.ds[1] += 16
        ins.then_inc(side.ds[0].h, 16)
        s, v = side.ds
        in_b.r[s] = max(in_b.r.get(s, 0), v)
        out_b.w[s] = max(out_b.w.get(s, 0), v)
        return ins

    def barrier(self):
        allb = {E.sem: E.cnt for E in self.E.values()}
        for b in self.bufs:
            if b.ds is not None:
                allb[b.ds[0]] = max(allb.get(b.ds[0], 0), b.ds[1])
        for E in self.E.values():
            E.waitd(allb)


class DsPool:
    def __init__(self, k):
        self.k = k
        self.free = []
        self.used = []

    def get(self, persist=False):
        if persist:
            return [self.k.sem("dmap%d" % len(self.k.sems)), 0]
        c = self.free.pop() if self.free else [self.k.sem("dmas%d" % len(self.k.sems)), 0]
        self.used.append(c)
        return c

    def recycle(self):
        self.free.extend(self.used)
        self.used = []


def _bcast_row(buf, row_ap):
    return row_ap.partition_broadcast(128)


def build_program(nlayers=DEPTH, debug=False, stop=None):
    k = K()
    nc = k.nc
    dsp = k.dsp

    def sbt(name, shape, dt, persist=False):
        return k.sb(name, shape, dt, persist)

    op, mm, dma = k.op, k.mm, k.dma

    def din(name, shape, dt=F32):
        return k.dram(name, shape, dt, "ExternalInput")

    x_in = din("x", [S, D])
    cT_in = din("cT", [128, 16])
    ada_w = din("ada_w", [nlayers * D, 6 * D])
    ada_b = din("ada_b", [nlayers, 6 * D])
    norm_mix = din("norm_mix", [nlayers, D])
    norm_ffn = din("norm_ffn", [nlayers, D])
    small = din("small", [nlayers, NSM])
    w_in = din("w_in_p", [nlayers * D, 5120])
    w_uq = din("w_uq_p", [nlayers * 512, 768])
    w_ukv = din("w_ukv_p", [nlayers * 256, 1024])
    rel_bias = din("rel_bias", [32, 12])
    w_branch = din("w_branch", [nlayers * 4 * 512, D])
    w_gate = din("w_gate", [nlayers * 4 * D, D])
    w_out = din("w_out", [nlayers * D, D])
    nf_ = (nlayers + 1) // 2
    nm_ = nlayers // 2
    ffn_w1 = din("ffn_w1", [nf_ * D, 5632])
    ffn_w3 = din("ffn_w3", [nf_ * D, 5632])
    ffn_w2 = din("ffn_w2", [nf_ * 5632, D])
    moe_router = din("moe_router", [nm_ * D, 8]) if nm_ else None
    moe_w1 = din("moe_w1", [nm_ * 8 * D, 2816]) if nm_ else None
    moe_w3 = din("moe_w3", [nm_ * 8 * D, 2816]) if nm_ else None
    moe_w2 = din("moe_w2", [nm_ * 8 * 2816, D]) if nm_ else None
    ident_in = din("ident", [128, 128], BF16)
    cs_in = din("cs_tab", [S, 64])
    negcm_in = din("negcm", [128, 4 * 512])
    negtri_in = din("negtri", [128, 128])
    ohd_in = din("ohd", [32, 384])
    ohs_in = din("ohs", [32, 384])
    negw_in = din("negw", [128, 256])
    tri_in = din("tri", [128, 128])
    y_out = k.dram("y", [S, D], F32, "ExternalOutput")

    XA = k.dram("XA", [S, D], F32)
    XB = k.dram("XB", [S, D], F32)
    HT = k.dram("HT", [16 * 128, S], BF16)
    FT = k.dram("FT", [NFB * 128, S], BF16)
    VT = k.dram("VT", [S, NVT], BF16)
    OT = k.dram("OT", [D, S], BF16)
    CUML = k.dram("CUML", [S, 4], F32)
    CUMT = k.dram("CUMT", [4, S], F32)
    IW = k.dram("IW", [S, 8], F32)
    MOD = k.dram("MOD", [nlayers, 6 * D], F32)
    TD = k.dram("TD", [12 * 128, 384], F32)
    MTD = k.dram("MTD", [NST * 128, 32 * 512], BF16)
    WSETS = []
    for si in range(2):
        WSETS.append(dict(
            WIN=k.dram("WIN%d" % si, [D, 5120], BF16), WUQ=k.dram("WUQ%d" % si, [512, 768], BF16),
            WUKV=k.dram("WUKV%d" % si, [256, 1024], BF16), WG=k.dram("WG%d" % si, [4 * D, D], BF16),
            WB=k.dram("WB%d" % si, [4 * 512, D], BF16), WO=k.dram("WO%d" % si, [D, D], BF16),
            FW1=k.dram("FW1_%d" % si, [8 * D, 2816], BF16), FW3=k.dram("FW3_%d" % si, [8 * D, 2816], BF16),
            FW2=k.dram("FW2_%d" % si, [8 * 2816, D], BF16)))
    dbg = {}
    if debug:
        for nm, shp, dt in (("dbg_ft", [NFB * 128, S], BF16), ("dbg_vt", [S, NVT], BF16),
                            ("dbg_ot", [D, S], BF16), ("dbg_xa", [S, D], F32),
                            ("dbg_mod", [nlayers, 2048], F32), ("dbg_cum", [S, 4], F32)):
            dbg[nm] = k.dram(nm, shp, dt, "ExternalOutput")

    PB = [k.ps("pb%d" % i, [128, 512], F32) for i in range(8)]

    def pbf(i):
        return PB[i][:].bitcast(BF16)

    ident = k.sb("ident", [128, 128], BF16, True)
    ones_b = k.sb("ones_b", [128, 128], BF16, True)
    ones_f = k.sb("ones_f", [128, 512], F32, True)
    negcm = k.sb("negcm", [128, 4, 512], F32, True)
    negtri = k.sb("negtri", [128, 128], F32, True)
    tri = k.sb("tri", [128, 128], F32, True)
    ew2 = k.sb("ew2", [128, 4, 256], BF16, True)
    ebs = k.sb("ebs", [128, 2, 2, 512], BF16, True)
    smt = k.sb("smt", [128, NSM], F32, True)
    neg29 = k.sb("neg29", [128, 1], F32, True)

    dma("sp", ident, ident[:], ident_in, ident_in[:, :], ident)
    dma("sp", negcm, negcm[:].rearrange("p a b -> p (a b)"), negcm_in, negcm_in[:, :], negcm)
    dma("sp", negtri, negtri[:], negtri_in, negtri_in[:, :], negtri)
    dma("sp", tri, tri[:], tri_in, tri_in[:, :], tri)
    op("dve", lambda e: e.memset(ones_b[:], 1.0), w=(ones_b,))
    op("dve", lambda e: e.memset(ones_f[:], 1.0), w=(ones_f,))
    op("dve", lambda e: e.memset(neg29[:], -1e29), w=(neg29,))
    pw2 = k.sb("pw2", [128, 32], F32, True)
    for kk_ in range(32):
        op("pool", lambda e, kk_=kk_: e.memset(pw2[:, kk_:kk_ + 1], float(2.0 ** (-kk_))), w=(pw2,))

    rr = [0]

    def cast_eng():
        rr[0] += 1
        return ("act", "dve", "pool")[rr[0] % 3]

    def copy_op(eng, out_ap, in_ap, r, w):
        if eng == "act":
            op("act", lambda e: e.copy(out=out_ap, in_=in_ap), r=r, w=w)
        else:
            op(eng, lambda e: e.tensor_copy(out=out_ap, in_=in_ap), r=r, w=w)

    k.begin_phase()
    ct = sbt("ct", [128, 16], F32)
    sc = sbt("sc", [128, 16], F32)
    awt = [sbt("awt%d" % i, [128, 16, 512], F32) for i in range(2)]
    abt = [sbt("abt%d" % i, [1, 512], F32) for i in range(2)]
    mrow = [sbt("mrow%d" % i, [1, 512], F32) for i in range(2)]
    dma("sp", ct, ct[:], cT_in, cT_in[:, :], ct)
    op("act", lambda e: e.activation(out=sc[:], in_=ct[:], func=AF.Silu), r=(ct,), w=(sc,))
    it = 0
    for L in range(nlayers):
        for n in range(24):
            a, bt, mr = awt[it % 2], abt[it % 2], mrow[it % 2]
            src = ada_w[L * D:(L + 1) * D, n * 512:(n + 1) * 512].rearrange("(k p) n -> p k n", p=128)
            dma("sp", a, a[:], ada_w, src, a)
            dma("sp", bt, bt[:], ada_b, ada_b[L:L + 1, n * 512:(n + 1) * 512], bt)
            pz = PB[it % 2]
            mm(pz, pz[0:1, :], [(sc[:, kc:kc + 1], a[:, kc, :]) for kc in range(16)], r=(sc, a))
            op("dve", lambda e, mr=mr, pz=pz, bt=bt: e.tensor_tensor(out=mr[:], in0=pz[0:1, :], in1=bt[:], op=ALU.add),
               r=(pz, bt), w=(mr,))
            dma("pool", MOD, MOD[L:L + 1, n * 512:(n + 1) * 512], mr, mr[:], mr)
            it += 1
    k.end_phase()
    dsp.recycle()

    k.begin_phase()
    relt = sbt("relt", [32, 12], F32)
    oht = [sbt("ohd_t", [32, 384], F32), sbt("ohs_t", [32, 384], F32)]
    negw = sbt("negw", [128, 256], F32)
    tbc = [sbt("tbc%d" % i, [32, 128], F32) for i in range(2)]
    tdt = [sbt("tdt%d" % i, [128, 384], F32) for i in range(2)]
    w2t = [sbt("w2t%d" % i, [128, 256], F32) for i in range(2)]
    dma("sp", relt, relt[:], rel_bias, rel_bias[:, :], relt)
    dma("sp", oht[0], oht[0][:], ohd_in, ohd_in[:, :], oht[0])
    dma("sp", oht[1], oht[1][:], ohs_in, ohs_in[:, :], oht[1])
    dma("sp", negw, negw[:], negw_in, negw_in[:, :], negw)
    for h in range(12):
        tb, td, w2 = tbc[h % 2], tdt[h % 2], w2t[h % 2]
        oh = oht[0] if h < 4 else oht[1]
        op("dve", lambda e, tb=tb, h=h: e.tensor_copy(out=tb[:], in_=relt[:, h:h + 1].to_broadcast([32, 128])),
           r=(relt,), w=(tb,))
        pz = PB[h % 2]
        mm(pz, pz[:, 0:384], [(tb[:], oh[:])], r=(tb, oh))
        op("act", lambda e, td=td, pz=pz: e.copy(out=td[:], in_=pz[:, 0:384]), r=(pz,), w=(td,))
        dma("pool", TD, TD[h * 128:(h + 1) * 128, :], td, td[:], td)
        skew = bass.AP(TD.t.tensor, h * 128 * 384 + 127, [[383, 128], [1, 256]])
        dma("sp", w2, w2[:], TD, skew, w2)
        if h < 4:
            op("act", lambda e, w2=w2, h=h: e.activation(out=ew2[:, h, :], in_=w2[:], func=AF.Exp), r=(w2,), w=(ew2,))
        else:
            hh = h - 4
            g, hi = hh // 4, hh % 4
            op("dve", lambda e, w2=w2: e.tensor_tensor(out=w2[:], in0=w2[:], in1=negw[:], op=ALU.add), r=(w2, negw), w=(w2,))
            for rel in range(2):
                cs = slice(128, 256) if rel == 0 else slice(0, 128)
                op("act", lambda e, w2=w2, g=g, rel=rel, hi=hi, cs=cs: e.activation(
                    out=ebs[:, g, rel, hi * 128:(hi + 1) * 128], in_=w2[:, cs], func=AF.Exp), r=(w2,), w=(ebs,))
    k.end_phase()
    dsp.recycle()

    BGW = 1024
    bgf = [k.sb("bgf%d" % i, [128, BGW], F32, True) for i in range(2)]
    bgb = [k.sb("bgb%d" % i, [128, BGW], BF16, True) for i in range(2)]

    def precast_items(L, BGW=BGW):
        W = WSETS[L % 2]
        j2 = L // 2
        items = []

        def add(src_b, src_ap, dst_b, dst_ap, R, C):
            ncb = -(-C // BGW)
            while C % ncb:
                ncb += 1
            cb = C // ncb
            nr = max(1, BGW // cb)
            nblk = R // 128
            for c in range(ncb):
                for b0 in range(0, nblk, nr):
                    n = min(nr, nblk - b0)
                    sv = src_ap[b0 * 128:(b0 + n) * 128, c * cb:(c + 1) * cb].rearrange("(n p) c -> p n c", p=128)
                    dv = dst_ap[b0 * 128:(b0 + n) * 128, c * cb:(c + 1) * cb].rearrange("(n p) c -> p n c", p=128)
                    items.append((src_b, sv, dst_b, dv, n, cb))
        add(w_in, w_in[L * D:(L + 1) * D, :], W["WIN"], W["WIN"][:, :], D, 5120)
        add(w_uq, w_uq[L * 512:(L + 1) * 512, :], W["WUQ"], W["WUQ"][:, :], 512, 768)
        add(w_ukv, w_ukv[L * 256:(L + 1) * 256, :], W["WUKV"], W["WUKV"][:, :], 256, 1024)
        add(w_gate, w_gate[L * 4 * D:(L + 1) * 4 * D, :], W["WG"], W["WG"][:, :], 4 * D, D)
        add(w_branch, w_branch[L * 2048:(L + 1) * 2048, :], W["WB"], W["WB"][:, :], 2048, D)
        add(w_out, w_out[L * D:(L + 1) * D, :], W["WO"], W["WO"][:, :], D, D)
        if L % 2 == 1:
            add(moe_w1, moe_w1[j2 * 8 * D:(j2 + 1) * 8 * D, :], W["FW1"], W["FW1"][:, :], 8 * D, 2816)
            add(moe_w3, moe_w3[j2 * 8 * D:(j2 + 1) * 8 * D, :], W["FW3"], W["FW3"][:, :], 8 * D, 2816)
            add(moe_w2, moe_w2[j2 * 8 * 2816:(j2 + 1) * 8 * 2816, :], W["FW2"], W["FW2"][:, :], 8 * 2816, D)
        else:
            for e_ in range(2):
                add(ffn_w1, ffn_w1[j2 * D:(j2 + 1) * D, e_ * 2816:(e_ + 1) * 2816], W["FW1"], W["FW1"][e_ * D:(e_ + 1) * D, :], D, 2816)
                add(ffn_w3, ffn_w3[j2 * D:(j2 + 1) * D, e_ * 2816:(e_ + 1) * 2816], W["FW3"], W["FW3"][e_ * D:(e_ + 1) * D, :], D, 2816)
            add(ffn_w2, ffn_w2[j2 * 5632:(j2 + 1) * 5632, :], W["FW2"], W["FW2"][0:5632, :], 5632, D)
        return items

    class Bg:
        def __init__(self):
            self.gen = None
            self.rate = 0.0
            self.credit = 0.0

        def start(self, L, hooks, fg=False):
            if fg:
                self.tf = [sbt("pcf%d" % i, [128, 4096], F32) for i in range(2)]
                self.tb = [sbt("pcb%d" % i, [128, 4096], BF16) for i in range(2)]
                items = precast_items(L, 4096)
            else:
                self.tf, self.tb = bgf, bgb
                items = precast_items(L)
            self.gen = self._run(items, fg)
            self.rate = len(items) / float(hooks)
            self.credit = 0.0

        def _run(self, items, fg=False):
            q = "sp" if fg else "pool"

            def load(t):
                src_b, sv, dst_b, dv, n, cb = items[t]
                ft = self.tf[t % 2]
                dma(q, ft, ft[:, 0:n * cb].rearrange("p (n c) -> p n c", n=n), src_b, sv, ft)
            load(0)
            for t in range(len(items)):
                src_b, sv, dst_b, dv, n, cb = items[t]
                ft, bt = self.tf[t % 2], self.tb[t % 2]
                if t + 1 < len(items):
                    load(t + 1)
                copy_op(("act", "dve", "pool")[t % 3] if fg else "pool", bt[:, 0:n * cb], ft[:, 0:n * cb], (ft,), (bt,))
                dma("pool", dst_b, dv, bt, bt[:, 0:n * cb].rearrange("p (n c) -> p n c", n=n), bt)
                yield

        def tick(self, w=1.0):
            if self.gen is None:
                return
            self.credit += self.rate * w
            while self.credit >= 1.0 and self.gen is not None:
                self.credit -= 1.0
                self._step()

        def _step(self):
            try:
                next(self.gen)
            except StopIteration:
                self.gen = None

        def finish(self):
            while self.gen is not None:
                self._step()

    bg = Bg()

    def rstd_from_ssq(ssq_ap, out_ap, d, bufs):
        op("act", lambda e: e.activation(out=out_ap, in_=ssq_ap, func=AF.Sqrt, scale=1.0 / d, bias=EPS), r=bufs, w=bufs)
        op("dve", lambda e: e.reciprocal(out=out_ap, in_=out_ap), r=bufs, w=bufs)

    def load_bcast(tile_b, tile_ap, src_b, row_ap):
        dma("sp", tile_b, tile_ap, src_b, row_ap.partition_broadcast(128), tile_b)

    def finish():
        if debug:
            k.begin_phase()
            cp = [sbt("cp%d" % i, [128, 4096], BF16) for i in range(2)]
            cpf = [sbt("cpf%d" % i, [128, 2048], F32) for i in range(2)]
            n = 0
            for src, dst, rows in ((FT, dbg["dbg_ft"], NFB * 128), (OT, dbg["dbg_ot"], D)):
                for r0 in range(0, rows, 128):
                    t = cp[n % 2]
                    n += 1
                    dma("sp", t, t[:], src, src[r0:r0 + 128, :], t)
                    dma("pool", dst, dst[r0:r0 + 128, :], t, t[:], t)
            for r0 in range(0, S, 128):
                t = cp[n % 2]
                n += 1
                dma("sp", t, t[:, 0:NVT], VT, VT[r0:r0 + 128, :], t)
                dma("pool", dbg["dbg_vt"], dbg["dbg_vt"][r0:r0 + 128, :], t, t[:, 0:NVT], t)
                t = cpf[n % 2]
                dma("sp", t, t[:], XA, XA[r0:r0 + 128, :], t)
                dma("pool", dbg["dbg_xa"], dbg["dbg_xa"][r0:r0 + 128, :], t, t[:], t)
                t = cpf[(n + 1) % 2]
                dma("sp", t, t[:, 0:4], CUML, CUML[r0:r0 + 128, :], t)
                dma("pool", dbg["dbg_cum"], dbg["dbg_cum"][r0:r0 + 128, :], t, t[:, 0:4], t)
            t = cpf[0]
            dma("sp", t, t[0:nlayers, :], MOD, MOD[:, 0:2048], t)
            dma("pool", dbg["dbg_mod"], dbg["dbg_mod"][:, :], t, t[0:nlayers, :], t)
            k.end_phase()
        k.barrier()
        k.es.close()
        return nc

    xcur = x_in
    for L in range(nlayers):
        moe = (L % 2 == 1)
        j2 = L // 2
        NE = 8 if moe else 2
        last = (L == nlayers - 1)
        xmid = XA
        xnext = y_out if last else XB

        if L == 0:
            k.begin_phase()
            bg.start(0, 1, fg=True)
            bg.finish()
            k.end_phase()
            dsp.recycle()
        bg.finish()
        k.barrier()
        WS = WSETS[L % 2]
        WIN, WUQ, WUKV, WG, WB, WO, FW1, FW3, FW2 = (WS[n_] for n_ in ("WIN", "WUQ", "WUKV", "WG", "WB", "WO", "FW1", "FW3", "FW2"))
        if not last:
            bg.start(L + 1, 3900 if moe else 2600)

        k.begin_phase()
        load_bcast(smt, smt[:], small, small[L:L + 1, :])
        gm = sbt("gm", [128, D], F32)
        sh = sbt("sh", [128, D], F32)
        tmpD = sbt("tmpD", [128, D], F32)
        load_bcast(gm, gm[:], MOD, MOD[L:L + 1, D:2 * D])
        load_bcast(tmpD, tmpD[:], norm_mix, norm_mix[L:L + 1, :])
        op("dve", lambda e: e.scalar_tensor_tensor(out=gm[:], in0=gm[:], scalar=1.0, in1=tmpD[:], op0=ALU.add, op1=ALU.mult),
           r=(gm, tmpD), w=(gm,))
        load_bcast(sh, sh[:], MOD, MOD[L:L + 1, 0:D])
        xt = [sbt("xt%d" % i, [128, D], F32) for i in range(2)]
        hb = sbt("hb", [128, D], BF16)
        hts = sbt("hts", [128, 16, 512], BF16)
        wint = [sbt("wint%d" % i, [128, 16, 512], BF16) for i in range(2)]
        wuqt = sbt("wuqt", [128, 4, 768], BF16)
        wukvt = sbt("wukvt", [128, 2, 1024], BF16)
        fts = sbt("fts", [128, NFB, 512], BF16)
        vts = [sbt("vts%d" % i, [128, NVT], BF16) for i in range(4)]
        zc = sbt("zc", [128, 1024], F32)
        sq = sbt("sq", [128, 1024], F32)
        nb = sbt("nb", [128, 1024], BF16)
        nT = sbt("nT", [128, 4, 128], BF16)
        st8 = sbt("st8", [128, 16], F32)
        rot = sbt("rot", [128, 4, 64], F32)
        rt = sbt("rt", [128, 4, 4, 32], F32)
        cst = [sbt("cst%d" % i, [128, 64], F32) for i in range(4)]
        iwt = sbt("iwt", [128, 8], F32)
        lft = sbt("lft", [128, 4], F32)
        cumt = sbt("cumt", [128, 4], F32)
        runt = sbt("runt", [128, 4], F32)
        cumTt = sbt("cumTt", [4, 128], F32)
        identf = sbt("identf", [128, 128], F32)
        dma("sp", wuqt, wuqt[:], WUQ, WUQ[:, :].rearrange("(k p) n -> p k n", p=128), wuqt)
        dma("sp", wukvt, wukvt[:], WUKV, WUKV[:, :].rearrange("(k p) n -> p k n", p=128), wukvt)
        op("dve", lambda e: e.memset(runt[:], 0.0), w=(runt,))
        op("dve", lambda e: e.tensor_copy(out=identf[:], in_=ident[:]), r=(ident,), w=(identf,))
        op("dve", lambda e: e.memset(fts[:, B_IK, :], 0.0), w=(fts,))

        def sm(name):
            o, w_ = SM_OFF[name]
            return smt[:, o:o + w_]

        def hnorm(src3, H, d, gain_ap, out3, extra=None):
            sq3 = sq[:, 0:H * d].rearrange("p (h d) -> p h d", h=H)
            op("dve", lambda e: e.tensor_tensor(out=sq3, in0=src3, in1=src3, op=ALU.mult), r=(zc,), w=(sq,))
            op("dve", lambda e: e.tensor_reduce(out=st8[:, 0:H], in_=sq3, axis=AX.X, op=ALU.add), r=(sq,), w=(st8,))
            rstd_from_ssq(st8[:, 0:H], st8[:, 0:H], d, (st8,))
            op("dve", lambda e: e.tensor_tensor(out=sq3, in0=src3, in1=st8[:, 0:H].unsqueeze(2).to_broadcast([128, H, d]), op=ALU.mult),
               r=(zc, st8), w=(sq,))
            op("dve", lambda e: e.tensor_tensor(out=out3, in0=sq3, in1=gain_ap.unsqueeze(1).to_broadcast([128, H, d]), op=ALU.mult),
               r=(sq, smt), w=(nb,))

        def transposes(src_b, blocks, bank, dst_b, dst_ap, rows=128):
            n = len(blocks)
            pv = pbf(bank)
            for i, bap in enumerate(blocks):
                op("pe", lambda e, i=i, bap=bap: e.transpose(out=pv[0:rows, i * 128:(i + 1) * 128], in_=bap, identity=ident[:]),
                   r=(src_b, ident) if i == 0 else (), w=(PB[bank],), sig=(i == n - 1))
            op("act", lambda e: e.copy(out=dst_ap, in_=pv[0:rows, 0:n * 128].rearrange("p (n t) -> p n t", n=n)),
               r=(PB[bank],), w=(dst_b,))

        def rope_apply(src3, out3, cs, H):
            cosb = cs[:, 0:32].unsqueeze(1).to_broadcast([128, H, 32])
            sinb = cs[:, 32:64].unsqueeze(1).to_broadcast([128, H, 32])
            x1, x2 = src3[:, :, 0:32], src3[:, :, 32:64]
            op("dve", lambda e: e.tensor_tensor(out=rt[:, 0], in0=x1, in1=cosb, op=ALU.mult), r=(rot, cst_b[0]), w=(rt,))
            op("dve", lambda e: e.tensor_tensor(out=rt[:, 1], in0=x2, in1=sinb, op=ALU.mult), r=(rot, cst_b[0]), w=(rt,))
            op("dve", lambda e: e.tensor_tensor(out=rt[:, 2], in0=x1, in1=sinb, op=ALU.mult), r=(rot, cst_b[0]), w=(rt,))
            op("dve", lambda e: e.tensor_tensor(out=rt[:, 3], in0=x2, in1=cosb, op=ALU.mult), r=(rot, cst_b[0]), w=(rt,))
            op("dve", lambda e: e.tensor_tensor(out=out3[:, :, 0:32], in0=rt[:, 0], in1=rt[:, 1], op=ALU.subtract), r=(rt,), w=(nb,))
            op("dve", lambda e: e.tensor_tensor(out=out3[:, :, 32:64], in0=rt[:, 2], in1=rt[:, 3], op=ALU.add), r=(rt,), w=(nb,))

        cst_b = [None]
        for st in range(NST):
            for tt in range(4):
                ti = st * 4 + tt
                xtile = xt[ti % 2]
                dma("sp", xtile, xtile[:], xcur, xcur[ti * 128:(ti + 1) * 128, :], xtile)
                dma("sp", cst[tt], cst[tt][:], cs_in, cs_in[ti * 128:(ti + 1) * 128, :], cst[tt])
                op("act", lambda e: e.activation(out=tmpD[:], in_=xtile[:], func=AF.Square, accum_out=st8[:, 8:9]),
                   r=(xtile,), w=(tmpD, st8))
                rstd_from_ssq(st8[:, 8:9], st8[:, 8:9], D, (st8,))
                op("dve", lambda e: e.scalar_tensor_tensor(out=tmpD[:], in0=xtile[:], scalar=st8[:, 8:9], in1=gm[:], op0=ALU.mult, op1=ALU.mult),
                   r=(xtile, st8, gm), w=(tmpD,))
                op("pool", lambda e: e.tensor_tensor(out=hb[:], in0=tmpD[:], in1=sh[:], op=ALU.add), r=(tmpD, sh), w=(hb,))
                for half in range(2):
                    transposes(hb, [hb[:, (half * 8 + i) * 128:(half * 8 + i + 1) * 128] for i in range(8)], 6 + half,
                               hts, hts[:, half * 8:half * 8 + 8, tt * 128:(tt + 1) * 128])
            for half in range(2):
                dma("pool", HT, HT[half * 1024:(half + 1) * 1024, st * 512:(st + 1) * 512].rearrange("(k p) t -> p k t", p=128),
                    hts, hts[:, half * 8:(half + 1) * 8, :], hts)
            for c in range(10):
                wt = wint[c % 2]
                ncols = {1: 320, 6: 332, 9: 256}.get(c, 512)
                dma("sp", wt, wt[:], WIN, WIN[:, c * 512:(c + 1) * 512].rearrange("(k p) n -> p k n", p=128), wt)
                for tt in range(4):
                    pz = PB[tt % 4]
                    ts_ = slice(tt * 128, (tt + 1) * 128)
                    mm(pz, pz[:, 0:ncols], [(hts[:, kc, ts_], wt[:, kc, 0:ncols]) for kc in range(16)], r=(hts, wt))
                for tt in range(4):
                    ti = st * 4 + tt
                    bg.tick()
                    cst_b[0] = cst[tt]
                    cs = cst[tt]
                    pz = PB[tt % 4]
                    ts_ = slice(tt * 128, (tt + 1) * 128)
                    vt = vts[tt]
                    if c in (4,):
                        op("act", lambda e: e.copy(out=vt[:, V_FOX:V_FOX + 512], in_=pz[:, 0:512]), r=(pz,), w=(vt,))
                        continue
                    op("act", lambda e: e.copy(out=zc[:, 0:ncols], in_=pz[:, 0:ncols]), r=(pz,), w=(zc,))
                    if c == 0:
                        hnorm(zc[:, 0:512].rearrange("p (h d) -> p h d", h=1), 1, 512, sm("cq"), nb[:, 0:512].rearrange("p (h d) -> p h d", h=1))
                        transposes(nb, [nb[:, i * 128:(i + 1) * 128] for i in range(4)], 6, nT, nT[:, 0:4, :])
                        p0, p1 = PB[4], PB[5]
                        mm(p0, p0[:, 0:512], [(nT[:, kc, :], wuqt[:, kc, 0:512]) for kc in range(4)], r=(nT, wuqt))
                        mm(p1, p1[:, 0:256], [(nT[:, kc, :], wuqt[:, kc, 512:768]) for kc in range(4)], r=(nT, wuqt))
                        op("act", lambda e: e.copy(out=zc[:, 0:512], in_=p0[:, 0:512]), r=(p0,), w=(zc,))
                        op("act", lambda e: e.copy(out=zc[:, 512:768], in_=p1[:, 0:256]), r=(p1,), w=(zc,))
                        op("dve", lambda e: e.tensor_tensor(out=sq[:, 0:768], in0=zc[:, 0:768], in1=zc[:, 0:768], op=ALU.mult), r=(zc,), w=(sq,))
                        op("dve", lambda e: e.tensor_reduce(out=st8[:, 0:4], in_=sq[:, 0:512].rearrange("p (h d) -> p h d", h=4), axis=AX.X, op=ALU.add), r=(sq,), w=(st8,))
                        op("dve", lambda e: e.tensor_reduce(out=st8[:, 4:8], in_=sq[:, 512:768].rearrange("p (h d) -> p h d", h=4), axis=AX.X, op=ALU.add), r=(sq,), w=(st8,))
                        op("dve", lambda e: e.tensor_tensor(out=st8[:, 0:4], in0=st8[:, 0:4], in1=st8[:, 4:8], op=ALU.add), r=(st8,), w=(st8,))
                        rstd_from_ssq(st8[:, 0:4], st8[:, 0:4], 192, (st8,))
                        o_, _w = SM_OFF["mq"]
                        gq_n, gq_r = smt[:, o_:o_ + 128], smt[:, o_ + 128:o_ + 192]
                        z3 = zc[:, 0:512].rearrange("p (h d) -> p h d", h=4)
                        s3 = sq[:, 0:512].rearrange("p (h d) -> p h d", h=4)
                        op("dve", lambda e: e.tensor_tensor(out=s3, in0=z3, in1=st8[:, 0:4].unsqueeze(2).to_broadcast([128, 4, 128]), op=ALU.mult), r=(zc, st8), w=(sq,))
                        op("dve", lambda e: e.tensor_tensor(out=nb[:, 0:512].rearrange("p (h d) -> p h d", h=4), in0=s3, in1=gq_n.unsqueeze(1).to_broadcast([128, 4, 128]), op=ALU.mult), r=(sq, smt), w=(nb,))
                        r3 = zc[:, 512:768].rearrange("p (h d) -> p h d", h=4)
                        op("dve", lambda e: e.tensor_tensor(out=rot[:], in0=r3, in1=st8[:, 0:4].unsqueeze(2).to_broadcast([128, 4, 64]), op=ALU.mult), r=(zc, st8), w=(rot,))
                        op("dve", lambda e: e.tensor_tensor(out=rot[:], in0=rot[:], in1=gq_r.unsqueeze(1).to_broadcast([128, 4, 64]), op=ALU.mult), r=(rot, smt), w=(rot,))
                        rope_apply(rot, nb[:, 512:768].rearrange("p (h d) -> p h d", h=4), cs, 4)
                        transposes(nb, [nb[:, i * 128:(i + 1) * 128] for i in range(6)], 7, fts, fts[:, B_QN:B_QN + 6, ts_])
                    elif c == 1:
                        hnorm(zc[:, 0:256].rearrange("p (h d) -> p h d", h=1), 1, 256, sm("ckv"), nb[:, 0:256].rearrange("p (h d) -> p h d", h=1))
                        transposes(nb, [nb[:, i * 128:(i + 1) * 128] for i in range(2)], 6, nT, nT[:, 0:2, :])
                        p0, p1 = PB[4], PB[5]
                        mm(p0, p0[:, 0:512], [(nT[:, kc, :], wukvt[:, kc, 0:512]) for kc in range(2)], r=(nT, wukvt))
                        mm(p1, p1[:, 0:512], [(nT[:, kc, :], wukvt[:, kc, 512:1024]) for kc in range(2)], r=(nT, wukvt))
                        op("act", lambda e: e.copy(out=vt[:, V_MLA:V_MLA + 512], in_=p1[:, 0:512]), r=(p1,), w=(vt,))
                        op("act", lambda e: e.copy(out=zc[:, 512:1024], in_=p0[:, 0:512]), r=(p0,), w=(zc,))
                        op("dve", lambda e: e.tensor_tensor(out=sq[:, 0:512], in0=zc[:, 512:1024], in1=zc[:, 512:1024], op=ALU.mult), r=(zc,), w=(sq,))
                        op("dve", lambda e: e.tensor_reduce(out=st8[:, 0:4], in_=sq[:, 0:512].rearrange("p (h d) -> p h d", h=4), axis=AX.X, op=ALU.add), r=(sq,), w=(st8,))
                        op("dve", lambda e: e.tensor_tensor(out=sq[:, 512:576], in0=zc[:, 256:320], in1=zc[:, 256:320], op=ALU.mult), r=(zc,), w=(sq,))
                        op("dve", lambda e: e.tensor_reduce(out=st8[:, 4:5], in_=sq[:, 512:576], axis=AX.X, op=ALU.add), r=(sq,), w=(st8,))
                        op("dve", lambda e: e.tensor_scalar(out=st8[:, 0:4], in0=st8[:, 0:4], scalar1=st8[:, 4:5], scalar2=None, op0=ALU.add), r=(st8,), w=(st8,))
                        rstd_from_ssq(st8[:, 0:4], st8[:, 0:4], 192, (st8,))
                        o_, _w = SM_OFF["mk"]
                        gk_n, gk_r = smt[:, o_:o_ + 128], smt[:, o_ + 128:o_ + 192]
                        z3 = zc[:, 512:1024].rearrange("p (h d) -> p h d", h=4)
                        s3 = sq[:, 0:512].rearrange("p (h d) -> p h d", h=4)
                        op("dve", lambda e: e.tensor_tensor(out=s3, in0=z3, in1=st8[:, 0:4].unsqueeze(2).to_broadcast([128, 4, 128]), op=ALU.mult), r=(zc, st8), w=(sq,))
                        op("dve", lambda e: e.tensor_tensor(out=nb[:, 0:512].rearrange("p (h d) -> p h d", h=4), in0=s3, in1=gk_n.unsqueeze(1).to_broadcast([128, 4, 128]), op=ALU.mult), r=(sq, smt), w=(nb,))
                        krb = zc[:, 256:320].unsqueeze(1).to_broadcast([128, 4, 64])
                        op("dve", lambda e: e.tensor_tensor(out=rot[:], in0=krb, in1=st8[:, 0:4].unsqueeze(2).to_broadcast([128, 4, 64]), op=ALU.mult), r=(zc, st8), w=(rot,))
                        op("dve", lambda e: e.tensor_tensor(out=rot[:], in0=rot[:], in1=gk_r.unsqueeze(1).to_broadcast([128, 4, 64]), op=ALU.mult), r=(rot, smt), w=(rot,))
                        rope_apply(rot, nb[:, 512:768].rearrange("p (h d) -> p h d", h=4), cs, 4)
                        transposes(nb, [nb[:, i * 128:(i + 1) * 128] for i in range(6)], 7, fts, fts[:, B_KN:B_KN + 6, ts_])
                    elif c in (2, 3, 5):
                        gname, blk = {2: ("fq", B_FQ), 3: ("fk", B_FK), 5: ("dq", B_DQ)}[c]
                        hnorm(zc[:, 0:512].rearrange("p (h d) -> p h d", h=4), 4, 128, sm(gname), nb[:, 0:512].rearrange("p (h d) -> p h d", h=4))
                        transposes(nb, [nb[:, i * 128:(i + 1) * 128] for i in range(4)], 6 + (c % 2), fts, fts[:, blk:blk + 4, ts_])
                    elif c == 6:
                        hnorm(zc[:, 0:128].rearrange("p (h d) -> p h d", h=1), 1, 128, sm("dk"), nb[:, 0:128].rearrange("p (h d) -> p h d", h=1))
                        op("pool", lambda e: e.tensor_copy(out=vt[:, V_DSA:V_DSA + 128], in_=zc[:, 128:256]), r=(zc,), w=(vt,))
                        op("pool", lambda e: e.tensor_copy(out=nb[:, 128:192], in_=zc[:, 256:320]), r=(zc,), w=(nb,))
                        transposes(nb, [nb[:, 0:128]], 6, fts, fts[:, B_DK:B_DK + 1, ts_])
                        transposes(nb, [nb[:, 128:192]], 7, fts, fts[0:64, B_IK:B_IK + 1, ts_], rows=64)
                        op("dve", lambda e: e.tensor_scalar(out=iwt[:], in0=zc[:, 320:328], scalar1=float(8 ** -0.5 * 64 ** -0.5), scalar2=None, op0=ALU.mult), r=(zc,), w=(iwt,))
                        dma("pool", IW, IW[ti * 128:(ti + 1) * 128, :], iwt, iwt[:], iwt)
                        op("dve", lambda e: e.tensor_tensor(out=lft[:], in0=zc[:, 328:332], in1=sm("fb"), op=ALU.add), r=(zc, smt), w=(lft,))
                        op("act", lambda e: e.activation(out=lft[:], in_=lft[:], func=AF.Exp, scale=-1.0), r=(lft,), w=(lft,))
                        op("act", lambda e: e.activation(out=lft[:], in_=lft[:], func=AF.Ln, bias=1.0), r=(lft,), w=(lft,))
                        p0 = PB[4]
                        mm(p0, p0[:, 0:4], [(tri[:], lft[:])], r=(tri, lft))
                        mm(p0, p0[:, 4:8], [(ones_f[:, 0:128], lft[:])], r=(ones_f, lft))
                        op("dve", lambda e: e.tensor_tensor(out=cumt[:], in0=p0[:, 0:4], in1=runt[:], op=ALU.add), r=(p0, runt), w=(cumt,))
                        op("dve", lambda e: e.tensor_tensor(out=runt[:], in0=p0[:, 4:8], in1=runt[:], op=ALU.add), r=(p0, runt), w=(runt,))
                        dma("pool", CUML, CUML[ti * 128:(ti + 1) * 128, :], cumt, cumt[:], cumt)
                        p1 = PB[5]
                        op("pe", lambda e: e.transpose(out=p1[0:4, 0:128], in_=cumt[:], identity=identf[:]), r=(cumt, identf), w=(p1,))
                        op("act", lambda e: e.copy(out=cumTt[:], in_=p1[0:4, 0:128]), r=(p1,), w=(cumTt,))
                        dma("pool", CUMT, CUMT[:, ti * 128:(ti + 1) * 128], cumTt, cumTt[:], cumTt)
                    elif c == 7:
                        op("pool", lambda e: e.tensor_copy(out=nb[:, 0:512], in_=zc[:, 0:512]), r=(zc,), w=(nb,))
                        transposes(nb, [nb[:, i * 128:(i + 1) * 128] for i in range(4)], 7, fts, fts[:, B_IQ:B_IQ + 4, ts_])
                    elif c == 8:
                        hnorm(zc[:, 0:512].rearrange("p (h d) -> p h d", h=8), 8, 64, sm("sq"), nb[:, 0:512].rearrange("p (h d) -> p h d", h=8))
                        transposes(nb, [nb[:, i * 128:(i + 1) * 128] for i in range(4)], 6, fts, fts[:, B_SQ:B_SQ + 4, ts_])
                    elif c == 9:
                        hnorm(zc[:, 0:128].rearrange("p (h d) -> p h d", h=2), 2, 64, sm("sk"), nb[:, 0:128].rearrange("p (h d) -> p h d", h=2))
                        op("pool", lambda e: e.tensor_copy(out=vt[:, V_SWA:V_SWA + 128], in_=zc[:, 128:256]), r=(zc,), w=(vt,))
                        transposes(nb, [nb[:, 0:128]], 7, fts, fts[:, B_SK:B_SK + 1, ts_])
                        dma("pool", VT, VT[ti * 128:(ti + 1) * 128, :], vt, vt[:], vt)
            for b0 in range(0, NFB, 7):
                dma("pool", FT, FT[b0 * 128:(b0 + 7) * 128, st * 512:(st + 1) * 512].rearrange("(b p) t -> p b t", p=128),
                    fts, fts[:, b0:b0 + 7, :], fts)
        k.end_phase()
        dsp.recycle()

        if stop == "p1":
            return finish()
        def attn_pair(psS, pairs, r, pT, exp_scale, bias_ap=None, bias_b=None, addmask=None, pre=None):
            mm(psS, psS[:, 0:512], pairs, r=r)
            if pre is not None:
                pre(psS)
            if addmask is not None:
                op("dve", lambda e: e.tensor_tensor(out=mtmp[:], in0=psS[:, 0:512], in1=addmask, op=ALU.add), r=(psS, negcm), w=(mtmp,))
                op("act", lambda e: e.activation(out=pT[:], in_=mtmp[:], func=AF.Exp, scale=exp_scale), r=(mtmp,), w=(pT,))
            else:
                op("act", lambda e: e.activation(out=pT[:], in_=psS[:, 0:512], func=AF.Exp, scale=exp_scale), r=(psS,), w=(pT,))

        def finalize(psO, psD, rows, out_b, rdt, extra_add=None):
            if extra_add is not None:
                op("dve", lambda e: e.tensor_tensor(out=rdt[0:rows, :], in0=psD[0:rows, :], in1=extra_add, op=ALU.add), r=(psD, est), w=(rdt,))
                op("dve", lambda e: e.reciprocal(out=rdt[0:rows, :], in_=rdt[0:rows, :]), r=(rdt,), w=(rdt,))
            else:
                op("dve", lambda e: e.reciprocal(out=rdt[0:rows, :], in_=psD[0:rows, :]), r=(psD,), w=(rdt,))
            op("dve", lambda e: e.tensor_tensor(out=out_b[0:rows, :], in0=psO[0:rows, :], in1=rdt[0:rows, :], op=ALU.mult), r=(psO, rdt), w=(out_b,))

        k.begin_phase()
        kn = [sbt("kn%d" % i, [128, S], BF16) for i in range(2)]
        kr = [sbt("kr%d" % i, [64, S], BF16) for i in range(2)]
        vv = [sbt("vv%d" % i, [128, 32, 128], BF16) for i in range(2)]
        qn = [sbt("qn%d" % i, [128, 512], BF16) for i in range(2)]
        qr = [sbt("qr%d" % i, [64, 512], BF16) for i in range(2)]
        pTa = [sbt("pTa%d" % i, [128, 512], BF16) for i in range(2)]
        mtmp = sbt("mtmp", [128, 512], F32)
        rdta = sbt("rdta", [128, 512], F32)
        otla = [sbt("otla%d" % i, [128, 512], BF16) for i in range(2)]
        ca = sbt("ca", [1, S], F32)
        cnq = [sbt("cnq%d" % i, [1, 512], F32) for i in range(2)]
        ikt = sbt("ikt", [64, S], BF16)
        iqt = [sbt("iqt%d" % i, [64, 8, 128], BF16) for i in range(2)]
        iwq = [sbt("iwq%d" % i, [128, 8], F32) for i in range(2)]
        Itl = [sbt("ItA", [128, S], F32), sbt("ItB", [128, S], F32)]
        bsl = [sbt("bsA", [128, 8], F32), sbt("bsB", [128, 8], F32)]
        stl = [sbt("stA", [128, 32], F32), sbt("stB", [128, 32], F32)]
        Mt = sbt("Mt", [128, S], BF16)
        MT = sbt("MT", [128, 32, 512], BF16)
        rl = [sbt("rl%d" % i, [128, 512], F32) for i in range(2)]
        m8 = sbt("m8", [128, 8], F32)

        def gen_mla_fox():
            cnt = 0
            for mixer in ("mla", "fox"):
                scale = (192 ** -0.5) if mixer == "mla" else (128 ** -0.5)
                qblk, kblk, vbase, obase = (B_QN, B_KN, V_MLA, 0) if mixer == "mla" else (B_FQ, B_FK, V_FOX, 512)
                for h in range(4):
                    knh, vvh = kn[h % 2], vv[h % 2]
                    dma("sp", knh, knh[:], FT, FT[(kblk + h) * 128:(kblk + h + 1) * 128, :], knh)
                    for q4 in range(4):
                        dma("sp", vvh, vvh[:, q4 * 8:(q4 + 1) * 8, :], VT,
                            VT[q4 * 1024:(q4 + 1) * 1024, vbase + h * 128:vbase + (h + 1) * 128].rearrange("(kb p) d -> p kb d", p=128), vvh)
                    if mixer == "mla":
                        krh = kr[h % 2]
                        dma("sp", krh, krh[:], FT, FT[B_KR * 128 + h * 64:B_KR * 128 + (h + 1) * 64, :], krh)
                    else:
                        dma("sp", ca, ca[:], CUMT, CUMT[h:h + 1, :], ca)
                        op("dve", lambda e: e.tensor_scalar(out=ca[:], in0=ca[:], scalar1=float(1.0 / scale), scalar2=None, op0=ALU.mult), r=(ca,), w=(ca,))
                    yield 0.5
                    for j in range(NST):
                        qs = slice(j * 512, (j + 1) * 512)
                        qnj = qn[(h * NST + j) % 2]
                        dma("sp", qnj, qnj[:], FT, FT[(qblk + h) * 128:(qblk + h + 1) * 128, qs], qnj)
                        if mixer == "mla":
                            qrj = qr[(h * NST + j) % 2]
                            dma("sp", qrj, qrj[:], FT, FT[B_QR * 128 + h * 64:B_QR * 128 + (h + 1) * 64, qs], qrj)
                        else:
                            cnj = cnq[(h * NST + j) % 2]
                            op("dve", lambda e: e.tensor_scalar(out=cnj[:], in0=ca[0:1, qs], scalar1=-1.0, scalar2=None, op0=ALU.mult), r=(ca,), w=(cnj,))
                        psO, psD = PB[4], PB[5]
                        nkb = 4 * j + 4
                        def qk(kb_, c_):
                            ks_ = slice(kb_ * 128, (kb_ + 1) * 128)
                            ps_ = PB[c_ % 2]
                            if mixer == "mla":
                                pairs = [(knh[:, ks_], qnj[:]), (krh[0:64, ks_], qrj[0:64, :])]
                                r = (knh, krh, qnj, qrj)
                            else:
                                pairs = [(knh[:, ks_], qnj[:]), (ca[0:1, ks_], ones_f[0:1, 0:512]), (ones_f[0:1, 0:128], cnj[0:1, :])]
                                r = (knh, qnj, ca, cnj, ones_f)
                            mm(ps_, ps_[:, 0:512], pairs, r=r)
                        qk(0, cnt)
                        for kb in range(nkb):
                            psS = PB[cnt % 2]
                            pT = pTa[cnt % 2]
                            cnt += 1
                            if kb + 1 < nkb:
                                qk(kb + 1, cnt)
                            if kb >= 4 * j:
                                op("dve", lambda e: e.tensor_tensor(out=mtmp[:], in0=psS[:, 0:512], in1=negcm[:, kb - 4 * j, :], op=ALU.add), r=(psS, negcm), w=(mtmp,))
                                op("act", lambda e: e.activation(out=pT[:], in_=mtmp[:], func=AF.Exp, scale=scale), r=(mtmp,), w=(pT,))
                            else:
                                op("act", lambda e: e.activation(out=pT[:], in_=psS[:, 0:512], func=AF.Exp, scale=scale), r=(psS,), w=(pT,))
                            mm(psO, psO[:, 0:512], [(vvh[:, kb, :], pT[:])], r=(vvh, pT), start=(kb == 0), stop=(kb == nkb - 1))
                            mm(psD, psD[:, 0:512], [(ones_b[:], pT[:])], r=(ones_b, pT), start=(kb == 0), stop=(kb == nkb - 1))
                            yield (3.5 if mixer == "mla" else 7.0)
                        ot = otla[j % 2]
                        finalize(psO, psD, 128, ot, rdta)
                        dma("pool", OT, OT[obase + h * 128:obase + (h + 1) * 128, qs], ot, ot[:], ot)
                        yield 3.0

        NIT = 30

        def gen_indexer():
            cnt = 0
            dma("sp", ikt, ikt[:], FT, FT[B_IK * 128:B_IK * 128 + 64, :], ikt)
            for j in range(NST):
                op("dve", lambda e: e.memset(MT[:, 4 * j:4 * j + 4, :], 0.0), w=(MT,))
                for up in range(2):
                    blks = []
                    for ui in range(2):
                        u = 2 * up + ui
                        i = 4 * j + u
                        nk = (i + 1) * 128
                        It, bs, stp = Itl[ui], bsl[ui], stl[ui]
                        blks.append((u, i, nk, It, bs, stp))
                        iq_, iw_ = iqt[i % 2], iwq[i % 2]
                        dma("sp", iq_, iq_[:], FT, FT[B_IQ * 128:(B_IQ + 4) * 128, i * 128:(i + 1) * 128].rearrange("(jj p) t -> p jj t", p=64), iq_)
                        dma("sp", iw_, iw_[:], IW, IW[i * 128:(i + 1) * 128, :], iw_)
                        for c0 in range(0, nk, 512):
                            cw = min(512, nk - c0)
                            for jj in range(8):
                                psS = PB[2 + cnt % 2]
                                rlt = rl[cnt % 2]
                                cnt += 1
                                mm(psS, psS[:, 0:cw], [(iq_[0:64, jj, :], ikt[0:64, c0:c0 + cw])], r=(iq_, ikt))
                                op("act", lambda e: e.activation(out=rlt[:, 0:cw], in_=psS[:, 0:cw], func=AF.Relu), r=(psS,), w=(rlt,))
                                if jj == 0:
                                    op("dve", lambda e: e.tensor_scalar(out=It[:, c0:c0 + cw], in0=rlt[:, 0:cw], scalar1=iw_[:, 0:1], scalar2=None, op0=ALU.mult),
                                       r=(rlt, iw_), w=(It,))
                                else:
                                    op("dve", lambda e: e.scalar_tensor_tensor(out=It[:, c0:c0 + cw], in0=rlt[:, 0:cw], scalar=iw_[:, jj:jj + 1], in1=It[:, c0:c0 + cw],
                                                                                 op0=ALU.mult, op1=ALU.add), r=(rlt, iw_, It), w=(It,))
                                if jj % 2 == 1:
                                    yield 2.0 * cw / 960.0 + 0.2
                        if nk > 256:
                            op("dve", lambda e: e.tensor_reduce(out=bs[:, 5:6], in_=It[:, 0:nk], axis=AX.X, op=ALU.max, apply_absolute_value=True), r=(It,), w=(bs,))
                            op("dve", lambda e: e.tensor_scalar(out=bs[:, 5:6], in0=bs[:, 5:6], scalar1=1.001, scalar2=1e-3, op0=ALU.mult, op1=ALU.add), r=(bs,), w=(bs,))
                            op("dve", lambda e: e.tensor_scalar(out=stp[:], in0=pw2[:], scalar1=bs[:, 5:6], scalar2=None, op0=ALU.mult), r=(pw2, bs), w=(stp,))
                            op("dve", lambda e: e.tensor_scalar(out=bs[:, 0:1], in0=bs[:, 5:6], scalar1=-1.0, scalar2=None, op0=ALU.mult), r=(bs,), w=(bs,))
                            op("dve", lambda e: e.memset(bs[:, 2:3], 0.0), w=(bs,))
                        op("dve", lambda e: e.tensor_tensor(out=It[:, i * 128:(i + 1) * 128], in0=It[:, i * 128:(i + 1) * 128], in1=negtri[:], op=ALU.add), r=(It, negtri), w=(It,))
                        yield 1.0
                    if blks[0][2] > 256:
                        for kk in range(NIT):
                            for (u, i, nk, It, bs, stp) in blks:
                                op("act", lambda e: e.activation(out=Mt[:, 0:nk], in_=It[:, 0:nk], func=AF.Sign, bias=bs[:, 2:3], scale=-1.0, accum_out=bs[:, 3:4]),
                                   r=(It, bs), w=(Mt, bs))
                                op("dve", lambda e: e.scalar_tensor_tensor(out=bs[:, 4:5], in0=bs[:, 3:4], scalar=float(nk - 512) + 0.5, in1=stp[:, kk:kk + 1],
                                                                             op0=ALU.is_le, op1=ALU.mult), r=(bs, stp), w=(bs,))
                                op("dve", lambda e: e.scalar_tensor_tensor(out=bs[:, 2:3], in0=bs[:, 4:5], scalar=stp[:, kk + 1:kk + 2], in1=bs[:, 0:1],
                                                                             op0=ALU.add, op1=ALU.add), r=(bs, stp), w=(bs,))
                                op("dve", lambda e: e.tensor_tensor(out=bs[:, 0:1], in0=bs[:, 0:1], in1=bs[:, 4:5], op=ALU.add), r=(bs,), w=(bs,))
                            yield (blks[0][2] + blks[1][2]) / 1400.0 + 1.0
                    for (u, i, nk, It, bs, stp) in blks:
                        if nk > 256:
                            thr, thr_b = bs[:, 0:1], bs
                        else:
                            thr, thr_b = neg29[:, 0:1], neg29
                        op("dve", lambda e: e.tensor_scalar(out=Mt[:, 0:nk], in0=It[:, 0:nk], scalar1=thr, scalar2=None, op0=ALU.is_ge), r=(It, thr_b), w=(Mt,))
                        for kb0 in range(0, i + 1, 8):
                            n = min(8, i + 1 - kb0)
                            tb_ = 6 + (kb0 // 8) % 2
                            pv = pbf(tb_)
                            for t_ in range(n):
                                kb = kb0 + t_
                                op("pe", lambda e: e.transpose(out=pv[:, t_ * 128:(t_ + 1) * 128], in_=Mt[:, kb * 128:(kb + 1) * 128], identity=ident[:]),
                                   r=(Mt, ident) if t_ == 0 else (), w=(PB[tb_],), sig=(t_ == n - 1))
                            op("act", lambda e: e.copy(out=MT[:, kb0:kb0 + n, u * 128:(u + 1) * 128], in_=pv[:, 0:n * 128].rearrange("p (n t) -> p n t", n=n)),
                               r=(PB[tb_],), w=(MT,))
                            yield 1.0
                nkb = 4 * j + 4
                dma("pool", MTD, MTD[j * 128:(j + 1) * 128, 0:nkb * 512], MT, MT[:, 0:nkb, :].rearrange("p a b -> p (a b)"), MT)
                yield 1.0

        gens = [[gen_mla_fox(), 0.0, 1.0], [gen_indexer(), 0.0, 2.0]]
        while gens:
            g_ = min(gens, key=lambda x: x[1])
            try:
                g_[1] += next(g_[0]) * g_[2]
            except StopIteration:
                gens.remove(g_)
            bg.tick(0.25)
        k.end_phase()
        dsp.recycle()

        k.begin_phase()
        dkt = sbt("dkt", [128, S], BF16)
        dvv = sbt("dvv", [128, 32, 128], BF16)
        mtj = [sbt("mtj%d" % i, [128, 32, 512], BF16) for i in range(2)]
        dq = [sbt("dq%d" % i, [128, 512], BF16) for i in range(2)]
        pTt = [sbt("pT%d" % i, [128, 512], BF16) for i in range(3)]
        rdt = sbt("rdt", [128, 512], F32)
        otl = [sbt("otl%d" % i, [128, 512], BF16) for i in range(2)]
        dma("sp", dkt, dkt[:], FT, FT[B_DK * 128:(B_DK + 1) * 128, :], dkt)
        for q4 in range(4):
            dma("sp", dvv, dvv[:, q4 * 8:(q4 + 1) * 8, :], VT,
                VT[q4 * 1024:(q4 + 1) * 1024, V_DSA:V_DSA + 128].rearrange("(kb p) d -> p kb d", p=128), dvv)
        dscale = 128 ** -0.5
        cnt = 0
        for j in range(NST):
            qs = slice(j * 512, (j + 1) * 512)
            nkb = 4 * j + 4
            MTj = mtj[j % 2]
            dma("sp", MTj, MTj[:, 0:nkb, :].rearrange("p a b -> p (a b)"), MTD, MTD[j * 128:(j + 1) * 128, 0:nkb * 512], MTj)
            for h in range(4):
                dqh = dq[h % 2]
                dma("sp", dqh, dqh[:], FT, FT[(B_DQ + h) * 128:(B_DQ + h + 1) * 128, qs], dqh)
                psO, psD = PB[4 + (h % 2) * 2], PB[5 + (h % 2) * 2]
                mm(PB[cnt % 3], PB[cnt % 3][:, 0:512], [(dkt[:, 0:128], dqh[:])], r=(dkt, dqh))
                for kb in range(nkb):
                    psS = PB[cnt % 3]
                    pT = pTt[cnt % 3]
                    cnt += 1
                    bg.tick()
                    if kb + 1 < nkb:
                        mm(PB[cnt % 3], PB[cnt % 3][:, 0:512], [(dkt[:, (kb + 1) * 128:(kb + 2) * 128], dqh[:])], r=(dkt, dqh))
                    op("act", lambda e: e.activation(out=pT[:], in_=psS[:, 0:512], func=AF.Exp, scale=dscale), r=(psS,), w=(pT,))
                    op("dve", lambda e: e.tensor_tensor(out=pT[:], in0=pT[:], in1=MTj[:, kb, :], op=ALU.mult), r=(pT, MTj), w=(pT,))
                    rb = kb - 4 * j
                    if rb >= -1:
                        lo, hi = max(0, rb * 128), min(512, rb * 128 + 256)
                        op("dve", lambda e: e.tensor_tensor(out=pT[:, lo:hi], in0=pT[:, lo:hi], in1=ew2[:, h, lo - rb * 128:hi - rb * 128], op=ALU.mult),
                           r=(pT, ew2), w=(pT,))
                    mm(psO, psO[:, 0:512], [(dvv[:, kb, :], pT[:])], r=(dvv, pT), start=(kb == 0), stop=(kb == nkb - 1))
                    mm(psD, psD[:, 0:512], [(ones_b[:], pT[:])], r=(ones_b, pT), start=(kb == 0), stop=(kb == nkb - 1))
                ot = otl[h % 2]
                finalize(psO, psD, 128, ot, rdt)
                dma("pool", OT, OT[1024 + h * 128:1024 + (h + 1) * 128, qs], ot, ot[:], ot)
        k.end_phase()
        dsp.recycle()

        k.begin_phase()
        skt = sbt("skt", [64, 2, S], BF16)
        svv = sbt("svv", [128, 32, 128], BF16)
        sqt = [sbt("sqt%d" % i, [64, 8, 128], BF16) for i in range(2)]
        pTt = [sbt("pT%d" % i, [128, 512], BF16) for i in range(2)]
        rdt = sbt("rdt", [128, 512], F32)
        otl = [sbt("otl%d" % i, [64, 512], BF16) for i in range(2)]
        es8 = sbt("es8", [128, 8], F32)
        est = sbt("est", [64, 2, 512], F32)
        dma("sp", skt, skt[:], FT, FT[B_SK * 128:(B_SK + 1) * 128, :].rearrange("(g p) t -> p g t", p=64), skt)
        for q4 in range(4):
            dma("sp", svv, svv[:, q4 * 8:(q4 + 1) * 8, :], VT,
                VT[q4 * 1024:(q4 + 1) * 1024, V_SWA:V_SWA + 128].rearrange("(kb p) d -> p kb d", p=128), svv)
        o_, _w = SM_OFF["sink"]
        op("act", lambda e: e.activation(out=es8[:], in_=smt[:, o_:o_ + 8], func=AF.Exp), r=(smt,), w=(es8,))
        for hh in range(8):
            op("dve", lambda e: e.tensor_copy(out=est[:, hh // 4, (hh % 4) * 128:(hh % 4 + 1) * 128], in_=es8[0:64, hh:hh + 1].to_broadcast([64, 128])),
               r=(es8,), w=(est,))
        sscale = 64 ** -0.5
        cnt = 0
        for i in range(NT):
            sq_ = sqt[i % 2]
            dma("sp", sq_, sq_[:], FT, FT[B_SQ * 128:(B_SQ + 4) * 128, i * 128:(i + 1) * 128].rearrange("(hh p) t -> p hh t", p=64), sq_)
            for g in range(2):
                psO, psD = PB[4 + (g % 2) * 2], PB[5 + (g % 2) * 2]
                rels = [(1, i)] if i == 0 else [(0, i - 1), (1, i)]
                bg.tick()
                for ri, (rel, kb) in enumerate(rels):
                    psS = PB[cnt % 2]
                    pT = pTt[cnt % 2]
                    cnt += 1
                    mm(psS, psS[:, 0:512], [(skt[0:64, g, kb * 128:(kb + 1) * 128], sq_[0:64, 4 * g:4 * g + 4, :])], r=(skt, sq_))
                    op("act", lambda e: e.activation(out=pT[:], in_=psS[:, 0:512], func=AF.Exp, scale=sscale), r=(psS,), w=(pT,))
                    op("dve", lambda e: e.tensor_tensor(out=pT[:], in0=pT[:], in1=ebs[:, g, rel, :], op=ALU.mult), r=(pT, ebs), w=(pT,))
                    mm(psO, psO[0:64, 0:512], [(svv[:, kb, g * 64:(g + 1) * 64], pT[:])], r=(svv, pT), start=(ri == 0), stop=(ri == len(rels) - 1))
                    mm(psD, psD[0:64, 0:512], [(ones_b[:, 0:64], pT[:])], r=(ones_b, pT), start=(ri == 0), stop=(ri == len(rels) - 1))
                ot = otl[g % 2]
                finalize(psO, psD, 64, ot, rdt, extra_add=est[:, g, :])
                dma("pool", OT, OT[1536 + 256 * g:1536 + 256 * (g + 1), i * 128:(i + 1) * 128].rearrange("(hi d) t -> d hi t", d=64),
                    ot, ot[:].rearrange("p (hi t) -> p hi t", hi=4), ot)
        k.end_phase()
        dsp.recycle()
        if stop == "attn":
            return finish()

        k.begin_phase()
        g1 = sbt("g1", [128, D], F32)
        load_bcast(g1, g1[:], MOD, MOD[L:L + 1, 2 * D:3 * D])
        hts = sbt("hts", [128, 16, 512], BF16)
        ots = sbt("ots", [128, 16, 512], BF16)
        wgt = [sbt("wgt%d" % i, [128, 16, 256], BF16) for i in range(4)]
        wbt = [sbt("wbt%d" % i, [128, 4, 256], BF16) for i in range(4)]
        mT = sbt("mT", [128, 16, 512], BF16)
        sg = [sbt("sg%d" % i, [128, 512], F32) for i in range(2)]
        tm = [sbt("tm%d" % i, [128, 512], F32) for i in range(2)]
        acc = sbt("acc", [128, 512], F32)
        wot = [sbt("wot%d" % i, [128, 16, 256], BF16) for i in range(2)]
        xt4 = [sbt("xt4_%d" % i, [128, D], F32) for i in range(4)]
        cnt = 0
        for st in range(NST):
            tsl = slice(st * 512, (st + 1) * 512)
            for half in range(2):
                dma("sp", hts, hts[:, half * 8:(half + 1) * 8, :], HT, HT[half * 1024:(half + 1) * 1024, tsl].rearrange("(k p) t -> p k t", p=128), hts)
                dma("sp", ots, ots[:, half * 8:(half + 1) * 8, :], OT, OT[half * 1024:(half + 1) * 1024, tsl].rearrange("(k p) t -> p k t", p=128), ots)
            for tt in range(4):
                ti = st * 4 + tt
                dma("sp", xt4[tt], xt4[tt][:], xcur, xcur[ti * 128:(ti + 1) * 128, :], xt4[tt])
            for mg in range(8):
                for i in range(4):
                    for half in range(2):
                        dma("sp", wgt[i], wgt[i][:, half * 8:(half + 1) * 8, :], WG,
                            WG[i * D + half * 1024:i * D + (half + 1) * 1024, mg * 256:(mg + 1) * 256].rearrange("(k p) n -> p k n", p=128), wgt[i])
                    dma("sp", wbt[i], wbt[i][:], WB, WB[i * 512:(i + 1) * 512, mg * 256:(mg + 1) * 256].rearrange("(k p) n -> p k n", p=128), wbt[i])
                for m2 in range(2):
                    m = 2 * mg + m2
                    ms = slice(m2 * 128, (m2 + 1) * 128)
                    for i in range(4):
                        psG, psB = PB[cnt % 2], PB[2 + cnt % 2]
                        sgt, tmt = sg[cnt % 2], tm[cnt % 2]
                        cnt += 1
                        bg.tick()
                        mm(psG, psG[:, 0:512], [(wgt[i][:, kc, ms], hts[:, kc, :]) for kc in range(16)], r=(wgt[i], hts))
                        mm(psB, psB[:, 0:512], [(wbt[i][:, kc, ms], ots[:, 4 * i + kc, :]) for kc in range(4)], r=(wbt[i], ots))
                        op("act", lambda e: e.activation(out=sgt[:], in_=psG[:, 0:512], func=AF.Sigmoid), r=(psG,), w=(sgt,))
                        if i == 0:
                            op("dve", lambda e: e.tensor_tensor(out=acc[:], in0=psB[:, 0:512], in1=sgt[:], op=ALU.mult), r=(psB, sgt), w=(acc,))
                        else:
                            op("dve", lambda e: e.tensor_tensor(out=tmt[:], in0=psB[:, 0:512], in1=sgt[:], op=ALU.mult), r=(psB, sgt), w=(tmt,))
                            if i < 3:
                                op("pool", lambda e: e.tensor_tensor(out=acc[:], in0=acc[:], in1=tmt[:], op=ALU.add), r=(acc, tmt), w=(acc,))
                            else:
                                op("pool", lambda e: e.tensor_tensor(out=mT[:, m, :], in0=acc[:], in1=tmt[:], op=ALU.add), r=(acc, tmt), w=(mT,))
            for n in range(8):
                wo = wot[n % 2]
                ns = slice(n * 256, (n + 1) * 256)
                for half in range(2):
                    dma("sp", wo, wo[:, half * 8:(half + 1) * 8, :], WO, WO[half * 1024:(half + 1) * 1024, ns].rearrange("(k p) n -> p k n", p=128), wo)
                for tt in range(4):
                    bg.tick()
                    pso = PB[4 + tt]
                    mm(pso, pso[:, 0:256], [(mT[:, kc, tt * 128:(tt + 1) * 128], wo[:, kc, :]) for kc in range(16)], r=(mT, wo))
                    op("dve", lambda e: e.tensor_tensor(out=tm[tt % 2][:, 0:256], in0=pso[:, 0:256], in1=g1[:, ns], op=ALU.mult), r=(pso, g1), w=(tm[tt % 2],))
                    op("pool", lambda e: e.tensor_tensor(out=xt4[tt][:, ns], in0=xt4[tt][:, ns], in1=tm[tt % 2][:, 0:256], op=ALU.add), r=(xt4[tt], tm[tt % 2]), w=(xt4[tt],))
            for tt in range(4):
                ti = st * 4 + tt
                dma("pool", xmid, xmid[ti * 128:(ti + 1) * 128, :], xt4[tt], xt4[tt][:], xt4[tt])
        k.end_phase()
        dsp.recycle()

        if stop == "p3":
            return finish()
        k.begin_phase()
        gm = sbt("gm2", [128, D], F32)
        sh = sbt("sh2", [128, D], F32)
        g2 = sbt("g2", [128, D], F32)
        tmpD = sbt("tmpD2", [128, D], F32)
        load_bcast(gm, gm[:], MOD, MOD[L:L + 1, 4 * D:5 * D])
        load_bcast(tmpD, tmpD[:], norm_ffn, norm_ffn[L:L + 1, :])
        op("dve", lambda e: e.scalar_tensor_tensor(out=gm[:], in0=gm[:], scalar=1.0, in1=tmpD[:], op0=ALU.add, op1=ALU.mult), r=(gm, tmpD), w=(gm,))
        load_bcast(sh, sh[:], MOD, MOD[L:L + 1, 3 * D:4 * D])
        load_bcast(g2, g2[:], MOD, MOD[L:L + 1, 5 * D:6 * D])
        xt4 = [sbt("xt4_%d" % i, [128, D], F32) for i in range(4)]
        hb = sbt("hb2", [128, D], BF16)
        h2T = sbt("h2T", [128, 16, 512], BF16)
        at = sbt("at", [128, 22, 512], BF16)
        w1t = [sbt("w1t%d" % i, [128, 16, 256], BF16) for i in range(2)]
        w3t = [sbt("w3t%d" % i, [128, 16, 256], BF16) for i in range(2)]
        w2t = [sbt("w2t%d" % i, [128, 11, 512], BF16) for i in range(2)]
        sg = [sbt("sgf%d" % i, [128, 512], F32) for i in range(2)]
        tm = [sbt("tmf%d" % i, [128, 512], F32) for i in range(2)]
        st8 = sbt("st8f", [128, 16], F32)
        if moe:
            rtf = sbt("rtf", [128, 16, 8], F32)
            rtb = sbt("rtb", [128, 16, 8], BF16)
            lg = sbt("lg", [128, 8], F32)
            m8 = sbt("m8f", [128, 8], F32)
            gt = [sbt("gt%d" % i, [128, 8], F32) for i in range(4)]
            e1 = sbt("e1", [128, 8], F32)
            dma("sp", rtf, rtf[:], moe_router, moe_router[j2 * D:(j2 + 1) * D, :].rearrange("(k p) n -> p k n", p=128), rtf)
            op("dve", lambda e: e.tensor_copy(out=rtb[:], in_=rtf[:]), r=(rtf,), w=(rtb,))
        cnt = 0
        wc = 0
        for st in range(NST):
            for tt in range(4):
                ti = st * 4 + tt
                xtile = xt4[tt]
                dma("sp", xtile, xtile[:], xmid, xmid[ti * 128:(ti + 1) * 128, :], xtile)
                op("act", lambda e: e.activation(out=tmpD[:], in_=xtile[:], func=AF.Square, accum_out=st8[:, 8:9]), r=(xtile,), w=(tmpD, st8))
                rstd_from_ssq(st8[:, 8:9], st8[:, 8:9], D, (st8,))
                op("dve", lambda e: e.scalar_tensor_tensor(out=tmpD[:], in0=xtile[:], scalar=st8[:, 8:9], in1=gm[:], op0=ALU.mult, op1=ALU.mult), r=(xtile, st8, gm), w=(tmpD,))
                op("pool", lambda e: e.tensor_tensor(out=hb[:], in0=tmpD[:], in1=sh[:], op=ALU.add), r=(tmpD, sh), w=(hb,))
                for half in range(2):
                    bank = 6 + half
                    pv = pbf(bank)
                    for i_ in range(8):
                        kc = half * 8 + i_
                        op("pe", lambda e: e.transpose(out=pv[:, i_ * 128:(i_ + 1) * 128], in_=hb[:, kc * 128:(kc + 1) * 128], identity=ident[:]),
                           r=(hb, ident) if i_ == 0 else (), w=(PB[bank],), sig=(i_ == 7))
                    op("act", lambda e: e.copy(out=h2T[:, half * 8:half * 8 + 8, tt * 128:(tt + 1) * 128], in_=pv[:, 0:1024].rearrange("p (n t) -> p n t", n=8)),
                       r=(PB[bank],), w=(h2T,))
                if moe:
                    pz = PB[4]
                    mm(pz, pz[:, 0:8], [(h2T[:, kc, tt * 128:(tt + 1) * 128], rtb[:, kc, :]) for kc in range(16)], r=(h2T, rtb))
                    op("dve", lambda e: e.tensor_copy(out=lg[:], in_=pz[:, 0:8]), r=(pz,), w=(lg,))
                    op("dve", lambda e: e.max(out=m8[:], in_=lg[:]), r=(lg,), w=(m8,))
                    op("dve", lambda e: e.tensor_tensor(out=st8[:, 0:1], in0=m8[:, 0:1], in1=m8[:, 1:2], op=ALU.subtract), r=(m8,), w=(st8,))
                    op("act", lambda e: e.activation(out=st8[:, 1:2], in_=st8[:, 0:1], func=AF.Sigmoid), r=(st8,), w=(st8,))
                    op("dve", lambda e: e.tensor_scalar(out=st8[:, 2:3], in0=st8[:, 1:2], scalar1=-1.0, scalar2=1.0, op0=ALU.mult, op1=ALU.add), r=(st8,), w=(st8,))
                    op("dve", lambda e: e.tensor_scalar(out=e1[:], in0=lg[:], scalar1=m8[:, 0:1], scalar2=st8[:, 1:2], op0=ALU.is_equal, op1=ALU.mult), r=(lg, m8, st8), w=(e1,))
                    op("dve", lambda e: e.tensor_scalar(out=gt[tt][:], in0=lg[:], scalar1=m8[:, 1:2], scalar2=st8[:, 2:3], op0=ALU.is_equal, op1=ALU.mult), r=(lg, m8, st8), w=(gt[tt],))
                    op("dve", lambda e: e.tensor_tensor(out=gt[tt][:], in0=gt[tt][:], in1=e1[:], op=ALU.add), r=(gt[tt], e1), w=(gt[tt],))
            for ex in range(NE):
                for fg in range(11):
                    w1, w3 = w1t[wc % 2], w3t[wc % 2]
                    wc += 1
                    for half in range(2):
                        dma("sp", w1, w1[:, half * 8:(half + 1) * 8, :], FW1,
                            FW1[ex * D + half * 1024:ex * D + (half + 1) * 1024, fg * 256:(fg + 1) * 256].rearrange("(k p) n -> p k n", p=128), w1)
                        dma("sp", w3, w3[:, half * 8:(half + 1) * 8, :], FW3,
                            FW3[ex * D + half * 1024:ex * D + (half + 1) * 1024, fg * 256:(fg + 1) * 256].rearrange("(k p) n -> p k n", p=128), w3)
                    for fc in range(2):
                        f = 2 * fg + fc
                        fs = slice(fc * 128, (fc + 1) * 128)
                        psG, psU = PB[cnt % 2], PB[2 + cnt % 2]
                        sgt = sg[cnt % 2]
                        cnt += 1
                        bg.tick()
                        mm(psG, psG[:, 0:512], [(w1[:, kc, fs], h2T[:, kc, :]) for kc in range(16)], r=(w1, h2T))
                        mm(psU, psU[:, 0:512], [(w3[:, kc, fs], h2T[:, kc, :]) for kc in range(16)], r=(w3, h2T))
                        op("act", lambda e: e.activation(out=sgt[:], in_=psG[:, 0:512], func=AF.Silu), r=(psG,), w=(sgt,))
                        op("dve", lambda e: e.tensor_tensor(out=at[:, f, :], in0=psU[:, 0:512], in1=sgt[:], op=ALU.mult), r=(psU, sgt), w=(at,))
                for n in range(4):
                    ns = slice(n * 512, (n + 1) * 512)
                    for f2 in range(2):
                        w2 = w2t[wc % 2]
                        wc += 1
                        bg.tick()
                        dma("sp", w2, w2[:], FW2, FW2[ex * 2816 + f2 * 1408:ex * 2816 + (f2 + 1) * 1408, ns].rearrange("(f p) n -> p f n", p=128), w2)
                        for tt in range(4):
                            psY = PB[4 + tt]
                            mm(psY, psY[:, 0:512], [(at[:, f2 * 11 + fl, tt * 128:(tt + 1) * 128], w2[:, fl, :]) for fl in range(11)], r=(at, w2),
                               start=(f2 == 0), stop=(f2 == 1))
                    for tt in range(4):
                        psY = PB[4 + tt]
                        tmt = tm[tt % 2]
                        if moe:
                            op("dve", lambda e: e.scalar_tensor_tensor(out=tmt[:], in0=psY[:, 0:512], scalar=gt[tt][:, ex:ex + 1], in1=g2[:, ns], op0=ALU.mult, op1=ALU.mult),
                               r=(psY, gt[tt], g2), w=(tmt,))
                        else:
                            op("dve", lambda e: e.tensor_tensor(out=tmt[:], in0=psY[:, 0:512], in1=g2[:, ns], op=ALU.mult), r=(psY, g2), w=(tmt,))
                        op("pool", lambda e: e.tensor_tensor(out=xt4[tt][:, ns], in0=xt4[tt][:, ns], in1=tmt[:], op=ALU.add), r=(xt4[tt], tmt), w=(xt4[tt],))
            for tt in range(4):
                ti = st * 4 + tt
                dma("pool", xnext, xnext[ti * 128:(ti + 1) * 128, :], xt4[tt], xt4[tt][:], xt4[tt])
        k.end_phase()
        dsp.recycle()
        xcur = xnext

    return finish()


def _rel_bucket_np(n):
    n = np.maximum(n, 0)
    nf = np.maximum(n, 1).astype(np.float32)
    large = 16 + (np.log(nf / 16) / math.log(128 / 16) * 16).astype(np.int32)
    large = np.minimum(large, 31)
    return np.where(n < 16, n, large)


def _constants():
    c = {}
    c["ident"] = np.eye(128, dtype=np.float32).astype(NPBF)
    half = 32
    freqs = (10000.0 ** (-np.arange(half, dtype=np.float32) / half)).astype(np.float32)
    ang = np.arange(S, dtype=np.float32)[:, None] * freqs[None, :]
    c["cs_tab"] = np.concatenate([np.cos(ang), np.sin(ang)], axis=1).astype(np.float32)
    s_ = np.arange(128)[:, None]
    t_ = np.arange(512)[None, :]
    c["negcm"] = np.concatenate([np.where(r * 128 + s_ <= t_, 0.0, NEG) for r in range(4)], axis=1).astype(np.float32)
    qq = np.arange(128)[:, None]
    kk = np.arange(128)[None, :]
    c["negtri"] = np.where(kk <= qq, 0.0, -1e30).astype(np.float32)
    c["tri"] = (np.arange(128)[:, None] <= np.arange(128)[None, :]).astype(np.float32)
    dist = np.arange(384) - 127
    bk = _rel_bucket_np(dist)
    oh = np.zeros((32, 384), np.float32)
    for j in range(384):
        if dist[j] >= 0:
            oh[bk[j], j] = 1.0
    c["ohs"] = oh.copy()
    ohd = oh.copy()
    ohd[31, dist >= 0] -= 1.0
    c["ohd"] = ohd
    tt = np.arange(256)[None, :]
    dd = tt - s_
    c["negw"] = np.where((dd >= 0) & (dd < 128), 0.0, NEG).astype(np.float32)
    return c


def _prep_inputs(inp, nl=DEPTH, cores=8):
    f = lambda a: np.ascontiguousarray(np.asarray(a, dtype=np.float32))
    nf_, nm_ = (nl + 1) // 2, nl // 2
    w_in = f(inp["w_in"][:nl])
    offs = np.cumsum([0, 512, 256, 64, 512, 512, 512, 4, 512, 128, 128, 512, 64, 8, 512, 128, 128])
    (cq, ckv, kr, fq, fk, fv, fg, dq, dk, dv, iq, ik, iw, sq, sk, sv) = [slice(offs[i], offs[i + 1]) for i in range(16)]
    wp = np.zeros((nl, D, 5120), np.float32)

    def put(c, o, sl):
        wp[:, :, c * 512 + o:c * 512 + o + (sl.stop - sl.start)] = w_in[:, :, sl]
    put(0, 0, cq); put(1, 0, ckv); put(1, 256, kr); put(2, 0, fq); put(3, 0, fk); put(4, 0, fv); put(5, 0, dq)
    put(6, 0, dk); put(6, 128, dv); put(6, 256, ik); put(6, 320, iw); put(6, 328, fg)
    put(7, 0, iq); put(8, 0, sq); put(9, 0, sk); put(9, 128, sv)
    uq = f(inp["mla_w_uq"][:nl]).reshape(nl, 512, 4, 192)
    uqp = np.concatenate([uq[..., :128].reshape(nl, 512, 512), uq[..., 128:].reshape(nl, 512, 256)], axis=-1)
    ukv = f(inp["mla_w_ukv"][:nl]).reshape(nl, 256, 4, 256)
    ukvp = np.concatenate([ukv[..., :128].reshape(nl, 256, 512), ukv[..., 128:].reshape(nl, 256, 512)], axis=-1)
    small = np.concatenate([f(inp[n][:nl]) for n in ("mla_cq_norm", "mla_ckv_norm", "mla_q_norm", "mla_k_norm", "fox_q_norm", "fox_k_norm",
                                                      "fox_f_bias", "dsa_q_norm", "dsa_k_norm", "swa_q_norm", "swa_k_norm", "swa_sinks")], axis=1)
    assert small.shape == (nl, NSM)
    shared = {
        "ada_w": f(inp["ada_w"][:nl]).reshape(nl * D, 6 * D), "ada_b": f(inp["ada_b"][:nl]),
        "norm_mix": f(inp["norm_mix"][:nl]), "norm_ffn": f(inp["norm_ffn"][:nl]), "small": np.ascontiguousarray(small),
        "w_in_p": wp.reshape(nl * D, 5120), "w_uq_p": np.ascontiguousarray(uqp).reshape(nl * 512, 768),
        "w_ukv_p": np.ascontiguousarray(ukvp).reshape(nl * 256, 1024), "rel_bias": f(inp["rel_bias"]),
        "w_branch": f(inp["w_branch"][:nl]).reshape(nl * 4 * 512, D), "w_gate": f(inp["w_gate"][:nl]).reshape(nl * 4 * D, D),
        "w_out": f(inp["w_out"][:nl]).reshape(nl * D, D),
        "ffn_w1": f(inp["ffn_w1"][:nf_]).reshape(nf_ * D, 5632), "ffn_w3": f(inp["ffn_w3"][:nf_]).reshape(nf_ * D, 5632),
        "ffn_w2": f(inp["ffn_w2"][:nf_]).reshape(nf_ * 5632, D),
    }
    if nm_:
        shared.update({
            "moe_router": f(inp["moe_router"][:nm_]).reshape(nm_ * D, 8),
            "moe_w1": f(inp["moe_w1"][:nm_]).reshape(nm_ * 8 * D, 2816), "moe_w3": f(inp["moe_w3"][:nm_]).reshape(nm_ * 8 * D, 2816),
            "moe_w2": f(inp["moe_w2"][:nm_]).reshape(nm_ * 8 * 2816, D),
        })
    shared.update(_constants())
    maps = []
    for b in range(cores):
        m = dict(shared)
        m["x"] = f(inp["x"][b])
        m["cT"] = np.ascontiguousarray(f(inp["c"][b]).reshape(16, 128).T)
        maps.append(m)
    return maps


def kernel(**inputs):
    maps = _prep_inputs(inputs)
    nc = build_program()
    res = run_bass_kernel_spmd(nc, maps, core_ids=list(range(8)))
    return np.stack([np.asarray(r["y"], dtype=np.float32) for r in res.results], axis=0)
```
